# Optimizing a Trainium2 kernel written in Bass

```python
import jax, jax.numpy as jnp
from jax import lax
import numpy as np

D_MODEL = 2048
BATCH = 1
SEQ = 8192
DEPTH = 1
DEC_BATCH = 2
DEC_SEQ = 16384
PAST_LEN = 128

GRID_W = 64
MIX_WIDTH = D_MODEL
ATTN_WIDTH = MIX_WIDTH // 2
POOL_WIDTH = MIX_WIDTH - ATTN_WIDTH
HEAD_DIM = 32
N_HEADS = ATTN_WIDTH // HEAD_DIM
WIN_ROWS = 8
WIN_COLS = 16
POOL_WINDOWS = (2, 4, 8, 16)
N_POOL_GROUPS = len(POOL_WINDOWS)
POOL_GROUP = POOL_WIDTH // N_POOL_GROUPS
IN_WIDTH = 3 * ATTN_WIDTH + POOL_WIDTH
N_EXPERTS = 32
TOP_K = 4
D_FF = D_MODEL
SWIGLU_LIMIT = 7.0
SWIGLU_ALPHA = 1.702
MOE_BLOCK = 256
LN_EPS = 1e-5
DEEPNORM_ALPHA = float((2 * DEPTH) ** 0.25)
DEEPNORM_BETA = float((8 * DEPTH) ** -0.25)

kernel_name = "hybrid_natten_pool_moe_encoder"


def layer_norm(x, g, b):
    xf = x.astype(jnp.float32)
    mu = jnp.mean(xf, axis=-1, keepdims=True)
    var = jnp.mean(jnp.square(xf - mu), axis=-1, keepdims=True)
    return ((xf - mu) * lax.rsqrt(var + LN_EPS) * g.astype(jnp.float32) + b.astype(jnp.float32)).astype(x.dtype)


def neighbourhood_attention(q, k, v, rpb):
    bsz, seq_len, n_heads, head_dim = q.shape
    rows = seq_len // GRID_W
    kh = min(WIN_ROWS, rows)
    kw = WIN_COLS
    cols = jnp.arange(GRID_W)
    col_start = jnp.clip(cols - kw // 2, 0, GRID_W - kw)
    col_idx = col_start[:, None] + jnp.arange(kw)[None, :]
    dc = col_idx - cols[:, None] + (WIN_COLS - 1)
    qg = q.reshape(bsz, rows, GRID_W, n_heads, head_dim) * (head_dim ** -0.5)
    kg = k.reshape(bsz, rows, GRID_W, n_heads, head_dim)
    vg = v.reshape(bsz, rows, GRID_W, n_heads, head_dim)

    def one_row(r):
        rs = jnp.clip(r - kh // 2, 0, rows - kh)
        qr = lax.dynamic_index_in_dim(qg, r, axis=1, keepdims=False)
        kb = lax.dynamic_slice_in_dim(kg, rs, kh, axis=1)
        vb = lax.dynamic_slice_in_dim(vg, rs, kh, axis=1)
        kn = kb[:, :, col_idx]
        vn = vb[:, :, col_idx]
        dr = rs + jnp.arange(kh) - r + (WIN_ROWS - 1)
        bias = rpb[:, dr[:, None, None], dc[None, :, :]]
        bias = jnp.transpose(bias, (0, 2, 1, 3)).astype(jnp.float32)
        s = jnp.einsum('bqhd,brqwhd->bhqrw', qr, kn).astype(jnp.float32) + bias[None]
        p = jax.nn.softmax(s, axis=(-2, -1)).astype(vn.dtype)
        return jnp.einsum('bhqrw,brqwhd->bqhd', p, vn)

    out = lax.map(one_row, jnp.arange(rows))
    out = jnp.transpose(out, (1, 0, 2, 3, 4))
    return out.reshape(bsz, seq_len, n_heads * head_dim)


def multiscale_pool(u, w_pool, pool_scale):
    bsz, seq_len, _ = u.shape
    uf = u.astype(jnp.float32).reshape(bsz, seq_len, N_POOL_GROUPS, POOL_GROUP)
    cs = jnp.concatenate([jnp.zeros((bsz, 1, N_POOL_GROUPS, POOL_GROUP), jnp.float32),
                          lax.cumsum(uf, axis=1)], axis=1)
    t = jnp.arange(seq_len)[:, None]
    half = jnp.array([w // 2 for w in POOL_WINDOWS], dtype=jnp.int32)[None, :]
    lo = jnp.clip(t - half, 0, seq_len)
    hi = jnp.clip(t + half, 0, seq_len)
    gidx = jnp.arange(N_POOL_GROUPS)[None, :]
    window_sum = cs[:, hi, gidx] - cs[:, lo, gidx]
    cnt = (hi - lo).astype(jnp.float32)[None, :, :, None]
    pooled = window_sum / cnt - uf
    mixed = jnp.einsum('blgc,gcd->blgd', pooled, w_pool.astype(jnp.float32))
    mixed = mixed * pool_scale.astype(jnp.float32).reshape(N_POOL_GROUPS, POOL_GROUP)
    return mixed.reshape(bsz, seq_len, POOL_WIDTH).astype(u.dtype)


def moe(h, w_router, b_router, w_gate_up, b_gate_up, w_down, b_down):
    bsz, seq_len, d = h.shape
    x = h.reshape(-1, d)
    n = x.shape[0]
    logits = (x @ w_router + b_router).astype(jnp.float32)
    top_val, top_idx = lax.top_k(logits, TOP_K)
    gates = jax.nn.softmax(top_val, axis=-1)
    n_assign = n * TOP_K
    e_flat = top_idx.reshape(-1).astype(jnp.int32)
    g_flat = gates.reshape(-1)
    tok_flat = jnp.arange(n_assign, dtype=jnp.int32) // TOP_K
    order = jnp.argsort(e_flat)
    e_sorted = e_flat[order]
    counts = jnp.zeros((N_EXPERTS,), jnp.int32).at[e_flat].add(1)
    padded = (counts + MOE_BLOCK - 1) // MOE_BLOCK * MOE_BLOCK
    off = jnp.cumsum(counts) - counts
    pend = jnp.cumsum(padded)
    poff = pend - padded
    dest = poff[e_sorted] + (jnp.arange(n_assign, dtype=jnp.int32) - off[e_sorted])
    n_slots = n_assign + N_EXPERTS * MOE_BLOCK
    n_blocks = n_slots // MOE_BLOCK
    slot_tok = jnp.full((n_slots,), n, jnp.int32).at[dest].set(tok_flat[order])
    slot_gate = jnp.zeros((n_slots,), jnp.float32).at[dest].set(g_flat[order])
    block_start = jnp.arange(n_blocks, dtype=jnp.int32) * MOE_BLOCK
    block_e = jnp.minimum(jnp.sum(block_start[:, None] >= pend[None, :], axis=1), N_EXPERTS - 1).astype(jnp.int32)
    x_pad = jnp.concatenate([x, jnp.zeros((1, d), x.dtype)], axis=0)

    def step(acc, blk):
        tok, gate, e = blk
        xb = x_pad[tok]
        gu = xb @ w_gate_up[e] + b_gate_up[e]
        g_lin = jnp.minimum(gu[:, :D_FF], SWIGLU_LIMIT)
        up = jnp.clip(gu[:, D_FF:], -SWIGLU_LIMIT, SWIGLU_LIMIT)
        act = (up + 1.0) * (g_lin * jax.nn.sigmoid(SWIGLU_ALPHA * g_lin))
        yb = act @ w_down[e] + b_down[e]
        return acc.at[tok].add(yb.astype(jnp.float32) * gate[:, None]), None

    acc, _ = lax.scan(step, jnp.zeros((n + 1, d), jnp.float32),
                      (slot_tok.reshape(n_blocks, MOE_BLOCK), slot_gate.reshape(n_blocks, MOE_BLOCK), block_e))
    return acc[:n].reshape(bsz, seq_len, d).astype(h.dtype)


def encoder_layer(x, w_in, rpb, w_pool, pool_scale, w_out, ln1_g, ln1_b,
                  w_router, b_router, w_gate_up, b_gate_up, w_down, b_down, ln2_g, ln2_b):
    bsz, seq_len, _ = x.shape
    proj = x @ w_in
    q = proj[..., :ATTN_WIDTH].reshape(bsz, seq_len, N_HEADS, HEAD_DIM)
    k = proj[..., ATTN_WIDTH:2 * ATTN_WIDTH].reshape(bsz, seq_len, N_HEADS, HEAD_DIM)
    v = proj[..., 2 * ATTN_WIDTH:3 * ATTN_WIDTH].reshape(bsz, seq_len, N_HEADS, HEAD_DIM)
    u = proj[..., 3 * ATTN_WIDTH:]
    attn = neighbourhood_attention(q, k, v, rpb)
    pool = multiscale_pool(u, w_pool, pool_scale)
    mix = jnp.concatenate([attn, pool], axis=-1) @ w_out
    h = layer_norm(DEEPNORM_ALPHA * x + mix, ln1_g, ln1_b)
    f = moe(h, w_router, b_router, w_gate_up, b_gate_up, w_down, b_down)
    return layer_norm(DEEPNORM_ALPHA * h + f, ln2_g, ln2_b)


def trunk(x, ln_in_g, ln_in_b, w_in, rpb, w_pool, pool_scale, w_out, ln1_g, ln1_b,
          w_router, b_router, w_gate_up, b_gate_up, w_down, b_down, ln2_g, ln2_b):
    h = layer_norm(x, ln_in_g, ln_in_b)
    for l in range(DEPTH):
        h = encoder_layer(h, w_in[l], rpb[l], w_pool[l], pool_scale[l], w_out[l], ln1_g[l], ln1_b[l],
                          w_router[l], b_router[l], w_gate_up[l], b_gate_up[l], w_down[l], b_down[l],
                          ln2_g[l], ln2_b[l])
    return h


def setup_inputs(seed: int = 0) -> dict:
    key = jax.random.key(seed)
    ks = jax.random.split(key, 20)
    f32 = jnp.float32
    nrm = lambda k, shape: jax.random.normal(k, shape, f32)
    x_prompt = nrm(ks[0], (BATCH, SEQ, D_MODEL))
    x_sample = nrm(ks[1], (DEC_BATCH, DEC_SEQ, D_MODEL))
    ln_in_g = 1.0 + 0.1 * nrm(ks[2], (D_MODEL,))
    ln_in_b = 0.01 * nrm(ks[3], (D_MODEL,))
    col_scale = jnp.concatenate([jnp.ones((2 * ATTN_WIDTH,), f32),
                                 jnp.full((ATTN_WIDTH,), DEEPNORM_BETA, f32),
                                 jnp.ones((POOL_WIDTH,), f32)])
    w_in = nrm(ks[4], (DEPTH, D_MODEL, IN_WIDTH)) * (D_MODEL ** -0.5) * col_scale
    rpb = 0.02 * nrm(ks[5], (DEPTH, N_HEADS, 2 * WIN_ROWS - 1, 2 * WIN_COLS - 1))
    w_pool = nrm(ks[6], (DEPTH, N_POOL_GROUPS, POOL_GROUP, POOL_GROUP)) * (POOL_GROUP ** -0.5)
    pool_scale = 1.0 + 0.1 * nrm(ks[7], (DEPTH, POOL_WIDTH))
    w_out = nrm(ks[8], (DEPTH, MIX_WIDTH, D_MODEL)) * (MIX_WIDTH ** -0.5) * DEEPNORM_BETA
    ln1_g = 1.0 + 0.1 * nrm(ks[9], (DEPTH, D_MODEL))
    ln1_b = 0.01 * nrm(ks[10], (DEPTH, D_MODEL))
    w_router = nrm(ks[11], (DEPTH, D_MODEL, N_EXPERTS)) * (D_MODEL ** -0.5)
    b_router = 0.01 * nrm(ks[12], (DEPTH, N_EXPERTS))
    w_gate_up = nrm(ks[13], (DEPTH, N_EXPERTS, D_MODEL, 2 * D_FF)) * (D_MODEL ** -0.5)
    b_gate_up = 0.01 * nrm(ks[14], (DEPTH, N_EXPERTS, 2 * D_FF))
    w_down = nrm(ks[15], (DEPTH, N_EXPERTS, D_FF, D_MODEL)) * (D_FF ** -0.5) * DEEPNORM_BETA
    b_down = 0.01 * nrm(ks[16], (DEPTH, N_EXPERTS, D_MODEL))
    ln2_g = 1.0 + 0.1 * nrm(ks[17], (DEPTH, D_MODEL))
    ln2_b = 0.01 * nrm(ks[18], (DEPTH, D_MODEL))
    return {"x_prompt": x_prompt, "x_sample": x_sample, "ln_in_g": ln_in_g, "ln_in_b": ln_in_b,
            "w_in": w_in, "rpb": rpb, "w_pool": w_pool, "pool_scale": pool_scale, "w_out": w_out,
            "ln1_g": ln1_g, "ln1_b": ln1_b, "w_router": w_router, "b_router": b_router,
            "w_gate_up": w_gate_up, "b_gate_up": b_gate_up, "w_down": w_down, "b_down": b_down,
            "ln2_g": ln2_g, "ln2_b": ln2_b}


def reference(x_prompt, x_sample, ln_in_g, ln_in_b, w_in, rpb, w_pool, pool_scale, w_out, ln1_g, ln1_b,
              w_router, b_router, w_gate_up, b_gate_up, w_down, b_down, ln2_g, ln2_b):
    y_prompt = trunk(x_prompt, ln_in_g, ln_in_b, w_in, rpb, w_pool, pool_scale, w_out, ln1_g, ln1_b,
                     w_router, b_router, w_gate_up, b_gate_up, w_down, b_down, ln2_g, ln2_b)
    y_sample = trunk(x_sample, ln_in_g, ln_in_b, w_in, rpb, w_pool, pool_scale, w_out, ln1_g, ln1_b,
                     w_router, b_router, w_gate_up, b_gate_up, w_down, b_down, ln2_g, ln2_b)
    return (y_prompt, y_sample)
```

```python
import contextlib
import numpy as np
import concourse.bass as bass
import concourse.mybir as mybir
from concourse.bass_utils import run_bass_kernel_spmd

F32 = mybir.dt.float32
BF16 = mybir.dt.bfloat16
I32 = mybir.dt.int32
AF = mybir.ActivationFunctionType
ALU = mybir.AluOpType
AX = mybir.AxisListType

D = 2048
KC = 16
NH = 32
GRID_W = 64
LN_EPS = 1e-5
ALPHA = float(2 ** 0.25)
NEG = -30000.0
VS = 34
UT = 1536
NMASK = 42
DBG = False


class Cfg:
    def __init__(self, E=32, DFF=2048, NU=5, NCORES=8, C=896):
        self.E, self.DFF, self.NU, self.NCORES, self.C = E, DFF, NU, NCORES, C
        self.FC = DFF // 128
        self.CT = C // 128
        self.NT = NU * 8
        self.NSLOT = E * C + 128
        self.DUMP = E * C
        ns, s = [], 0
        while s < C:
            n = min(512, C - s)
            if C % 448 == 0:
                n = min(448, C - s)
            ns.append((s, n)); s += n
        self.NS = ns


def pair_offsets(p):
    if p == 0:
        return [-2, -1, 0, 1, 2, 3]
    if p == 7:
        return [-3, -2, -1, 0, 1, 2]
    return [-2, -1, 0, 1, 2]


def mask_index(p, i):
    if p == 0:
        return i
    return 6 + (p - 1) * 5 + i


class Trk:
    def __init__(self, nc, es):
        self.nc = nc
        self.eng = {"pe": nc.tensor, "act": nc.scalar, "dve": nc.vector, "pool": nc.gpsimd, "sp": nc.sync}
        self.sem = {}
        self.cnt = {}
        for e in ("pe", "act", "dve", "pool"):
            self.sem[e] = es.enter_context(nc.semaphore("sem_" + e))
            self.cnt[e] = 0
        self.dq = {}
        for q, k in (("sp", 8), ("pool", 8), ("act", 4)):
            sems = []
            for i in range(k):
                name = "dq_%s_%d" % (q, i)
                self.sem[name] = es.enter_context(nc.semaphore(name))
                self.cnt[name] = 0
                sems.append(name)
            self.dq[q] = [sems, 0]
        self.state = {}
        self.seen = {e: {} for e in self.eng}

    def _st(self, k):
        s = self.state.get(k)
        if s is None:
            s = {"w": None, "r": {}}
            self.state[k] = s
        return s

    def _waits(self, engine, reads, writes, extra=()):
        need = {}

        def add(ev):
            if ev is None:
                return
            s, v = ev
            if engine == "pe" and s == "pe":
                return
            if need.get(s, 0) < v:
                need[s] = v

        for k in reads:
            add(self._st(k)["w"])
        for k in writes:
            st = self._st(k)
            add(st["w"])
            for s, v in st["r"].items():
                add((s, v))
        for ev in extra:
            add(ev)
        seen = self.seen[engine]
        for s, v in need.items():
            if seen.get(s, 0) >= v:
                continue
            self.eng[engine].wait_ge(self.sem[s], v)
            seen[s] = v

    def _record(self, ev, reads, writes):
        s, v = ev
        for k in reads:
            r = self._st(k)["r"]
            if r.get(s, 0) < v:
                r[s] = v
        for k in writes:
            self.state[k] = {"w": ev, "r": {}}

    def op(self, engine, fn, reads=(), writes=(), signal=True):
        self._waits(engine, reads, writes)
        ins = fn()
        if signal:
            self.cnt[engine] += 1
            ins.then_inc(self.sem[engine], 1)
            ev = (engine, self.cnt[engine])
        else:
            ev = (engine, self.cnt[engine] + 1)
        self._record(ev, reads, writes)
        return ins

    def dma(self, q, fn, reads=(), writes=()):
        sems, i = self.dq[q]
        name = sems[i % len(sems)]
        self.dq[q][1] = i + 1
        prev = (name, self.cnt[name])
        self._waits(q, reads, writes, extra=(prev,) if self.cnt[name] else ())
        ins = fn(self.eng[q])
        self.cnt[name] += 16
        ins.then_inc(self.sem[name], 16)
        ev = (name, self.cnt[name])
        self._record(ev, reads, writes)
        return ev

    def wait_all(self, engine, keys):
        self.barrier()

    def barrier(self):
        evs = [(s, v) for s, v in self.cnt.items() if v > 0]
        for e in ("pe", "act", "dve", "pool", "sp"):
            seen = self.seen[e]
            for s, v in evs:
                if s == e and e == "pe":
                    continue
                if seen.get(s, 0) >= v:
                    continue
                self.eng[e].wait_ge(self.sem[s], v)
                seen[s] = v
        self.state = {}


def build(cfg):
    nc = bass.Bass("TRN2", target_bir_lowering=False)
    E, DFF, FC, NU, C, CT, NT = cfg.E, cfg.DFF, cfg.FC, cfg.NU, cfg.C, cfg.CT, cfg.NT
    NSLOT, DUMP = cfg.NSLOT, cfg.DUMP

    def din(name, shape, dt=F32):
        return nc.dram_tensor(name, list(shape), dt, kind="ExternalInput").ap()

    def dscr(name, shape, dt):
        return nc.dram_tensor(name, list(shape), dt, kind="Internal").ap()

    x_in = din("x_in", [NU * UT, D])
    mask_in = din("mask_in", [NU * 128, NMASK * 128])
    bias_in = din("bias_in", [NH * 128, 7 * 128])
    pv_in = din("pv_in", [NU, 16])
    rc_in = din("rc_in", [NU, 64])
    lnv = din("lnv", [6, D])
    lncol = din("lncol", [128, 32])
    w_in = din("w_in", [D, 4096])
    w_pool = din("w_pool", [4 * 256, 256])
    pscol = din("pscol", [128, 8])
    w_out = din("w_out", [D, D])
    w_router = din("w_router", [D, E])
    b_router = din("b_router", [1, E])
    w_gu = din("w_gu", [E * D, 2 * DFF])
    bgcol = din("bgcol", [128, E * 2 * FC])
    w_dn = din("w_dn", [E * DFF, D])
    b_dn = din("b_dn", [E, D])
    consts = din("consts", [128, 128 * 3 + 8 + E])
    y_out = nc.dram_tensor("y_out", [NU * 1024, D], F32, kind="ExternalOutput").ap()

    qt_d = dscr("qt_d", [1024, 1024], BF16)
    kt_d = dscr("kt_d", [1024, UT], BF16)
    v_d = dscr("v_d", [UT, NH * VS], BF16)
    pm_d = dscr("pm_d", [1024, 1024], BF16)
    h_d = dscr("h_d", [NT * 128, D], F32)
    xg_d = dscr("xg_d", [NSLOT, D], BF16)
    yy_d = dscr("yy_d", [NSLOT, D], F32)

    es = contextlib.ExitStack()
    with es:
        T = Trk(nc, es)
        op, dma = T.op, T.dma

        uid = [0]

        def sb(stack, name, shape, dt):
            uid[0] += 1
            return stack.enter_context(nc.sbuf_tensor("%s_%d" % (name, uid[0]), list(shape), dt))

        PS = [es.enter_context(nc.psum_tensor("ps%d" % i, [128, 1024], F32)) for i in range(2)]
        PO = [es.enter_context(nc.psum_tensor("po%d" % i, [128, 512], F32)) for i in range(2)]
        PT = [es.enter_context(nc.psum_tensor("pt%d" % i, [128, 512], BF16)) for i in range(2)]
        banks = [(PS[0], 0), (PS[0], 512), (PS[1], 0), (PS[1], 512), (PO[0], 0), (PO[1], 0)]

        def bank(i, n=512):
            t, o = banks[i]
            return t[:, o:o + n]

        def bkey(i):
            return "bank%d" % i

        pskeys = [("bank0", "bank1"), ("bank2", "bank3")]
        ptkey = ["ptb0", "ptb1"]

        cst = sb(es, "cst", [128, 128 * 3 + 8 + E], F32)
        ident_b = sb(es, "ident_b", [128, 128], BF16)
        ustr_b = sb(es, "ustr_b", [128, 128], BF16)
        ones_b = sb(es, "ones_b", [128, 128], BF16)
        SL = sb(es, "SL", [128, NT, 4], I32)
        GT = sb(es, "GT", [128, NT, 4], F32)
        cum = sb(es, "cum", [128, E], F32)
        zer = sb(es, "zer", [128, D], F32)
        dma("sp", lambda q: q.dma_start(out=cst[:], in_=consts), writes=["cst"])
        ident_f = cst[:, 0:128]
        hm = cst[:, 384:388]
        ebase = cst[:, 392:392 + E]
        op("dve", lambda: nc.vector.tensor_copy(out=ident_b[:], in_=cst[:, 0:128]), ["cst"], ["ident_b"])
        op("dve", lambda: nc.vector.tensor_copy(out=ustr_b[:], in_=cst[:, 128:256]), ["cst"], ["ustr_b"])
        op("dve", lambda: nc.vector.tensor_copy(out=ones_b[:], in_=cst[:, 256:384]), ["cst"], ["ones_b"])
        op("dve", lambda: nc.vector.memset(cum[:], 0.0), [], ["cum"])
        op("pool", lambda: nc.gpsimd.memset(zer[:], 0.0), [], ["zer"])
        dma("sp", lambda q: q.dma_start(out=yy_d[DUMP:DUMP + 128, :], in_=zer[:]), ["zer"], ["yy_dump"])
        zb = zer[:].bitcast(BF16)
        for r0 in range(0, NSLOT, 256):
            nr = min(256, NSLOT - r0)
            dma("sp", lambda q, r0=r0, nr=nr: q.dma_start(
                out=xg_d[r0:r0 + nr, :].rearrange("(p two) d -> p (two d)", two=nr // 128), in_=zb[:, 0:D * (nr // 128)]),
                ["zer"], ["xg_zero"])

        def ln_stats(xt_ap, xkey, st6, mv, rstd, tag):
            for c in range(4):
                op("dve", lambda c=c: nc.vector.bn_stats(out=st6[:, c * 6:(c + 1) * 6], in_=xt_ap[:, c * 512:(c + 1) * 512]),
                   [xkey], [tag + "st6"])
            op("dve", lambda: nc.vector.bn_aggr(out=mv, in_=st6[:, 0:24]), [tag + "st6"], [tag + "mv"])
            op("dve", lambda: nc.vector.tensor_scalar(out=rstd, in0=mv[:, 1:2], scalar1=LN_EPS, scalar2=None, op0=ALU.add),
               [tag + "mv"], [tag + "rstd"])
            op("act", lambda: nc.scalar.activation(out=rstd, in_=rstd, func=AF.Sqrt), [tag + "rstd"], [tag + "rstd"])
            op("dve", lambda: nc.vector.reciprocal(out=rstd, in_=rstd), [tag + "rstd"], [tag + "rstd"])

        for u in range(NU):
            xu = x_in[u * UT:(u + 1) * UT, :]
            with contextlib.ExitStack() as us:
                stats = sb(us, "stats", [128, 8, 2], F32)
                with contextlib.ExitStack() as a:
                    XT = sb(a, "XT", [128, KC, UT], BF16)
                    xt = [sb(a, "xt%d" % i, [128, D], F32) for i in range(2)]
                    xh = sb(a, "xh", [128, D], BF16)
                    st6 = sb(a, "st6", [128, 24], F32)
                    mv = sb(a, "mv", [128, 2], F32)
                    rstd = sb(a, "rstd", [128, 1], F32)
                    lnc = sb(a, "lnc", [128, 32], F32)
                    wr = [sb(a, "wr%d" % i, [128, KC, 256], BF16) for i in range(3)]
                    QTs = sb(a, "QTs", [128, 1024], BF16)
                    KTs = sb(a, "KTs", [128, UT], BF16)
                    Vb = sb(a, "Vb", [128, 12, NH, VS], BF16)
                    UF = sb(a, "UF", [128, UT], F32)
                    PA = sb(a, "PA", [128, UT], F32)
                    PB = sb(a, "PB", [128, UT], F32)
                    PTg = sb(a, "PTg", [128, 2, 1024], BF16)
                    PMs = sb(a, "PMs", [128, 1024], BF16)
                    wp = sb(a, "wp", [128, 4, 2, 256], BF16)
                    psc = sb(a, "psc", [128, 8], F32)
                    pvb = sb(a, "pvb", [128, 16], F32)
                    rcb = sb(a, "rcb", [128, 64], F32)
                    tmp8 = sb(a, "tmp8", [128, 8], F32)

                    dma("sp", lambda q: q.dma_start(out=lnc[:], in_=lncol), writes=["lnc"])
                    dma("sp", lambda q: q.dma_start(out=psc[:], in_=pscol), writes=["psc"])
                    dma("sp", lambda q: q.dma_start(out=pvb[:], in_=pv_in[u:u + 1, :].partition_broadcast(128)), writes=["pvb"])
                    dma("sp", lambda q: q.dma_start(out=rcb[:], in_=rc_in[u:u + 1, :].partition_broadcast(128)), writes=["rcb"])
                    dma("pool", lambda q: q.dma_start(out=wp[:], in_=w_pool.rearrange("(g k p) n -> p g k n", g=4, k=2, p=128)),
                        writes=["wp"])
                    op("pool", lambda: nc.gpsimd.memset(Vb[:], 1.0), [], ["Vb"])

                    for t in range(12):
                        xb = xt[t % 2]
                        xk = "xt%d" % (t % 2)
                        dma("sp", lambda q, t=t, xb=xb: q.dma_start(out=xb[:], in_=xu[t * 128:(t + 1) * 128, :]), writes=[xk])
                        ln_stats(xb, xk, st6, mv[:], rstd[:], "a")
                        if 2 <= t < 10:
                            op("dve", lambda t=t: nc.vector.tensor_copy(out=stats[:, t - 2, 0:1], in_=mv[:, 0:1]), ["amv"], ["stats"])
                            op("dve", lambda t=t: nc.vector.tensor_copy(out=stats[:, t - 2, 1:2], in_=rstd[:]), ["arstd"], ["stats"])
                        op("dve", lambda xb=xb: nc.vector.tensor_scalar(out=xh[:], in0=xb[:], scalar1=mv[:, 0:1], scalar2=rstd[:],
                                                                       op0=ALU.subtract, op1=ALU.mult),
                           [xk, "amv", "arstd"], ["xh"])
                        for cg in range(4):
                            pt = PT[cg % 2]
                            pk = ptkey[cg % 2]
                            for ci in range(4):
                                c = cg * 4 + ci
                                op("pe", lambda c=c, ci=ci, pt=pt: nc.tensor.transpose(out=pt[:, ci * 128:(ci + 1) * 128],
                                                                                      in_=xh[:, c * 128:(c + 1) * 128], identity=ident_b[:]),
                                   ["xh", "ident_b"], [pk], signal=(ci == 3))
                            for ci in range(4):
                                c = cg * 4 + ci
                                op("act", lambda c=c, ci=ci, pt=pt, t=t: nc.scalar.activation(
                                    out=XT[:, c, t * 128:(t + 1) * 128], in_=pt[:, ci * 128:(ci + 1) * 128], func=AF.Identity,
                                    bias=lnc[:, 16 + c:17 + c], scale=lnc[:, c:c + 1]), [pk, "lnc"], ["XT"])

                    w_in_v = w_in.rearrange("(k p) n -> p k n", p=128)
                    for pc in range(16):
                        wb = wr[pc % 3]
                        wk = "wr%d" % (pc % 3)
                        dma("pool", lambda q, pc=pc, wb=wb: q.dma_start(out=wb[:], in_=w_in_v[:, :, pc * 256:(pc + 1) * 256]), writes=[wk])
                        kind = pc // 4
                        if kind in (0, 1):
                            for mi in range(2):
                                mc = (pc % 4) * 2 + mi
                                ntl = [(256, 512), (768, 512)] if kind == 0 else [(0, 512), (512, 512), (1024, 512)]
                                dst = QTs if kind == 0 else KTs
                                dk = "QTs" if kind == 0 else "KTs"
                                for j, (n0, nn) in enumerate(ntl):
                                    bi = j % 2
                                    for k in range(KC):
                                        op("pe", lambda k=k, mi=mi, n0=n0, nn=nn, bi=bi, wb=wb: nc.tensor.matmul(
                                            bank(bi, nn), lhsT=wb[:, k, mi * 128:(mi + 1) * 128], rhs=XT[:, k, n0:n0 + nn],
                                            start=(k == 0), stop=(k == KC - 1)), [wk, "XT"], [bkey(bi)], signal=(k == KC - 1))
                                    d0 = n0 - 256 if kind == 0 else n0
                                    op("act", lambda d0=d0, nn=nn, bi=bi, dst=dst, kind=kind: nc.scalar.activation(
                                        out=dst[:, d0:d0 + nn], in_=bank(bi, nn), func=AF.Copy,
                                        scale=(32.0 ** -0.5 if kind == 0 else 1.0)), [bkey(bi)], [dk])
                                dd = qt_d if kind == 0 else kt_d
                                dma("sp", lambda q, mc=mc, dst=dst, dd=dd: q.dma_start(out=dd[mc * 128:(mc + 1) * 128, :], in_=dst[:]),
                                    [dk], ["qk_d"])
                        elif kind == 2:
                            h0 = (pc % 4) * 8
                            for t in range(12):
                                bi = t % 2
                                for k in range(KC):
                                    op("pe", lambda k=k, t=t, bi=bi, wb=wb: nc.tensor.matmul(
                                        bank(bi, 256), lhsT=XT[:, k, t * 128:(t + 1) * 128], rhs=wb[:, k, :],
                                        start=(k == 0), stop=(k == KC - 1)), [wk, "XT"], [bkey(bi)], signal=(k == KC - 1))
                                op("dve", lambda t=t, bi=bi, h0=h0: nc.vector.tensor_copy(
                                    out=Vb[:, t, h0:h0 + 8, 0:32], in_=bank(bi, 256).rearrange("p (h d) -> p h d", h=8)),
                                   [bkey(bi)], ["Vb"])
                            if pc % 4 == 3:
                                dma("sp", lambda q: q.dma_start(out=v_d.rearrange("(t p) f -> p t f", p=128),
                                                                in_=Vb[:].rearrange("p t h s -> p t (h s)")), ["Vb"], ["v_d"])
                        else:
                            g = pc % 4
                            for mi in range(2):
                                for j, (n0, nn) in enumerate([(128, 512), (640, 512), (1152, 256)]):
                                    bi = j % 2
                                    for k in range(KC):
                                        op("pe", lambda k=k, mi=mi, n0=n0, nn=nn, bi=bi, wb=wb: nc.tensor.matmul(
                                            bank(bi, nn), lhsT=wb[:, k, mi * 128:(mi + 1) * 128], rhs=XT[:, k, n0:n0 + nn],
                                            start=(k == 0), stop=(k == KC - 1)), [wk, "XT"], [bkey(bi)], signal=(k == KC - 1))
                                    op("act", lambda n0=n0, nn=nn, bi=bi: nc.scalar.activation(
                                        out=UF[:, n0:n0 + nn], in_=bank(bi, nn), func=AF.Copy), [bkey(bi)], ["UF"])
                                op("dve", lambda: nc.vector.tensor_tensor(out=UF[:, 248:256], in0=UF[:, 248:256], in1=pvb[:, 0:8], op=ALU.mult),
                                   ["UF", "pvb"], ["UF"])
                                op("dve", lambda: nc.vector.tensor_tensor(out=UF[:, 1280:1288], in0=UF[:, 1280:1288], in1=pvb[:, 8:16], op=ALU.mult),
                                   ["UF", "pvb"], ["UF"])
                                lo, hi = 192, 1344
                                op("dve", lambda: nc.vector.tensor_tensor(out=PA[:, lo:hi], in0=UF[:, lo - 1:hi - 1], in1=UF[:, lo:hi], op=ALU.add),
                                   ["UF"], ["PA"])
                                cur, oth, ck, ok_ = PA, PB, "PA", "PB"
                                sh = 1
                                for lvl in range(g):
                                    lo += sh; hi -= sh
                                    op("dve", lambda cur=cur, oth=oth, lo=lo, hi=hi, sh=sh: nc.vector.tensor_tensor(
                                        out=oth[:, lo:hi], in0=cur[:, lo - sh:hi - sh], in1=cur[:, lo + sh:hi + sh], op=ALU.add), [ck], [ok_])
                                    cur, oth, ck, ok_ = oth, cur, ok_, ck
                                    sh *= 2
                                wsz = 2 << g
                                op("dve", lambda cur=cur, mi=mi, wsz=wsz: nc.vector.scalar_tensor_tensor(
                                    out=PTg[:, mi, :], in0=cur[:, 256:1280], scalar=1.0 / wsz, in1=UF[:, 256:1280],
                                    op0=ALU.mult, op1=ALU.subtract), [ck, "UF"], ["PTg"])
                                for (c0, r0) in ((256, 0), (1272, 8)):
                                    op("dve", lambda cur=cur, c0=c0, r0=r0, g=g: nc.vector.tensor_tensor(
                                        out=tmp8[:], in0=cur[:, c0:c0 + 8], in1=rcb[:, g * 16 + r0:g * 16 + r0 + 8], op=ALU.mult),
                                       [ck, "rcb"], ["tmp8"])
                                    op("dve", lambda c0=c0, mi=mi: nc.vector.tensor_tensor(
                                        out=PTg[:, mi, c0 - 256:c0 - 248], in0=tmp8[:], in1=UF[:, c0:c0 + 8], op=ALU.subtract),
                                       ["tmp8", "UF"], ["PTg"])
                            for half in range(2):
                                oc = 2 * g + half
                                for nt in range(2):
                                    bi = nt
                                    for kc in range(2):
                                        op("pe", lambda kc=kc, half=half, nt=nt, bi=bi, g=g: nc.tensor.matmul(
                                            bank(bi, 512), lhsT=wp[:, g, kc, half * 128:(half + 1) * 128], rhs=PTg[:, kc, nt * 512:(nt + 1) * 512],
                                            start=(kc == 0), stop=(kc == 1)), ["wp", "PTg"], [bkey(bi)], signal=(kc == 1))
                                    op("act", lambda nt=nt, bi=bi, oc=oc: nc.scalar.activation(
                                        out=PMs[:, nt * 512:(nt + 1) * 512], in_=bank(bi, 512), func=AF.Identity, scale=psc[:, oc:oc + 1]),
                                       [bkey(bi), "psc"], ["PMs"])
                                dma("sp", lambda q, oc=oc: q.dma_start(out=pm_d[oc * 128:(oc + 1) * 128, :], in_=PMs[:]), ["PMs"], ["pm_d"])
                    T.wait_all("sp", ["XT", "wr0", "wr1", "wr2", "Vb", "QTs", "KTs", "PMs", "UF", "PA", "PB", "PTg", "xt0", "xt1", "xh"])

                CATT = sb(us, "CATT", [128, KC, 1024], BF16)
                with contextlib.ExitStack() as b:
                    QT = sb(b, "QT", [128, 8, 1024], BF16)
                    KT = sb(b, "KT", [128, 8, UT], BF16)
                    V = sb(b, "V", [128, 12, NH, VS], BF16)
                    MK = sb(b, "MK", [128, NMASK, 128], BF16)
                    BS = [sb(b, "BS%d" % i, [128, 7, 128], BF16) for i in range(2)]
                    QH = [sb(b, "QH%d" % i, [128, 1024], BF16) for i in range(2)]
                    PE_ = [sb(b, "PE%d" % i, [128, 768], BF16) for i in range(3)]
                    ATM = sb(b, "ATM", [128, 8, 1024], BF16)
                    rden = sb(b, "rden", [128, 8], F32)
                    dma("sp", lambda q: q.dma_start(out=QT[:], in_=qt_d.rearrange("(c p) t -> p c t", p=128)), ["qk_d"], ["QT"])
                    dma("sp", lambda q: q.dma_start(out=KT[:], in_=kt_d.rearrange("(c p) t -> p c t", p=128)), ["qk_d"], ["KT"])
                    dma("sp", lambda q: q.dma_start(out=V[:].rearrange("p t h s -> p t (h s)"),
                                                    in_=v_d.rearrange("(t p) f -> p t f", p=128)), ["v_d"], ["V"])
                    dma("pool", lambda q: q.dma_start(out=MK[:].rearrange("p m k -> p (m k)"), in_=mask_in[u * 128:(u + 1) * 128, :]),
                        writes=["MK"])
                    dma("sp", lambda q: q.dma_start(out=CATT[:, 8:16, :], in_=pm_d.rearrange("(c p) t -> p c t", p=128)), ["pm_d"], ["CATTp"])
                    ei = 0
                    for h in range(NH):
                        bs = BS[h % 2]; bsk = "BS%d" % (h % 2)
                        qh = QH[h % 2]; qhk = "QH%d" % (h % 2)
                        dma("pool", lambda q, h=h, bs=bs: q.dma_start(out=bs[:].rearrange("p o k -> p (o k)"),
                                                                      in_=bias_in[h * 128:(h + 1) * 128, :]), writes=[bsk])
                        op("dve", lambda h=h, qh=qh: nc.vector.tensor_scalar(out=qh[:], in0=QT[:, h // 4, :], scalar1=hm[:, h % 4:h % 4 + 1],
                                                                            scalar2=None, op0=ALU.mult), ["QT", "cst"], [qhk])
                        po = PO[h % 2]; pok = "po%d" % (h % 2)
                        for p in range(8):
                            offs = pair_offsets(p)
                            ps = PS[(h * 8 + p) % 2]; psk = "psS%d" % ((h * 8 + p) % 2)
                            for i, o in enumerate(offs):
                                kt0 = (2 + p + o) * 128
                                sl = ps[:, i * 128:(i + 1) * 128]
                                op("pe", lambda sl=sl, kt0=kt0, h=h, p=p, qh=qh: nc.tensor.matmul(
                                    sl, lhsT=KT[:, h // 4, kt0:kt0 + 128], rhs=qh[:, p * 128:(p + 1) * 128], start=True, stop=False),
                                   ["KT", qhk], [psk], signal=False)
                                op("pe", lambda sl=sl, o=o, bs=bs: nc.tensor.matmul(
                                    sl, lhsT=bs[:, o + 3, :], rhs=ident_b[:], start=False, stop=False), [bsk, "ident_b"], [psk], signal=False)
                                mi_ = mask_index(p, i)
                                op("pe", lambda sl=sl, mi_=mi_: nc.tensor.matmul(
                                    sl, lhsT=MK[:, mi_, :], rhs=ident_b[:], start=False, stop=True), ["MK", "ident_b"], [psk],
                                   signal=(i == len(offs) - 1))
                            pe_ = PE_[ei % 3]; pek = "PE%d" % (ei % 3); ei += 1
                            n = len(offs) * 128
                            op("act", lambda pe_=pe_, ps=ps: nc.scalar.activation(out=pe_[:, 0:512], in_=ps[:, 0:512], func=AF.Exp),
                               [psk], [pek])
                            op("act", lambda pe_=pe_, ps=ps, n=n: nc.scalar.activation(out=pe_[:, 512:n], in_=ps[:, 512:n], func=AF.Exp),
                               [psk], [pek])
                            for i, o in enumerate(offs):
                                kt = 2 + p + o
                                op("pe", lambda i=i, kt=kt, h=h, p=p, pe_=pe_, po=po: nc.tensor.matmul(
                                    po[:, p * 64:p * 64 + 33], lhsT=pe_[:, i * 128:(i + 1) * 128], rhs=V[:, kt, h, 0:33],
                                    start=(i == 0), stop=(i == len(offs) - 1)), [pek, "V"], [pok], signal=(i == len(offs) - 1))
                        pov = po[:].rearrange("p (a s) -> p a s", s=64)
                        op("dve", lambda pov=pov: nc.vector.reciprocal(out=rden[:].unsqueeze(2), in_=pov[:, :, 32:33]), [pok], ["rden"])
                        op("dve", lambda pov=pov, h=h: nc.vector.tensor_tensor(
                            out=ATM[:, :, h * 32:(h + 1) * 32], in0=pov[:, :, 0:32], in1=rden[:].unsqueeze(2).to_broadcast([128, 8, 32]),
                            op=ALU.mult), [pok, "rden"], ["ATM"])
                    for p in range(8):
                        for cg in range(2):
                            pt = PT[cg % 2]; pk = ptkey[cg % 2]
                            for ci in range(4):
                                c = cg * 4 + ci
                                op("pe", lambda c=c, ci=ci, pt=pt, p=p: nc.tensor.transpose(
                                    out=pt[:, ci * 128:(ci + 1) * 128], in_=ATM[:, p, c * 128:(c + 1) * 128], identity=ident_b[:]),
                                   ["ATM", "ident_b"], [pk], signal=(ci == 3))
                            op("dve", lambda cg=cg, pt=pt, p=p: nc.vector.tensor_copy(
                                out=CATT[:, cg * 4:(cg + 1) * 4, p * 128:(p + 1) * 128], in_=pt[:].rearrange("p (c t) -> p c t", c=4)),
                               [pk], ["CATTa"])
                    T.wait_all("sp", ["QT", "KT", "V", "MK", "BS0", "BS1", "QH0", "QH1", "PE0", "PE1", "PE2", "ATM", "rden"])

                with contextlib.ExitStack() as c_:
                    WO = sb(c_, "WO", [128, KC, D], BF16)
                    LB = sb(c_, "LB", [128, 4, D], F32)
                    WR = sb(c_, "WR", [128, KC, E], F32)
                    BR = sb(c_, "BR", [128, E], F32)
                    xt = [sb(c_, "cxt%d" % i, [128, D], F32) for i in range(1)]
                    HP = sb(c_, "HP", [128, D], F32)
                    H = [sb(c_, "H%d" % i, [128, D], F32) for i in range(2)]
                    HB = [sb(c_, "HB%d" % i, [128, D], BF16) for i in range(1)]
                    HT = sb(c_, "HT", [128, KC, 128], F32)
                    st6 = sb(c_, "cst6", [128, 24], F32)
                    mv = sb(c_, "cmv", [128, 2], F32)
                    rstd = sb(c_, "crstd", [128, 1], F32)
                    L = sb(c_, "L", [128, E], F32)
                    LW = sb(c_, "LW", [128, E], F32)
                    OH = sb(c_, "OH", [128, 4, E], F32)
                    SELf = sb(c_, "SELf", [128, E], F32)
                    SELb = sb(c_, "SELb", [128, E], BF16)
                    CUMb = sb(c_, "CUMb", [128, E], BF16)
                    MX = sb(c_, "MX", [128, 4], F32)
                    EX = sb(c_, "EX", [128, 4], F32)
                    nm0 = sb(c_, "nm0", [128, 1], F32)
                    ssum = sb(c_, "ssum", [128, 1], F32)
                    SLOT = sb(c_, "SLOT", [128, E], F32)
                    OKf = sb(c_, "OKf", [128, E], F32)
                    TMP = sb(c_, "TMP", [128, E], F32)
                    slf = sb(c_, "slf", [128, 4], F32)
                    w_out_v = w_out.rearrange("(k p) n -> p k n", p=128)
                    for j in range(4):
                        dma("pool", lambda q, j=j: q.dma_start(out=WO[:, :, j * 512:(j + 1) * 512], in_=w_out_v[:, :, j * 512:(j + 1) * 512]),
                            writes=["WO%d" % j])
                    for j in range(4):
                        dma("sp", lambda q, j=j: q.dma_start(out=LB[:, j, :], in_=lnv[j:j + 1, :].partition_broadcast(128)), writes=["LB%d" % j if j != 2 else "LB2_"])
                    dma("sp", lambda q: q.dma_start(out=WR[:], in_=w_router.rearrange("(k p) e -> p k e", p=128)), writes=["WR"])
                    dma("sp", lambda q: q.dma_start(out=BR[:], in_=b_router[0:1, :].partition_broadcast(128)), writes=["BR"])
                    for tt in range(8):
                        tg = u * 8 + tt
                        xb = xt[0]; xk = "cxt0"
                        Hc = H[tt % 2]; hk = "H%d" % (tt % 2)
                        HBc = HB[0]; hbk = "HB0"
                        dma("sp", lambda q, tt=tt, xb=xb: q.dma_start(out=xb[:], in_=xu[256 + tt * 128:256 + (tt + 1) * 128, :]), writes=[xk])
                        for j in range(4):
                            for c in range(KC):
                                op("pe", lambda j=j, c=c, tt=tt: nc.tensor.matmul(
                                    bank(j, 512), lhsT=CATT[:, c, tt * 128:(tt + 1) * 128], rhs=WO[:, c, j * 512:(j + 1) * 512],
                                    start=(c == 0), stop=(c == KC - 1)), ["CATTa", "CATTp", "WO%d" % j], [bkey(j)], signal=(c == KC - 1))
                        op("dve", lambda xb=xb, tt=tt: nc.vector.tensor_scalar(out=xb[:], in0=xb[:], scalar1=stats[:, tt, 0:1], scalar2=stats[:, tt, 1:2],
                                                                              op0=ALU.subtract, op1=ALU.mult), [xk, "stats"], [xk])
                        op("pool", lambda xb=xb: nc.gpsimd.tensor_tensor(out=xb[:], in0=xb[:], in1=LB[:, 0, :], op=ALU.mult), [xk, "LB0"], [xk])
                        op("pool", lambda xb=xb: nc.gpsimd.tensor_tensor(out=xb[:], in0=xb[:], in1=LB[:, 1, :], op=ALU.add), [xk, "LB1"], [xk])
                        for j in range(4):
                            op("dve", lambda j=j, xb=xb: nc.vector.scalar_tensor_tensor(
                                out=HP[:, j * 512:(j + 1) * 512], in0=xb[:, j * 512:(j + 1) * 512], scalar=ALPHA, in1=bank(j, 512),
                                op0=ALU.mult, op1=ALU.add), [xk, bkey(j)], ["HP"])
                        ln_stats(HP, "HP", st6, mv[:], rstd[:], "c")
                        op("dve", lambda: nc.vector.tensor_scalar(out=HP[:], in0=HP[:], scalar1=mv[:, 0:1], scalar2=rstd[:],
                                                                  op0=ALU.subtract, op1=ALU.mult), ["HP", "cmv", "crstd"], ["HP"])
                        op("pool", lambda: nc.gpsimd.tensor_tensor(out=HP[:], in0=HP[:], in1=LB[:, 2, :], op=ALU.mult), ["HP", "LB2_"], ["HP"])
                        op("dve", lambda Hc=Hc: nc.vector.tensor_tensor(out=Hc[:], in0=HP[:], in1=LB[:, 3, :], op=ALU.add), ["HP", "LB3"], [hk])
                        dma("sp", lambda q, tg=tg, Hc=Hc: q.dma_start(out=h_d[tg * 128:(tg + 1) * 128, :], in_=Hc[:]), [hk], ["h_d"])
                        op("act", lambda Hc=Hc, HBc=HBc: nc.scalar.activation(out=HBc[:], in_=Hc[:], func=AF.Copy), [hk], [hbk])
                        for cg in range(4):
                            bi = 4 + (cg % 2)
                            for ci in range(4):
                                c = cg * 4 + ci
                                op("pe", lambda c=c, ci=ci, bi=bi, Hc=Hc: nc.tensor.transpose(
                                    out=bank(bi, 512)[:, ci * 128:(ci + 1) * 128], in_=Hc[:, c * 128:(c + 1) * 128], identity=ident_f),
                                   [hk, "cst"], [bkey(bi)], signal=(ci == 3))
                            op("act", lambda cg=cg, bi=bi: nc.scalar.activation(
                                out=HT[:, cg * 4:(cg + 1) * 4, :], in_=bank(bi, 512).rearrange("p (c t) -> p c t", c=4), func=AF.Copy),
                               [bkey(bi)], ["HT"])
                        for c in range(KC):
                            op("pe", lambda c=c: nc.tensor.matmul(bank(0, E), lhsT=HT[:, c, :], rhs=WR[:, c, :], start=(c == 0), stop=(c == KC - 1)),
                               ["HT", "WR"], [bkey(0)], signal=(c == KC - 1))
                        op("dve", lambda: nc.vector.tensor_tensor(out=L[:], in0=bank(0, E), in1=BR[:], op=ALU.add), [bkey(0), "BR"], ["L"])
                        op("dve", lambda: nc.vector.tensor_copy(out=LW[:], in_=L[:]), ["L"], ["LW"])
                        for k in range(4):
                            op("dve", lambda k=k: nc.vector.reduce_max(out=MX[:, k:k + 1], in_=LW[:], axis=AX.X), ["LW"], ["MX"])
                            op("dve", lambda k=k: nc.vector.tensor_scalar(out=OH[:, k, :], in0=LW[:], scalar1=MX[:, k:k + 1], scalar2=None,
                                                                         op0=ALU.is_equal), ["LW", "MX"], ["OH"])
                            op("dve", lambda k=k: nc.vector.scalar_tensor_tensor(out=LW[:], in0=OH[:, k, :], scalar=-1e30, in1=LW[:],
                                                                                op0=ALU.mult, op1=ALU.add), ["OH", "LW"], ["LW"])
                        op("dve", lambda: nc.vector.tensor_tensor(out=SELf[:], in0=OH[:, 0, :], in1=OH[:, 1, :], op=ALU.add), ["OH"], ["SELf"])
                        op("dve", lambda: nc.vector.tensor_tensor(out=SELf[:], in0=SELf[:], in1=OH[:, 2, :], op=ALU.add), ["OH", "SELf"], ["SELf"])
                        op("dve", lambda: nc.vector.tensor_tensor(out=SELf[:], in0=SELf[:], in1=OH[:, 3, :], op=ALU.add), ["OH", "SELf"], ["SELf"])
                        op("dve", lambda: nc.vector.tensor_copy(out=SELb[:], in_=SELf[:]), ["SELf"], ["SELb"])
                        op("dve", lambda: nc.vector.tensor_copy(out=CUMb[:], in_=cum[:]), ["cum"], ["CUMb"])
                        op("dve", lambda: nc.vector.tensor_scalar(out=nm0[:], in0=MX[:, 0:1], scalar1=-1.0, scalar2=None, op0=ALU.mult), ["MX"], ["nm0"])
                        op("act", lambda: nc.scalar.activation(out=EX[:], in_=MX[:], func=AF.Exp, bias=nm0[:], scale=1.0), ["MX", "nm0"], ["EX"])
                        op("dve", lambda: nc.vector.reduce_sum(out=ssum[:], in_=EX[:], axis=AX.X), ["EX"], ["ssum"])
                        op("dve", lambda: nc.vector.reciprocal(out=ssum[:], in_=ssum[:]), ["ssum"], ["ssum"])
                        op("dve", lambda tg=tg: nc.vector.tensor_scalar(out=GT[:, tg, :], in0=EX[:], scalar1=ssum[:], scalar2=None, op0=ALU.mult),
                           ["EX", "ssum"], ["GT"])
                        op("pe", lambda: nc.tensor.matmul(bank(1, E), lhsT=ustr_b[:], rhs=SELb[:], start=True, stop=False),
                           ["ustr_b", "SELb"], [bkey(1)], signal=False)
                        op("pe", lambda: nc.tensor.matmul(bank(1, E), lhsT=ones_b[:], rhs=CUMb[:], start=False, stop=True),
                           ["ones_b", "CUMb"], [bkey(1)], signal=True)
                        op("dve", lambda: nc.vector.tensor_scalar(out=OKf[:], in0=bank(1, E), scalar1=float(C), scalar2=None, op0=ALU.is_lt),
                           [bkey(1)], ["OKf"])
                        op("dve", lambda: nc.vector.tensor_tensor(out=SLOT[:], in0=bank(1, E), in1=ebase, op=ALU.add), [bkey(1), "cst"], ["SLOT"])
                        op("dve", lambda: nc.vector.tensor_scalar(out=SLOT[:], in0=SLOT[:], scalar1=-float(DUMP), scalar2=None, op0=ALU.add),
                           ["SLOT"], ["SLOT"])
                        op("dve", lambda: nc.vector.tensor_tensor(out=SLOT[:], in0=SLOT[:], in1=OKf[:], op=ALU.mult), ["SLOT", "OKf"], ["SLOT"])
                        op("dve", lambda: nc.vector.tensor_scalar(out=SLOT[:], in0=SLOT[:], scalar1=float(DUMP), scalar2=None, op0=ALU.add),
                           ["SLOT"], ["SLOT"])
                        op("dve", lambda: nc.vector.tensor_tensor(out=cum[:], in0=cum[:], in1=SELf[:], op=ALU.add), ["cum", "SELf", "CUMb"], ["cum"])
                        for k in range(4):
                            op("dve", lambda k=k: nc.vector.tensor_tensor(out=TMP[:], in0=OH[:, k, :], in1=SLOT[:], op=ALU.mult), ["OH", "SLOT"], ["TMP"])
                            op("dve", lambda k=k: nc.vector.reduce_sum(out=slf[:, k:k + 1], in_=TMP[:], axis=AX.X), ["TMP"], ["slf"])
                        op("dve", lambda tg=tg: nc.vector.tensor_copy(out=SL[:, tg, :], in_=slf[:]), ["slf"], ["SL"])
                        for k in range(4):
                            dma("pool", lambda q, k=k, tg=tg, HBc=HBc: q.indirect_dma_start(
                                out=xg_d[:, :], out_offset=bass.IndirectOffsetOnAxis(ap=SL[:, tg, k:k + 1], axis=0),
                                in_=HBc[:], in_offset=None), [hbk, "SL"], ["xg_d"])
                    T.wait_all("sp", ["WO", "LB", "WR", "BR", "cxt0", "cxt1", "HP", "H0", "H1", "HB0", "HB1", "HT", "CATTa", "CATTp",
                                      "L", "LW", "OH", "SELf", "SELb", "CUMb", "MX", "EX", "nm0", "ssum", "SLOT", "OKf", "TMP", "slf", "stats"])

        with contextlib.ExitStack() as m_:
            XG = [sb(m_, "XG%d" % i, [128, D], BF16) for i in range(2)]
            XGT = sb(m_, "XGT", [128, KC, C], BF16)
            ACTT = sb(m_, "ACTT", [128, FC, C], BF16)
            wr = [sb(m_, "mw%d" % i, [128, KC, 512], BF16) for i in range(3)]
            BG = sb(m_, "BG", [128, E * 2 * FC], F32)
            BG1 = sb(m_, "BG1", [128, E * 2 * FC], F32)
            bdf = [sb(m_, "bdf%d" % i, [1, D], F32) for i in range(2)]
            bdb = [sb(m_, "bdb%d" % i, [1, D], BF16) for i in range(2)]
            GL = [sb(m_, "GL%d" % i, [128, 512], F32) for i in range(2)]
            SG = [sb(m_, "SG%d" % i, [128, 512], F32) for i in range(2)]
            TU = [sb(m_, "TU%d" % i, [128, 512], F32) for i in range(2)]
            YS = [sb(m_, "YS%d" % i, [128, 512], F32) for i in range(4)]
            dma("sp", lambda q: q.dma_start(out=BG[:], in_=bgcol), writes=["BG"])
            op("dve", lambda: nc.vector.tensor_scalar(out=BG1[:], in0=BG[:], scalar1=1.0, scalar2=None, op0=ALU.add), ["BG"], ["BG1"])
            MB = 2 if FC >= 2 else 1
            wslot = 0
            epi = 0
            ysi = 0
            for e in range(E):
                w_gu_v = w_gu[e * D:(e + 1) * D, :].rearrange("(k p) (g n) -> p k g n", p=128, g=2)
                w_dn_v = w_dn[e * DFF:(e + 1) * DFF, :].rearrange("(k p) n -> p k n", p=128)
                dma("sp", lambda q, e=e: q.dma_start(out=bdf[e % 2][:], in_=b_dn[e:e + 1, :]), writes=["bdf%d" % (e % 2)])
                op("dve", lambda e=e: nc.vector.tensor_copy(out=bdb[e % 2][:], in_=bdf[e % 2][:]), ["bdf%d" % (e % 2)], ["bdb%d" % (e % 2)])
                for st in range(CT):
                    xg = XG[st % 2]; xgk = "XG%d" % (st % 2)
                    r0 = e * C + st * 128
                    dma("sp", lambda q, r0=r0, xg=xg: q.dma_start(out=xg[:], in_=xg_d[r0:r0 + 128, :]), ["xg_d"], [xgk])
                    for cg in range(4):
                        pt = PT[cg % 2]; pk = ptkey[cg % 2]
                        for ci in range(4):
                            c = cg * 4 + ci
                            op("pe", lambda c=c, ci=ci, pt=pt, xg=xg: nc.tensor.transpose(
                                out=pt[:, ci * 128:(ci + 1) * 128], in_=xg[:, c * 128:(c + 1) * 128], identity=ident_b[:]),
                               [xgk, "ident_b"], [pk], signal=(ci == 3))
                        eng = "dve" if cg % 2 == 0 else "act"
                        if eng == "dve":
                            op("dve", lambda cg=cg, pt=pt, st=st: nc.vector.tensor_copy(
                                out=XGT[:, cg * 4:(cg + 1) * 4, st * 128:(st + 1) * 128], in_=pt[:].rearrange("p (c t) -> p c t", c=4)),
                               [pk], ["XGT"])
                        else:
                            op("act", lambda cg=cg, pt=pt, st=st: nc.scalar.activation(
                                out=XGT[:, cg * 4:(cg + 1) * 4, st * 128:(st + 1) * 128], in_=pt[:].rearrange("p (c t) -> p c t", c=4),
                                func=AF.Copy), [pk], ["XGT"])
                for pj in range(FC // MB):
                    wb = wr[wslot % 3]; wk = "mw%d" % (wslot % 3); wslot += 1
                    for g in range(2):
                        dma("pool", lambda q, pj=pj, g=g, wb=wb: q.dma_start(
                            out=wb[:, :, g * 128 * MB:(g + 1) * 128 * MB], in_=w_gu_v[:, :, g, pj * 128 * MB:(pj + 1) * 128 * MB]), writes=[wk + "g%d" % g])
                    for mi in range(MB):
                        m = pj * MB + mi
                        bgc = (e * 2 + 0) * FC + m
                        buc = (e * 2 + 1) * FC + m
                        for (n0, nn) in cfg.NS:
                            bg_, bu_ = (0, 1) if epi % 2 == 0 else (2, 3)
                            gl = GL[epi % 2]; sg = SG[epi % 2]; tu = TU[epi % 2]
                            glk, sgk, tuk = "GL%d" % (epi % 2), "SG%d" % (epi % 2), "TU%d" % (epi % 2)
                            epi += 1
                            for g, bb in ((0, bg_), (1, bu_)):
                                for k in range(KC):
                                    op("pe", lambda k=k, g=g, bb=bb, mi=mi, n0=n0, nn=nn, wb=wb: nc.tensor.matmul(
                                        bank(bb, nn), lhsT=wb[:, k, g * 128 * MB + mi * 128:g * 128 * MB + (mi + 1) * 128],
                                        rhs=XGT[:, k, n0:n0 + nn], start=(k == 0), stop=(k == KC - 1)),
                                       [wk + "g%d" % g, "XGT"], [bkey(bb)], signal=(k == KC - 1))
                            op("dve", lambda gl=gl, bg_=bg_, nn=nn, bgc=bgc: nc.vector.tensor_scalar(
                                out=gl[:, 0:nn], in0=bank(bg_, nn), scalar1=BG[:, bgc:bgc + 1], scalar2=7.0, op0=ALU.add, op1=ALU.min),
                               [bkey(bg_), "BG"], [glk])
                            op("act", lambda gl=gl, sg=sg, nn=nn: nc.scalar.activation(out=sg[:, 0:nn], in_=gl[:, 0:nn], func=AF.Sigmoid, scale=1.702),
                               [glk], [sgk])
                            op("dve", lambda tu=tu, bu_=bu_, nn=nn, buc=buc: nc.vector.tensor_scalar(
                                out=tu[:, 0:nn], in0=bank(bu_, nn), scalar1=BG1[:, buc:buc + 1], scalar2=8.0, op0=ALU.add, op1=ALU.min),
                               [bkey(bu_), "BG1"], [tuk])
                            op("dve", lambda tu=tu, gl=gl, nn=nn: nc.vector.scalar_tensor_tensor(
                                out=tu[:, 0:nn], in0=tu[:, 0:nn], scalar=-6.0, in1=gl[:, 0:nn], op0=ALU.max, op1=ALU.mult),
                               [tuk, glk], [tuk])
                            op("pool", lambda tu=tu, sg=sg, nn=nn, m=m, n0=n0: nc.gpsimd.tensor_tensor(
                                out=ACTT[:, m, n0:n0 + nn], in0=tu[:, 0:nn], in1=sg[:, 0:nn], op=ALU.mult), [tuk, sgk], ["ACTT"])
                for j in range(4):
                    wb = wr[wslot % 3]; wk = "mw%d" % (wslot % 3); wslot += 1
                    dma("pool", lambda q, j=j, wb=wb: q.dma_start(out=wb[:, 0:FC, :], in_=w_dn_v[:, :, j * 512:(j + 1) * 512]), writes=[wk + "g0", wk + "g1"])
                    for st in range(CT):
                        bi = 4 + (ysi % 2)
                        ys = YS[ysi % 4]; ysk = "YS%d" % (ysi % 4); ysi += 1
                        for m in range(FC):
                            op("pe", lambda m=m, st=st, bi=bi, wb=wb: nc.tensor.matmul(
                                bank(bi, 512), lhsT=ACTT[:, m, st * 128:(st + 1) * 128], rhs=wb[:, m, :], start=(m == 0), stop=False),
                               ["ACTT", wk + "g0", wk + "g1"], [bkey(bi)], signal=False)
                        op("pe", lambda bi=bi, j=j, e=e: nc.tensor.matmul(
                            bank(bi, 512), lhsT=ones_b[0:1, :], rhs=bdb[e % 2][0:1, j * 512:(j + 1) * 512], start=False, stop=True),
                           ["ones_b", "bdb%d" % (e % 2)], [bkey(bi)], signal=True)
                        if ysi % 2 == 0:
                            op("act", lambda ys=ys, bi=bi: nc.scalar.activation(out=ys[:], in_=bank(bi, 512), func=AF.Copy), [bkey(bi)], [ysk])
                        else:
                            op("dve", lambda ys=ys, bi=bi: nc.vector.tensor_copy(out=ys[:], in_=bank(bi, 512)), [bkey(bi)], [ysk])
                        r0 = e * C + st * 128
                        dma("sp", lambda q, r0=r0, j=j, ys=ys: q.dma_start(out=yy_d[r0:r0 + 128, j * 512:(j + 1) * 512], in_=ys[:]), [ysk], ["yy_d"])
            T.wait_all("sp", ["XG0", "XG1", "XGT", "ACTT", "mw0", "mw1", "mw2", "BG", "BG1", "bdf0", "bdf1", "bdb0", "bdb1",
                              "GL0", "GL1", "SG0", "SG1", "TU0", "TU1", "YS0", "YS1", "YS2", "YS3"])

        with contextlib.ExitStack() as f_:
            YG = [[sb(f_, "YG%d_%d" % (i, k), [128, D], F32) for k in range(4)] for i in range(2)]
            HH = [sb(f_, "HH%d" % i, [128, D], F32) for i in range(2)]
            OU = [sb(f_, "OU%d" % i, [128, D], F32) for i in range(2)]
            LB2 = sb(f_, "LB2", [128, 2, D], F32)
            st6 = sb(f_, "fst6", [128, 24], F32)
            mv = sb(f_, "fmv", [128, 2], F32)
            rstd = sb(f_, "frstd", [128, 1], F32)
            for j in range(2):
                dma("sp", lambda q, j=j: q.dma_start(out=LB2[:, j, :], in_=lnv[4 + j:5 + j, :].partition_broadcast(128)), writes=["LF%d" % j])
            for tg in range(NT):
                i = tg % 2
                hh = HH[i]; hhk = "HH%d" % i
                ou = OU[i]; ouk = "OU%d" % i
                dma("sp", lambda q, tg=tg, hh=hh: q.dma_start(out=hh[:], in_=h_d[tg * 128:(tg + 1) * 128, :]), ["h_d"], [hhk])
                for k in range(4):
                    dma("pool", lambda q, tg=tg, k=k, i=i: q.indirect_dma_start(
                        out=YG[i][k][:], out_offset=None, in_=yy_d[:, :],
                        in_offset=bass.IndirectOffsetOnAxis(ap=SL[:, tg, k:k + 1], axis=0)),
                        ["yy_d", "yy_dump", "SL"], ["YG%d_%d" % (i, k)])
                op("act", lambda hh=hh: nc.scalar.activation(out=hh[:], in_=hh[:], func=AF.Copy, scale=ALPHA), [hhk], [hhk])
                for k in range(4):
                    op("dve", lambda tg=tg, k=k, i=i, hh=hh: nc.vector.scalar_tensor_tensor(
                        out=hh[:], in0=YG[i][k][:], scalar=GT[:, tg, k:k + 1], in1=hh[:], op0=ALU.mult, op1=ALU.add),
                       ["YG%d_%d" % (i, k), "GT", hhk], [hhk])
                ln_stats(hh, hhk, st6, mv[:], rstd[:], "f")
                op("dve", lambda hh=hh: nc.vector.tensor_scalar(out=hh[:], in0=hh[:], scalar1=mv[:, 0:1], scalar2=rstd[:],
                                                                op0=ALU.subtract, op1=ALU.mult), [hhk, "fmv", "frstd"], [hhk])
                op("pool", lambda hh=hh: nc.gpsimd.tensor_tensor(out=hh[:], in0=hh[:], in1=LB2[:, 0, :], op=ALU.mult), [hhk, "LF0"], [hhk])
                op("dve", lambda hh=hh, ou=ou: nc.vector.tensor_tensor(out=ou[:], in0=hh[:], in1=LB2[:, 1, :], op=ALU.add), [hhk, "LF1"], [ouk])
                dma("sp", lambda q, tg=tg, ou=ou: q.dma_start(out=y_out[tg * 128:(tg + 1) * 128, :], in_=ou[:]), [ouk], ["y_out"])
            keys = ["y_out", "OU0", "OU1", "HH0", "HH1", "LB2"] + ["YG%d_%d" % (i, k) for i in range(2) for k in range(4)]
            T.wait_all("sp", keys)
            for q in ("sp", "pool", "act"):
                for name in T.dq[q][0]:
                    if T.cnt[name]:
                        nc.sync.wait_ge(T.sem[name], T.cnt[name])
    return nc


def _unit_table(seq_rows):
    units = []
    for si, R in enumerate(seq_rows):
        for b in range(R // 16):
            units.append((si, b))
    return units


def _geometry(R, b):
    mask = np.full((128, NMASK, 128), NEG, np.float32)
    c = np.arange(64)
    cs = np.clip(c - 8, 0, 48)
    colok = (c[None, :] >= cs[:, None]) & (c[None, :] < cs[:, None] + 16)
    for p in range(8):
        for i, o in enumerate(pair_offsets(p)):
            mi = mask_index(p, i)
            for qr in range(2):
                r = 16 * b + 2 * p + qr
                rs = min(max(r - 4, 0), R - 8)
                for kr in range(2):
                    ka = 16 * b + 2 * p + 2 * o + kr
                    if 0 <= ka < R and rs <= ka < rs + 8:
                        blk = np.where(colok, 0.0, NEG).astype(np.float32)
                        mask[qr * 64:(qr + 1) * 64, mi, kr * 64:(kr + 1) * 64] = blk
    L = R * 64
    t0 = 16 * b * 64
    pv = np.zeros(16, np.float32)
    for j in range(8):
        pv[j] = 1.0 if 0 <= t0 - 8 + j < L else 0.0
        pv[8 + j] = 1.0 if 0 <= t0 + 1024 + j < L else 0.0
    rc = np.zeros(64, np.float32)
    for g, w in enumerate((2, 4, 8, 16)):
        half = w // 2
        for j in range(8):
            for (off, tt) in ((0, t0 + j), (8, t0 + 1016 + j)):
                lo = min(max(tt - half, 0), L)
                hi = min(max(tt + half, 0), L)
                rc[g * 16 + off + j] = 1.0 / float(hi - lo)
    return mask, pv, rc


def _bias_table(rpb):
    H = rpb.shape[0]
    out = np.zeros((H, 128, 7, 128), np.float32)
    c = np.arange(64)
    dc = c[None, :] - c[:, None] + 15
    okc = (dc >= 0) & (dc <= 30)
    dcc = np.clip(dc, 0, 30)
    for o in range(-3, 4):
        for qr in range(2):
            for kr in range(2):
                dr = 2 * o + kr - qr + 7
                if 0 <= dr <= 14:
                    blk = np.where(okc[None], rpb[:, dr, :][:, dcc], 0.0)
                    out[:, qr * 64:(qr + 1) * 64, o + 3, kr * 64:(kr + 1) * 64] = blk
    return out.reshape(H * 128, 7 * 128)


def prepare_inputs(cfg, xs, p):
    E, DFF, FC, NU, C = cfg.E, cfg.DFF, cfg.FC, cfg.NU, cfg.C
    seq_rows = [x.shape[0] // GRID_W for x in xs]
    units = _unit_table(seq_rows)
    assert len(units) == NU * cfg.NCORES, (len(units), NU, cfg.NCORES)
    f32 = np.float32
    lnv = np.stack([p["ln_in_g"], p["ln_in_b"], p["ln1_g"], p["ln1_b"], p["ln2_g"], p["ln2_b"]]).astype(f32)
    lncol = np.concatenate([p["ln_in_g"].reshape(16, 128).T, p["ln_in_b"].reshape(16, 128).T], axis=1).astype(f32)
    pscol = np.ascontiguousarray(p["pool_scale"].reshape(8, 128).T).astype(f32)
    bgcol = np.ascontiguousarray(p["b_gate_up"].reshape(E * 2 * FC, 128).T).astype(f32)
    consts = np.zeros((128, 128 * 3 + 8 + E), f32)
    consts[:, 0:128] = np.eye(128, dtype=f32)
    consts[:, 128:256] = np.triu(np.ones((128, 128), f32), 1)
    consts[:, 256:384] = 1.0
    for j in range(4):
        consts[32 * j:32 * (j + 1), 384 + j] = 1.0
    consts[:, 392:392 + E] = (np.arange(E, dtype=f32) * C)[None, :]
    shared = {
        "bias_in": _bias_table(p["rpb"].astype(f32)), "lnv": lnv, "lncol": np.ascontiguousarray(lncol),
        "w_in": p["w_in"], "w_pool": p["w_pool"].reshape(4 * 256, 256), "pscol": pscol, "w_out": p["w_out"],
        "w_router": p["w_router"], "b_router": p["b_router"].reshape(1, E), "w_gu": p["w_gate_up"].reshape(E * D, 2 * DFF),
        "bgcol": bgcol, "w_dn": p["w_down"].reshape(E * DFF, D), "b_dn": p["b_down"].reshape(E, D), "consts": consts,
    }
    in_maps = []
    for c in range(cfg.NCORES):
        xin = np.zeros((NU * UT, D), f32)
        masks = np.zeros((NU * 128, NMASK * 128), f32)
        pvs = np.zeros((NU, 16), f32)
        rcs = np.zeros((NU, 64), f32)
        for ui in range(NU):
            si, b = units[c * NU + ui]
            R = seq_rows[si]
            t_lo = (16 * b - 4) * 64
            t_hi = t_lo + UT
            a, bnd = max(t_lo, 0), min(t_hi, R * 64)
            xin[ui * UT + (a - t_lo): ui * UT + (bnd - t_lo)] = xs[si][a:bnd]
            mk, pv, rc = _geometry(R, b)
            masks[ui * 128:(ui + 1) * 128] = mk.reshape(128, NMASK * 128)
            pvs[ui] = pv
            rcs[ui] = rc
        m = dict(shared)
        m.update({"x_in": xin, "mask_in": masks, "pv_in": pvs, "rc_in": rcs})
        in_maps.append(m)
    return in_maps, units


def assemble(cfg, results, units, seq_lens):
    outs = [np.zeros((L, D), np.float32) for L in seq_lens]
    for c in range(cfg.NCORES):
        y = results[c]["y_out"]
        for ui in range(cfg.NU):
            si, b = units[c * cfg.NU + ui]
            outs[si][b * 1024:(b + 1) * 1024] = y[ui * 1024:(ui + 1) * 1024]
    return outs


_NC_CACHE = {}


def run(cfg, xs, p):
    key = (cfg.E, cfg.DFF, cfg.NU, cfg.NCORES, cfg.C)
    if key not in _NC_CACHE:
        _NC_CACHE[key] = build(cfg)
    nc = _NC_CACHE[key]
    in_maps, units = prepare_inputs(cfg, xs, p)
    res = run_bass_kernel_spmd(nc, in_maps, core_ids=list(range(cfg.NCORES)))
    return assemble(cfg, res.results, units, [x.shape[0] for x in xs])


def kernel(x_prompt, x_sample, ln_in_g, ln_in_b, w_in, rpb, w_pool, pool_scale, w_out, ln1_g, ln1_b,
           w_router, b_router, w_gate_up, b_gate_up, w_down, b_down, ln2_g, ln2_b):
    cfg = Cfg()
    a = lambda v: np.asarray(v, dtype=np.float32)
    p = {"ln_in_g": a(ln_in_g), "ln_in_b": a(ln_in_b), "w_in": a(w_in)[0], "rpb": a(rpb)[0], "w_pool": a(w_pool)[0],
         "pool_scale": a(pool_scale)[0], "w_out": a(w_out)[0], "ln1_g": a(ln1_g)[0], "ln1_b": a(ln1_b)[0],
         "w_router": a(w_router)[0], "b_router": a(b_router)[0], "w_gate_up": a(w_gate_up)[0], "b_gate_up": a(b_gate_up)[0],
         "w_down": a(w_down)[0], "b_down": a(b_down)[0], "ln2_g": a(ln2_g)[0], "ln2_b": a(ln2_b)[0]}
    xp = a(x_prompt)
    xsm = a(x_sample)
    xs = [xp[i] for i in range(xp.shape[0])] + [xsm[i] for i in range(xsm.shape[0])]
    outs = run(cfg, xs, p)
    y_prompt = np.stack(outs[:xp.shape[0]]).astype(np.float32)
    y_sample = np.stack(outs[xp.shape[0]:]).astype(np.float32)
    return (y_prompt, y_sample)
```

```python
import contextlib
import numpy as np
import concourse.bass as bass
import concourse.mybir as mybir
from concourse.bass_utils import run_bass_kernel_spmd

F32 = mybir.dt.float32
BF16 = mybir.dt.bfloat16
I32 = mybir.dt.int32
AF = mybir.ActivationFunctionType
ALU = mybir.AluOpType
AX = mybir.AxisListType

D = 2048
KC = 16
NH = 32
GRID_W = 64
LN_EPS = 1e-5
ALPHA = float(2 ** 0.25)
NEG = -30000.0
VS = 34
UT = 1536
NMASK = 42
DBG = False


class Cfg:
    def __init__(self, E=32, DFF=2048, NU=5, NCORES=8, C=768):
        self.E, self.DFF, self.NU, self.NCORES, self.C = E, DFF, NU, NCORES, C
        self.FC = DFF // 128
        self.CT = C // 128
        self.NT = NU * 8
        self.NSLOT = E * C + 128
        self.DUMP = E * C
        ns, s = [], 0
        while s < C:
            n = min(512, C - s)
            if C % 448 == 0:
                n = min(448, C - s)
            ns.append((s, n)); s += n
        self.NS = ns


def pair_offsets(p):
    if p == 0:
        return [-2, -1, 0, 1, 2, 3]
    if p == 7:
        return [-3, -2, -1, 0, 1, 2]
    return [-2, -1, 0, 1, 2]


def mask_index(p, i):
    if p == 0:
        return i
    return 6 + (p - 1) * 5 + i


class Trk:
    def __init__(self, nc, es):
        self.nc = nc
        self.eng = {"pe": nc.tensor, "act": nc.scalar, "dve": nc.vector, "pool": nc.gpsimd, "sp": nc.sync}
        self.sem = {}
        self.cnt = {}
        for e in ("pe", "act", "dve", "pool"):
            self.sem[e] = es.enter_context(nc.semaphore("sem_" + e))
            self.cnt[e] = 0
        self.dq = {}
        for q, k in (("sp", 8), ("pool", 8), ("act", 4)):
            sems = []
            for i in range(k):
                name = "dq_%s_%d" % (q, i)
                self.sem[name] = es.enter_context(nc.semaphore(name))
                self.cnt[name] = 0
                sems.append(name)
            self.dq[q] = [sems, 0]
        self.state = {}
        self.seen = {e: {} for e in self.eng}

    def _st(self, k):
        s = self.state.get(k)
        if s is None:
            s = {"w": None, "r": {}}
            self.state[k] = s
        return s

    def _waits(self, engine, reads, writes, extra=()):
        need = {}

        def add(ev):
            if ev is None:
                return
            s, v = ev
            if engine == "pe" and s == "pe":
                return
            if need.get(s, 0) < v:
                need[s] = v

        for k in reads:
            add(self._st(k)["w"])
        for k in writes:
            st = self._st(k)
            add(st["w"])
            for s, v in st["r"].items():
                add((s, v))
        for ev in extra:
            add(ev)
        seen = self.seen[engine]
        for s, v in need.items():
            if seen.get(s, 0) >= v:
                continue
            self.eng[engine].wait_ge(self.sem[s], v)
            seen[s] = v

    def _record(self, ev, reads, writes):
        s, v = ev
        for k in reads:
            r = self._st(k)["r"]
            if r.get(s, 0) < v:
                r[s] = v
        for k in writes:
            self.state[k] = {"w": ev, "r": {}}

    def op(self, engine, fn, reads=(), writes=(), signal=True):
        self._waits(engine, reads, writes)
        ins = fn()
        if signal:
            self.cnt[engine] += 1
            ins.then_inc(self.sem[engine], 1)
            ev = (engine, self.cnt[engine])
        else:
            ev = (engine, self.cnt[engine] + 1)
        self._record(ev, reads, writes)
        return ins

    def dma(self, q, fn, reads=(), writes=()):
        sems, i = self.dq[q]
        name = sems[i % len(sems)]
        self.dq[q][1] = i + 1
        prev = (name, self.cnt[name])
        self._waits(q, reads, writes, extra=(prev,) if self.cnt[name] else ())
        ins = fn(self.eng[q])
        self.cnt[name] += 16
        ins.then_inc(self.sem[name], 16)
        ev = (name, self.cnt[name])
        self._record(ev, reads, writes)
        return ev

    def wait_all(self, engine, keys):
        self.barrier()

    def barrier(self):
        evs = [(s, v) for s, v in self.cnt.items() if v > 0]
        for e in ("pe", "act", "dve", "pool", "sp"):
            seen = self.seen[e]
            for s, v in evs:
                if s == e and e == "pe":
                    continue
                if seen.get(s, 0) >= v:
                    continue
                self.eng[e].wait_ge(self.sem[s], v)
                seen[s] = v
        self.state = {}


def build(cfg):
    nc = bass.Bass("TRN2", target_bir_lowering=False)
    E, DFF, FC, NU, C, CT, NT = cfg.E, cfg.DFF, cfg.FC, cfg.NU, cfg.C, cfg.CT, cfg.NT
    NSLOT, DUMP = cfg.NSLOT, cfg.DUMP

    def din(name, shape, dt=F32):
        return nc.dram_tensor(name, list(shape), dt, kind="ExternalInput").ap()

    def dscr(name, shape, dt):
        return nc.dram_tensor(name, list(shape), dt, kind="Internal").ap()

    x_in = din("x_in", [NU * UT, D])
    mask_in = din("mask_in", [NU * 128, NMASK * 128])
    bias_in = din("bias_in", [NH * 128, 7 * 128])
    pv_in = din("pv_in", [NU, 16])
    rc_in = din("rc_in", [NU, 64])
    lnv = din("lnv", [6, D])
    lncol = din("lncol", [128, 32])
    w_in = din("w_in", [D, 4096])
    w_pool = din("w_pool", [4 * 256, 256])
    pscol = din("pscol", [128, 8])
    w_out = din("w_out", [D, D])
    w_router = din("w_router", [D, E])
    b_router = din("b_router", [1, E])
    w_gu = din("w_gu", [E * D, 2 * DFF])
    bgcol = din("bgcol", [128, E * 2 * FC])
    w_dn = din("w_dn", [E * DFF, D])
    b_dn = din("b_dn", [E, D])
    consts = din("consts", [128, 128 * 3 + 8 + E])
    y_out = nc.dram_tensor("y_out", [NU * 1024, D], F32, kind="ExternalOutput").ap()

    qt_d = dscr("qt_d", [1024, 1024], BF16)
    kt_d = dscr("kt_d", [1024, UT], BF16)
    v_d = dscr("v_d", [UT, NH * VS], BF16)
    pm_d = dscr("pm_d", [1024, 1024], BF16)
    h_d = dscr("h_d", [NT * 128, D], F32)
    xg_d = dscr("xg_d", [NSLOT, D], BF16)
    yy_d = dscr("yy_d", [NSLOT, D], F32)

    es = contextlib.ExitStack()
    with es:
        T = Trk(nc, es)
        op, dma = T.op, T.dma

        uid = [0]

        def sb(stack, name, shape, dt):
            uid[0] += 1
            return stack.enter_context(nc.sbuf_tensor("%s_%d" % (name, uid[0]), list(shape), dt))

        PS = [es.enter_context(nc.psum_tensor("ps%d" % i, [128, 1024], F32)) for i in range(2)]
        PO = [es.enter_context(nc.psum_tensor("po%d" % i, [128, 512], F32)) for i in range(2)]
        PT = [es.enter_context(nc.psum_tensor("pt%d" % i, [128, 512], BF16)) for i in range(2)]
        banks = [(PS[0], 0), (PS[0], 512), (PS[1], 0), (PS[1], 512), (PO[0], 0), (PO[1], 0)]

        def bank(i, n=512):
            t, o = banks[i]
            return t[:, o:o + n]

        def bkey(i):
            return "bank%d" % i

        pskeys = [("bank0", "bank1"), ("bank2", "bank3")]
        ptkey = ["ptb0", "ptb1"]

        cst = sb(es, "cst", [128, 128 * 3 + 8 + E], F32)
        ident_b = sb(es, "ident_b", [128, 128], BF16)
        ustr_b = sb(es, "ustr_b", [128, 128], BF16)
        ones_b = sb(es, "ones_b", [128, 128], BF16)
        SL = sb(es, "SL", [128, NT, 4], I32)
        GT = sb(es, "GT", [128, NT, 4], F32)
        cum = sb(es, "cum", [128, E], F32)
        zer = sb(es, "zer", [128, D], F32)
        dma("sp", lambda q: q.dma_start(out=cst[:], in_=consts), writes=["cst"])
        ident_f = cst[:, 0:128]
        hm = cst[:, 384:388]
        ebase = cst[:, 392:392 + E]
        op("dve", lambda: nc.vector.tensor_copy(out=ident_b[:], in_=cst[:, 0:128]), ["cst"], ["ident_b"])
        op("dve", lambda: nc.vector.tensor_copy(out=ustr_b[:], in_=cst[:, 128:256]), ["cst"], ["ustr_b"])
        op("dve", lambda: nc.vector.tensor_copy(out=ones_b[:], in_=cst[:, 256:384]), ["cst"], ["ones_b"])
        op("dve", lambda: nc.vector.memset(cum[:], 0.0), [], ["cum"])
        op("pool", lambda: nc.gpsimd.memset(zer[:], 0.0), [], ["zer"])
        dma("sp", lambda q: q.dma_start(out=yy_d[DUMP:DUMP + 128, :], in_=zer[:]), ["zer"], ["yy_dump"])
        zb = zer[:].bitcast(BF16)
        for r0 in range(0, NSLOT, 256):
            nr = min(256, NSLOT - r0)
            dma("sp", lambda q, r0=r0, nr=nr: q.dma_start(
                out=xg_d[r0:r0 + nr, :].rearrange("(p two) d -> p (two d)", two=nr // 128), in_=zb[:, 0:D * (nr // 128)]),
                ["zer"], ["xg_zero"])

        def ln_stats(xt_ap, xkey, st6, mv, rstd, tag):
            for c in range(4):
                op("dve", lambda c=c: nc.vector.bn_stats(out=st6[:, c * 6:(c + 1) * 6], in_=xt_ap[:, c * 512:(c + 1) * 512]),
                   [xkey], [tag + "st6"])
            op("dve", lambda: nc.vector.bn_aggr(out=mv, in_=st6[:, 0:24]), [tag + "st6"], [tag + "mv"])
            op("dve", lambda: nc.vector.tensor_scalar(out=rstd, in0=mv[:, 1:2], scalar1=LN_EPS, scalar2=None, op0=ALU.add),
               [tag + "mv"], [tag + "rstd"])
            op("act", lambda: nc.scalar.activation(out=rstd, in_=rstd, func=AF.Sqrt), [tag + "rstd"], [tag + "rstd"])
            op("dve", lambda: nc.vector.reciprocal(out=rstd, in_=rstd), [tag + "rstd"], [tag + "rstd"])

        for u in range(NU):
            xu = x_in[u * UT:(u + 1) * UT, :]
            with contextlib.ExitStack() as us:
                stats = sb(us, "stats", [128, 8, 2], F32)
                with contextlib.ExitStack() as a:
                    XT = sb(a, "XT", [128, KC, UT], BF16)
                    xt = [sb(a, "xt%d" % i, [128, D], F32) for i in range(2)]
                    xh = sb(a, "xh", [128, D], BF16)
                    st6 = sb(a, "st6", [128, 24], F32)
                    mv = sb(a, "mv", [128, 2], F32)
                    rstd = sb(a, "rstd", [128, 1], F32)
                    lnc = sb(a, "lnc", [128, 32], F32)
                    wr = [sb(a, "wr%d" % i, [128, KC, 256], BF16) for i in range(3)]
                    QTs = sb(a, "QTs", [128, 1024], BF16)
                    KTs = sb(a, "KTs", [128, UT], BF16)
                    Vb = sb(a, "Vb", [128, 12, NH, VS], BF16)
                    UF = sb(a, "UF", [128, UT], F32)
                    PA = sb(a, "PA", [128, UT], F32)
                    PB = sb(a, "PB", [128, UT], F32)
                    PTg = sb(a, "PTg", [128, 2, 1024], BF16)
                    PMs = sb(a, "PMs", [128, 1024], BF16)
                    wp = sb(a, "wp", [128, 4, 2, 256], BF16)
                    psc = sb(a, "psc", [128, 8], F32)
                    pvb = sb(a, "pvb", [128, 16], F32)
                    rcb = sb(a, "rcb", [128, 64], F32)
                    tmp8 = sb(a, "tmp8", [128, 8], F32)

                    dma("sp", lambda q: q.dma_start(out=lnc[:], in_=lncol), writes=["lnc"])
                    dma("sp", lambda q: q.dma_start(out=psc[:], in_=pscol), writes=["psc"])
                    dma("sp", lambda q: q.dma_start(out=pvb[:], in_=pv_in[u:u + 1, :].partition_broadcast(128)), writes=["pvb"])
                    dma("sp", lambda q: q.dma_start(out=rcb[:], in_=rc_in[u:u + 1, :].partition_broadcast(128)), writes=["rcb"])
                    dma("pool", lambda q: q.dma_start(out=wp[:], in_=w_pool.rearrange("(g k p) n -> p g k n", g=4, k=2, p=128)),
                        writes=["wp"])
                    op("pool", lambda: nc.gpsimd.memset(Vb[:], 1.0), [], ["Vb"])

                    for t in range(12):
                        xb = xt[t % 2]
                        xk = "xt%d" % (t % 2)
                        dma("sp", lambda q, t=t, xb=xb: q.dma_start(out=xb[:], in_=xu[t * 128:(t + 1) * 128, :]), writes=[xk])
                        ln_stats(xb, xk, st6, mv[:], rstd[:], "a")
                        if 2 <= t < 10:
                            op("dve", lambda t=t: nc.vector.tensor_copy(out=stats[:, t - 2, 0:1], in_=mv[:, 0:1]), ["amv"], ["stats"])
                            op("dve", lambda t=t: nc.vector.tensor_copy(out=stats[:, t - 2, 1:2], in_=rstd[:]), ["arstd"], ["stats"])
                        op("dve", lambda xb=xb: nc.vector.tensor_scalar(out=xh[:], in0=xb[:], scalar1=mv[:, 0:1], scalar2=rstd[:],
                                                                       op0=ALU.subtract, op1=ALU.mult),
                           [xk, "amv", "arstd"], ["xh"])
                        for cg in range(4):
                            pt = PT[cg % 2]
                            pk = ptkey[cg % 2]
                            for ci in range(4):
                                c = cg * 4 + ci
                                op("pe", lambda c=c, ci=ci, pt=pt: nc.tensor.transpose(out=pt[:, ci * 128:(ci + 1) * 128],
                                                                                      in_=xh[:, c * 128:(c + 1) * 128], identity=ident_b[:]),
                                   ["xh", "ident_b"], [pk], signal=(ci == 3))
                            for ci in range(4):
                                c = cg * 4 + ci
                                op("act", lambda c=c, ci=ci, pt=pt, t=t: nc.scalar.activation(
                                    out=XT[:, c, t * 128:(t + 1) * 128], in_=pt[:, ci * 128:(ci + 1) * 128], func=AF.Identity,
                                    bias=lnc[:, 16 + c:17 + c], scale=lnc[:, c:c + 1]), [pk, "lnc"], ["XT"])

                    w_in_v = w_in.rearrange("(k p) n -> p k n", p=128)
                    for pc in range(16):
                        wb = wr[pc % 3]
                        wk = "wr%d" % (pc % 3)
                        dma("pool", lambda q, pc=pc, wb=wb: q.dma_start(out=wb[:], in_=w_in_v[:, :, pc * 256:(pc + 1) * 256]), writes=[wk])
                        kind = pc // 4
                        if kind in (0, 1):
                            for mi in range(2):
                                mc = (pc % 4) * 2 + mi
                                ntl = [(256, 512), (768, 512)] if kind == 0 else [(0, 512), (512, 512), (1024, 512)]
                                dst = QTs if kind == 0 else KTs
                                dk = "QTs" if kind == 0 else "KTs"
                                for j, (n0, nn) in enumerate(ntl):
                                    bi = j % 2
                                    for k in range(KC):
                                        op("pe", lambda k=k, mi=mi, n0=n0, nn=nn, bi=bi, wb=wb: nc.tensor.matmul(
                                            bank(bi, nn), lhsT=wb[:, k, mi * 128:(mi + 1) * 128], rhs=XT[:, k, n0:n0 + nn],
                                            start=(k == 0), stop=(k == KC - 1)), [wk, "XT"], [bkey(bi)], signal=(k == KC - 1))
                                    d0 = n0 - 256 if kind == 0 else n0
                                    op("act", lambda d0=d0, nn=nn, bi=bi, dst=dst, kind=kind: nc.scalar.activation(
                                        out=dst[:, d0:d0 + nn], in_=bank(bi, nn), func=AF.Copy,
                                        scale=(32.0 ** -0.5 if kind == 0 else 1.0)), [bkey(bi)], [dk])
                                dd = qt_d if kind == 0 else kt_d
                                dma("sp", lambda q, mc=mc, dst=dst, dd=dd: q.dma_start(out=dd[mc * 128:(mc + 1) * 128, :], in_=dst[:]),
                                    [dk], ["qk_d"])
                        elif kind == 2:
                            h0 = (pc % 4) * 8
                            for t in range(12):
                                bi = t % 2
                                for k in range(KC):
                                    op("pe", lambda k=k, t=t, bi=bi, wb=wb: nc.tensor.matmul(
                                        bank(bi, 256), lhsT=XT[:, k, t * 128:(t + 1) * 128], rhs=wb[:, k, :],
                                        start=(k == 0), stop=(k == KC - 1)), [wk, "XT"], [bkey(bi)], signal=(k == KC - 1))
                                op("dve", lambda t=t, bi=bi, h0=h0: nc.vector.tensor_copy(
                                    out=Vb[:, t, h0:h0 + 8, 0:32], in_=bank(bi, 256).rearrange("p (h d) -> p h d", h=8)),
                                   [bkey(bi)], ["Vb"])
                            if pc % 4 == 3:
                                dma("sp", lambda q: q.dma_start(out=v_d.rearrange("(t p) f -> p t f", p=128),
                                                                in_=Vb[:].rearrange("p t h s -> p t (h s)")), ["Vb"], ["v_d"])
                        else:
                            g = pc % 4
                            for mi in range(2):
                                for j, (n0, nn) in enumerate([(128, 512), (640, 512), (1152, 256)]):
                                    bi = j % 2
                                    for k in range(KC):
                                        op("pe", lambda k=k, mi=mi, n0=n0, nn=nn, bi=bi, wb=wb: nc.tensor.matmul(
                                            bank(bi, nn), lhsT=wb[:, k, mi * 128:(mi + 1) * 128], rhs=XT[:, k, n0:n0 + nn],
                                            start=(k == 0), stop=(k == KC - 1)), [wk, "XT"], [bkey(bi)], signal=(k == KC - 1))
                                    op("act", lambda n0=n0, nn=nn, bi=bi: nc.scalar.activation(
                                        out=UF[:, n0:n0 + nn], in_=bank(bi, nn), func=AF.Copy), [bkey(bi)], ["UF"])
                                op("dve", lambda: nc.vector.tensor_tensor(out=UF[:, 248:256], in0=UF[:, 248:256], in1=pvb[:, 0:8], op=ALU.mult),
                                   ["UF", "pvb"], ["UF"])
                                op("dve", lambda: nc.vector.tensor_tensor(out=UF[:, 1280:1288], in0=UF[:, 1280:1288], in1=pvb[:, 8:16], op=ALU.mult),
                                   ["UF", "pvb"], ["UF"])
                                lo, hi = 192, 1344
                                op("dve", lambda: nc.vector.tensor_tensor(out=PA[:, lo:hi], in0=UF[:, lo - 1:hi - 1], in1=UF[:, lo:hi], op=ALU.add),
                                   ["UF"], ["PA"])
                                cur, oth, ck, ok_ = PA, PB, "PA", "PB"
                                sh = 1
                                for lvl in range(g):
                                    lo += sh; hi -= sh
                                    op("dve", lambda cur=cur, oth=oth, lo=lo, hi=hi, sh=sh: nc.vector.tensor_tensor(
                                        out=oth[:, lo:hi], in0=cur[:, lo - sh:hi - sh], in1=cur[:, lo + sh:hi + sh], op=ALU.add), [ck], [ok_])
                                    cur, oth, ck, ok_ = oth, cur, ok_, ck
                                    sh *= 2
                                wsz = 2 << g
                                op("dve", lambda cur=cur, mi=mi, wsz=wsz: nc.vector.scalar_tensor_tensor(
                                    out=PTg[:, mi, :], in0=cur[:, 256:1280], scalar=1.0 / wsz, in1=UF[:, 256:1280],
                                    op0=ALU.mult, op1=ALU.subtract), [ck, "UF"], ["PTg"])
                                for (c0, r0) in ((256, 0), (1272, 8)):
                                    op("dve", lambda cur=cur, c0=c0, r0=r0, g=g: nc.vector.tensor_tensor(
                                        out=tmp8[:], in0=cur[:, c0:c0 + 8], in1=rcb[:, g * 16 + r0:g * 16 + r0 + 8], op=ALU.mult),
                                       [ck, "rcb"], ["tmp8"])
                                    op("dve", lambda c0=c0, mi=mi: nc.vector.tensor_tensor(
                                        out=PTg[:, mi, c0 - 256:c0 - 248], in0=tmp8[:], in1=UF[:, c0:c0 + 8], op=ALU.subtract),
                                       ["tmp8", "UF"], ["PTg"])
                            for half in range(2):
                                oc = 2 * g + half
                                for nt in range(2):
                                    bi = nt
                                    for kc in range(2):
                                        op("pe", lambda kc=kc, half=half, nt=nt, bi=bi, g=g: nc.tensor.matmul(
                                            bank(bi, 512), lhsT=wp[:, g, kc, half * 128:(half + 1) * 128], rhs=PTg[:, kc, nt * 512:(nt + 1) * 512],
                                            start=(kc == 0), stop=(kc == 1)), ["wp", "PTg"], [bkey(bi)], signal=(kc == 1))
                                    op("act", lambda nt=nt, bi=bi, oc=oc: nc.scalar.activation(
                                        out=PMs[:, nt * 512:(nt + 1) * 512], in_=bank(bi, 512), func=AF.Identity, scale=psc[:, oc:oc + 1]),
                                       [bkey(bi), "psc"], ["PMs"])
                                dma("sp", lambda q, oc=oc: q.dma_start(out=pm_d[oc * 128:(oc + 1) * 128, :], in_=PMs[:]), ["PMs"], ["pm_d"])
                    T.wait_all("sp", ["XT", "wr0", "wr1", "wr2", "Vb", "QTs", "KTs", "PMs", "UF", "PA", "PB", "PTg", "xt0", "xt1", "xh"])

                CATT = sb(us, "CATT", [128, KC, 1024], BF16)
                with contextlib.ExitStack() as b:
                    QT = sb(b, "QT", [128, 8, 1024], BF16)
                    KT = sb(b, "KT", [128, 8, UT], BF16)
                    V = sb(b, "V", [128, 12, NH, VS], BF16)
                    MK = sb(b, "MK", [128, NMASK, 128], BF16)
                    BS = [sb(b, "BS%d" % i, [128, 7, 128], BF16) for i in range(2)]
                    QH = [sb(b, "QH%d" % i, [128, 1024], BF16) for i in range(2)]
                    PE_ = [sb(b, "PE%d" % i, [128, 768], BF16) for i in range(3)]
                    ATM = sb(b, "ATM", [128, 8, 1024], BF16)
                    rden = sb(b, "rden", [128, 8], F32)
                    CB = [sb(b, "CB%d" % i, [128, NMASK, 128], BF16) for i in range(2)]
                    dma("sp", lambda q: q.dma_start(out=QT[:], in_=qt_d.rearrange("(c p) t -> p c t", p=128)), ["qk_d"], ["QT"])
                    dma("sp", lambda q: q.dma_start(out=KT[:], in_=kt_d.rearrange("(c p) t -> p c t", p=128)), ["qk_d"], ["KT"])
                    dma("sp", lambda q: q.dma_start(out=V[:].rearrange("p t h s -> p t (h s)"),
                                                    in_=v_d.rearrange("(t p) f -> p t f", p=128)), ["v_d"], ["V"])
                    dma("pool", lambda q: q.dma_start(out=MK[:].rearrange("p m k -> p (m k)"), in_=mask_in[u * 128:(u + 1) * 128, :]),
                        writes=["MK"])
                    dma("sp", lambda q: q.dma_start(out=CATT[:, 8:16, :], in_=pm_d.rearrange("(c p) t -> p c t", p=128)), ["pm_d"], ["CATTp"])
                    ei = 0
                    for h in range(NH):
                        bs = BS[h % 2]; bsk = "BS%d" % (h % 2)
                        qh = QH[h % 2]; qhk = "QH%d" % (h % 2)
                        dma("pool", lambda q, h=h, bs=bs: q.dma_start(out=bs[:].rearrange("p o k -> p (o k)"),
                                                                      in_=bias_in[h * 128:(h + 1) * 128, :]), writes=[bsk])
                        op("dve", lambda h=h, qh=qh: nc.vector.tensor_scalar(out=qh[:], in0=QT[:, h // 4, :], scalar1=hm[:, h % 4:h % 4 + 1],
                                                                            scalar2=None, op0=ALU.mult), ["QT", "cst"], [qhk])
                        po = PO[h % 2]; pok = "po%d" % (h % 2)
                        cb = CB[h % 2]; cbk = "CB%d" % (h % 2)
                        for p in range(8):
                            offs = pair_offsets(p)
                            n_ = len(offs); mi0 = mask_index(p, 0); o0 = offs[0] + 3
                            op("pool", lambda cb=cb, bs=bs, n_=n_, mi0=mi0, o0=o0: nc.gpsimd.tensor_tensor(
                                out=cb[:, mi0:mi0 + n_, :], in0=MK[:, mi0:mi0 + n_, :], in1=bs[:, o0:o0 + n_, :], op=ALU.add),
                               ["MK", bsk], [cbk])

                        def pv_stage(p, pe_, pek, offs):
                            for i, o in enumerate(offs):
                                kt = 2 + p + o
                                op("pe", lambda i=i, kt=kt: nc.tensor.matmul(
                                    po[:, p * 64:p * 64 + 33], lhsT=pe_[:, i * 128:(i + 1) * 128], rhs=V[:, kt, h, 0:33],
                                    start=(i == 0), stop=(i == len(offs) - 1)), [pek, "V"], [pok], signal=(i == len(offs) - 1))

                        pending = None
                        for p in range(8):
                            offs = pair_offsets(p)
                            ps = PS[(h * 8 + p) % 2]; psk = "psS%d" % ((h * 8 + p) % 2)
                            for i, o in enumerate(offs):
                                kt0 = (2 + p + o) * 128
                                sl = ps[:, i * 128:(i + 1) * 128]
                                op("pe", lambda sl=sl, kt0=kt0: nc.tensor.matmul(
                                    sl, lhsT=KT[:, h // 4, kt0:kt0 + 128], rhs=qh[:, p * 128:(p + 1) * 128], start=True, stop=False),
                                   ["KT", qhk], [psk], signal=False)
                                mi_ = mask_index(p, i)
                                op("pe", lambda sl=sl, mi_=mi_: nc.tensor.matmul(
                                    sl, lhsT=cb[:, mi_, :], rhs=ident_b[:], start=False, stop=True), [cbk, "ident_b"], [psk],
                                   signal=(i == len(offs) - 1))
                            pe_ = PE_[ei % 3]; pek = "PE%d" % (ei % 3); ei += 1
                            n = len(offs) * 128
                            op("act", lambda: nc.scalar.activation(out=pe_[:, 0:512], in_=ps[:, 0:512], func=AF.Exp), [psk], [pek])
                            op("act", lambda: nc.scalar.activation(out=pe_[:, 512:n], in_=ps[:, 512:n], func=AF.Exp), [psk], [pek])
                            if pending is not None:
                                pv_stage(*pending)
                            pending = (p, pe_, pek, offs)
                        pv_stage(*pending)
                        pov = po[:].rearrange("p (a s) -> p a s", s=64)
                        op("dve", lambda pov=pov: nc.vector.reciprocal(out=rden[:].unsqueeze(2), in_=pov[:, :, 32:33]), [pok], ["rden"])
                        op("dve", lambda pov=pov, h=h: nc.vector.tensor_tensor(
                            out=ATM[:, :, h * 32:(h + 1) * 32], in0=pov[:, :, 0:32], in1=rden[:].unsqueeze(2).to_broadcast([128, 8, 32]),
                            op=ALU.mult), [pok, "rden"], ["ATM"])
                    for p in range(8):
                        for cg in range(2):
                            pt = PT[cg % 2]; pk = ptkey[cg % 2]
                            for ci in range(4):
                                c = cg * 4 + ci
                                op("pe", lambda c=c, ci=ci, pt=pt, p=p: nc.tensor.transpose(
                                    out=pt[:, ci * 128:(ci + 1) * 128], in_=ATM[:, p, c * 128:(c + 1) * 128], identity=ident_b[:]),
                                   ["ATM", "ident_b"], [pk], signal=(ci == 3))
                            op("dve", lambda cg=cg, pt=pt, p=p: nc.vector.tensor_copy(
                                out=CATT[:, cg * 4:(cg + 1) * 4, p * 128:(p + 1) * 128], in_=pt[:].rearrange("p (c t) -> p c t", c=4)),
                               [pk], ["CATTa"])
                    T.wait_all("sp", ["QT", "KT", "V", "MK", "BS0", "BS1", "QH0", "QH1", "PE0", "PE1", "PE2", "ATM", "rden"])

                with contextlib.ExitStack() as c_:
                    WO = sb(c_, "WO", [128, KC, D], BF16)
                    LB = sb(c_, "LB", [128, 4, D], F32)
                    WR = sb(c_, "WR", [128, KC, E], F32)
                    BR = sb(c_, "BR", [128, E], F32)
                    xt = [sb(c_, "cxt%d" % i, [128, D], F32) for i in range(1)]
                    HP = sb(c_, "HP", [128, D], F32)
                    H = [sb(c_, "H%d" % i, [128, D], F32) for i in range(2)]
                    HB = [sb(c_, "HB%d" % i, [128, D], BF16) for i in range(1)]
                    HT = sb(c_, "HT", [128, KC, 128], F32)
                    st6 = sb(c_, "cst6", [128, 24], F32)
                    mv = sb(c_, "cmv", [128, 2], F32)
                    rstd = sb(c_, "crstd", [128, 1], F32)
                    L = sb(c_, "L", [128, E], F32)
                    LW = sb(c_, "LW", [128, E], F32)
                    OH = sb(c_, "OH", [128, 4, E], F32)
                    SELf = sb(c_, "SELf", [128, E], F32)
                    SELb = sb(c_, "SELb", [128, E], BF16)
                    CUMb = sb(c_, "CUMb", [128, E], BF16)
                    MX = sb(c_, "MX", [128, 4], F32)
                    EX = sb(c_, "EX", [128, 4], F32)
                    nm0 = sb(c_, "nm0", [128, 1], F32)
                    ssum = sb(c_, "ssum", [128, 1], F32)
                    SLOT = sb(c_, "SLOT", [128, E], F32)
                    OKf = sb(c_, "OKf", [128, E], F32)
                    TMP = sb(c_, "TMP", [128, E], F32)
                    slf = sb(c_, "slf", [128, 4], F32)
                    w_out_v = w_out.rearrange("(k p) n -> p k n", p=128)
                    for j in range(4):
                        dma("pool", lambda q, j=j: q.dma_start(out=WO[:, :, j * 512:(j + 1) * 512], in_=w_out_v[:, :, j * 512:(j + 1) * 512]),
                            writes=["WO%d" % j])
                    for j in range(4):
                        dma("sp", lambda q, j=j: q.dma_start(out=LB[:, j, :], in_=lnv[j:j + 1, :].partition_broadcast(128)), writes=["LB%d" % j if j != 2 else "LB2_"])
                    dma("sp", lambda q: q.dma_start(out=WR[:], in_=w_router.rearrange("(k p) e -> p k e", p=128)), writes=["WR"])
                    dma("sp", lambda q: q.dma_start(out=BR[:], in_=b_router[0:1, :].partition_broadcast(128)), writes=["BR"])
                    for tt in range(8):
                        tg = u * 8 + tt
                        xb = xt[0]; xk = "cxt0"
                        Hc = H[tt % 2]; hk = "H%d" % (tt % 2)
                        HBc = HB[0]; hbk = "HB0"
                        dma("sp", lambda q, tt=tt, xb=xb: q.dma_start(out=xb[:], in_=xu[256 + tt * 128:256 + (tt + 1) * 128, :]), writes=[xk])
                        for j in range(4):
                            for c in range(KC):
                                op("pe", lambda j=j, c=c, tt=tt: nc.tensor.matmul(
                                    bank(j, 512), lhsT=CATT[:, c, tt * 128:(tt + 1) * 128], rhs=WO[:, c, j * 512:(j + 1) * 512],
                                    start=(c == 0), stop=(c == KC - 1)), ["CATTa", "CATTp", "WO%d" % j], [bkey(j)], signal=(c == KC - 1))
                        op("dve", lambda xb=xb, tt=tt: nc.vector.tensor_scalar(out=xb[:], in0=xb[:], scalar1=stats[:, tt, 0:1], scalar2=stats[:, tt, 1:2],
                                                                              op0=ALU.subtract, op1=ALU.mult), [xk, "stats"], [xk])
                        op("pool", lambda xb=xb: nc.gpsimd.tensor_tensor(out=xb[:], in0=xb[:], in1=LB[:, 0, :], op=ALU.mult), [xk, "LB0"], [xk])
                        op("pool", lambda xb=xb: nc.gpsimd.tensor_tensor(out=xb[:], in0=xb[:], in1=LB[:, 1, :], op=ALU.add), [xk, "LB1"], [xk])
                        for j in range(4):
                            op("dve", lambda j=j, xb=xb: nc.vector.scalar_tensor_tensor(
                                out=HP[:, j * 512:(j + 1) * 512], in0=xb[:, j * 512:(j + 1) * 512], scalar=ALPHA, in1=bank(j, 512),
                                op0=ALU.mult, op1=ALU.add), [xk, bkey(j)], ["HP"])
                        ln_stats(HP, "HP", st6, mv[:], rstd[:], "c")
                        op("dve", lambda: nc.vector.tensor_scalar(out=HP[:], in0=HP[:], scalar1=mv[:, 0:1], scalar2=rstd[:],
                                                                  op0=ALU.subtract, op1=ALU.mult), ["HP", "cmv", "crstd"], ["HP"])
                        op("pool", lambda: nc.gpsimd.tensor_tensor(out=HP[:], in0=HP[:], in1=LB[:, 2, :], op=ALU.mult), ["HP", "LB2_"], ["HP"])
                        op("dve", lambda Hc=Hc: nc.vector.tensor_tensor(out=Hc[:], in0=HP[:], in1=LB[:, 3, :], op=ALU.add), ["HP", "LB3"], [hk])
                        dma("sp", lambda q, tg=tg, Hc=Hc: q.dma_start(out=h_d[tg * 128:(tg + 1) * 128, :], in_=Hc[:]), [hk], ["h_d"])
                        op("act", lambda Hc=Hc, HBc=HBc: nc.scalar.activation(out=HBc[:], in_=Hc[:], func=AF.Copy), [hk], [hbk])
                        for cg in range(4):
                            bi = 4 + (cg % 2)
                            for ci in range(4):
                                c = cg * 4 + ci
                                op("pe", lambda c=c, ci=ci, bi=bi, Hc=Hc: nc.tensor.transpose(
                                    out=bank(bi, 512)[:, ci * 128:(ci + 1) * 128], in_=Hc[:, c * 128:(c + 1) * 128], identity=ident_f),
                                   [hk, "cst"], [bkey(bi)], signal=(ci == 3))
                            op("act", lambda cg=cg, bi=bi: nc.scalar.activation(
                                out=HT[:, cg * 4:(cg + 1) * 4, :], in_=bank(bi, 512).rearrange("p (c t) -> p c t", c=4), func=AF.Copy),
                               [bkey(bi)], ["HT"])
                        for c in range(KC):
                            op("pe", lambda c=c: nc.tensor.matmul(bank(0, E), lhsT=HT[:, c, :], rhs=WR[:, c, :], start=(c == 0), stop=(c == KC - 1)),
                               ["HT", "WR"], [bkey(0)], signal=(c == KC - 1))
                        op("dve", lambda: nc.vector.tensor_tensor(out=L[:], in0=bank(0, E), in1=BR[:], op=ALU.add), [bkey(0), "BR"], ["L"])
                        op("dve", lambda: nc.vector.tensor_copy(out=LW[:], in_=L[:]), ["L"], ["LW"])
                        for k in range(4):
                            op("dve", lambda k=k: nc.vector.reduce_max(out=MX[:, k:k + 1], in_=LW[:], axis=AX.X), ["LW"], ["MX"])
                            op("dve", lambda k=k: nc.vector.tensor_scalar(out=OH[:, k, :], in0=LW[:], scalar1=MX[:, k:k + 1], scalar2=None,
                                                                         op0=ALU.is_equal), ["LW", "MX"], ["OH"])
                            op("dve", lambda k=k: nc.vector.scalar_tensor_tensor(out=LW[:], in0=OH[:, k, :], scalar=-1e30, in1=LW[:],
                                                                                op0=ALU.mult, op1=ALU.add), ["OH", "LW"], ["LW"])
                        op("dve", lambda: nc.vector.tensor_tensor(out=SELf[:], in0=OH[:, 0, :], in1=OH[:, 1, :], op=ALU.add), ["OH"], ["SELf"])
                        op("dve", lambda: nc.vector.tensor_tensor(out=SELf[:], in0=SELf[:], in1=OH[:, 2, :], op=ALU.add), ["OH", "SELf"], ["SELf"])
                        op("dve", lambda: nc.vector.tensor_tensor(out=SELf[:], in0=SELf[:], in1=OH[:, 3, :], op=ALU.add), ["OH", "SELf"], ["SELf"])
                        op("dve", lambda: nc.vector.tensor_copy(out=SELb[:], in_=SELf[:]), ["SELf"], ["SELb"])
                        op("dve", lambda: nc.vector.tensor_copy(out=CUMb[:], in_=cum[:]), ["cum"], ["CUMb"])
                        op("dve", lambda: nc.vector.tensor_scalar(out=nm0[:], in0=MX[:, 0:1], scalar1=-1.0, scalar2=None, op0=ALU.mult), ["MX"], ["nm0"])
                        op("act", lambda: nc.scalar.activation(out=EX[:], in_=MX[:], func=AF.Exp, bias=nm0[:], scale=1.0), ["MX", "nm0"], ["EX"])
                        op("dve", lambda: nc.vector.reduce_sum(out=ssum[:], in_=EX[:], axis=AX.X), ["EX"], ["ssum"])
                        op("dve", lambda: nc.vector.reciprocal(out=ssum[:], in_=ssum[:]), ["ssum"], ["ssum"])
                        op("dve", lambda tg=tg: nc.vector.tensor_scalar(out=GT[:, tg, :], in0=EX[:], scalar1=ssum[:], scalar2=None, op0=ALU.mult),
                           ["EX", "ssum"], ["GT"])
                        op("pe", lambda: nc.tensor.matmul(bank(1, E), lhsT=ustr_b[:], rhs=SELb[:], start=True, stop=False),
                           ["ustr_b", "SELb"], [bkey(1)], signal=False)
                        op("pe", lambda: nc.tensor.matmul(bank(1, E), lhsT=ones_b[:], rhs=CUMb[:], start=False, stop=True),
                           ["ones_b", "CUMb"], [bkey(1)], signal=True)
                        op("dve", lambda: nc.vector.tensor_scalar(out=OKf[:], in0=bank(1, E), scalar1=float(C), scalar2=None, op0=ALU.is_lt),
                           [bkey(1)], ["OKf"])
                        op("dve", lambda: nc.vector.tensor_tensor(out=SLOT[:], in0=bank(1, E), in1=ebase, op=ALU.add), [bkey(1), "cst"], ["SLOT"])
                        op("dve", lambda: nc.vector.tensor_scalar(out=SLOT[:], in0=SLOT[:], scalar1=-float(DUMP), scalar2=None, op0=ALU.add),
                           ["SLOT"], ["SLOT"])
                        op("dve", lambda: nc.vector.tensor_tensor(out=SLOT[:], in0=SLOT[:], in1=OKf[:], op=ALU.mult), ["SLOT", "OKf"], ["SLOT"])
                        op("dve", lambda: nc.vector.tensor_scalar(out=SLOT[:], in0=SLOT[:], scalar1=float(DUMP), scalar2=None, op0=ALU.add),
                           ["SLOT"], ["SLOT"])
                        op("dve", lambda: nc.vector.tensor_tensor(out=cum[:], in0=cum[:], in1=SELf[:], op=ALU.add), ["cum", "SELf", "CUMb"], ["cum"])
                        for k in range(4):
                            op("dve", lambda k=k: nc.vector.tensor_tensor(out=TMP[:], in0=OH[:, k, :], in1=SLOT[:], op=ALU.mult), ["OH", "SLOT"], ["TMP"])
                            op("dve", lambda k=k: nc.vector.reduce_sum(out=slf[:, k:k + 1], in_=TMP[:], axis=AX.X), ["TMP"], ["slf"])
                        op("dve", lambda tg=tg: nc.vector.tensor_copy(out=SL[:, tg, :], in_=slf[:]), ["slf"], ["SL"])
                        for k in range(4):
                            dma("pool", lambda q, k=k, tg=tg, HBc=HBc: q.indirect_dma_start(
                                out=xg_d[:, :], out_offset=bass.IndirectOffsetOnAxis(ap=SL[:, tg, k:k + 1], axis=0),
                                in_=HBc[:], in_offset=None), [hbk, "SL"], ["xg_d"])
                    T.wait_all("sp", ["WO", "LB", "WR", "BR", "cxt0", "cxt1", "HP", "H0", "H1", "HB0", "HB1", "HT", "CATTa", "CATTp",
                                      "L", "LW", "OH", "SELf", "SELb", "CUMb", "MX", "EX", "nm0", "ssum", "SLOT", "OKf", "TMP", "slf", "stats"])

        with contextlib.ExitStack() as m_:
            XG = [sb(m_, "XG%d" % i, [128, D], BF16) for i in range(2)]
            XGT = sb(m_, "XGT", [128, KC, C], BF16)
            ACTT = sb(m_, "ACTT", [128, FC, C], BF16)
            wr = [sb(m_, "mw%d" % i, [128, KC, 512], BF16) for i in range(3)]
            BG = sb(m_, "BG", [128, E * 2 * FC], F32)
            BG1 = sb(m_, "BG1", [128, E * 2 * FC], F32)
            bdf = [sb(m_, "bdf%d" % i, [1, D], F32) for i in range(2)]
            bdb = [sb(m_, "bdb%d" % i, [1, D], BF16) for i in range(2)]
            GL = [sb(m_, "GL%d" % i, [128, 512], F32) for i in range(2)]
            SG = [sb(m_, "SG%d" % i, [128, 512], F32) for i in range(2)]
            TU = [sb(m_, "TU%d" % i, [128, 512], F32) for i in range(2)]
            YS = [sb(m_, "YS%d" % i, [128, 512], F32) for i in range(4)]
            dma("sp", lambda q: q.dma_start(out=BG[:], in_=bgcol), writes=["BG"])
            op("dve", lambda: nc.vector.tensor_scalar(out=BG1[:], in0=BG[:], scalar1=1.0, scalar2=None, op0=ALU.add), ["BG"], ["BG1"])
            MB = 2 if FC >= 2 else 1
            wslot = 0
            epi = 0
            ysi = 0
            for e in range(E):
                w_gu_v = w_gu[e * D:(e + 1) * D, :].rearrange("(k p) (g n) -> p k g n", p=128, g=2)
                w_dn_v = w_dn[e * DFF:(e + 1) * DFF, :].rearrange("(k p) n -> p k n", p=128)
                dma("sp", lambda q, e=e: q.dma_start(out=bdf[e % 2][:], in_=b_dn[e:e + 1, :]), writes=["bdf%d" % (e % 2)])
                op("dve", lambda e=e: nc.vector.tensor_copy(out=bdb[e % 2][:], in_=bdf[e % 2][:]), ["bdf%d" % (e % 2)], ["bdb%d" % (e % 2)])
                for st in range(CT):
                    xg = XG[st % 2]; xgk = "XG%d" % (st % 2)
                    r0 = e * C + st * 128
                    dma("sp", lambda q, r0=r0, xg=xg: q.dma_start(out=xg[:], in_=xg_d[r0:r0 + 128, :]), ["xg_d"], [xgk])
                    for cg in range(4):
                        pt = PT[cg % 2]; pk = ptkey[cg % 2]
                        for ci in range(4):
                            c = cg * 4 + ci
                            op("pe", lambda c=c, ci=ci, pt=pt, xg=xg: nc.tensor.transpose(
                                out=pt[:, ci * 128:(ci + 1) * 128], in_=xg[:, c * 128:(c + 1) * 128], identity=ident_b[:]),
                               [xgk, "ident_b"], [pk], signal=(ci == 3))
                        eng = "dve" if cg % 2 == 0 else "act"
                        if eng == "dve":
                            op("dve", lambda cg=cg, pt=pt, st=st: nc.vector.tensor_copy(
                                out=XGT[:, cg * 4:(cg + 1) * 4, st * 128:(st + 1) * 128], in_=pt[:].rearrange("p (c t) -> p c t", c=4)),
                               [pk], ["XGT"])
                        else:
                            op("act", lambda cg=cg, pt=pt, st=st: nc.scalar.activation(
                                out=XGT[:, cg * 4:(cg + 1) * 4, st * 128:(st + 1) * 128], in_=pt[:].rearrange("p (c t) -> p c t", c=4),
                                func=AF.Copy), [pk], ["XGT"])
                for pj in range(FC // MB):
                    wb = wr[wslot % 3]; wk = "mw%d" % (wslot % 3); wslot += 1
                    for g in range(2):
                        dma("pool", lambda q, pj=pj, g=g, wb=wb: q.dma_start(
                            out=wb[:, :, g * 128 * MB:(g + 1) * 128 * MB], in_=w_gu_v[:, :, g, pj * 128 * MB:(pj + 1) * 128 * MB]), writes=[wk + "g%d" % g])
                    for mi in range(MB):
                        m = pj * MB + mi
                        bgc = (e * 2 + 0) * FC + m
                        buc = (e * 2 + 1) * FC + m
                        for (n0, nn) in cfg.NS:
                            bg_, bu_ = (0, 1) if epi % 2 == 0 else (2, 3)
                            gl = GL[epi % 2]; sg = SG[epi % 2]; tu = TU[epi % 2]
                            glk, sgk, tuk = "GL%d" % (epi % 2), "SG%d" % (epi % 2), "TU%d" % (epi % 2)
                            epi += 1
                            for g, bb in ((0, bg_), (1, bu_)):
                                for k in range(KC):
                                    op("pe", lambda k=k, g=g, bb=bb, mi=mi, n0=n0, nn=nn, wb=wb: nc.tensor.matmul(
                                        bank(bb, nn), lhsT=wb[:, k, g * 128 * MB + mi * 128:g * 128 * MB + (mi + 1) * 128],
                                        rhs=XGT[:, k, n0:n0 + nn], start=(k == 0), stop=(k == KC - 1)),
                                       [wk + "g%d" % g, "XGT"], [bkey(bb)], signal=(k == KC - 1))
                            op("dve", lambda gl=gl, bg_=bg_, nn=nn, bgc=bgc: nc.vector.tensor_scalar(
                                out=gl[:, 0:nn], in0=bank(bg_, nn), scalar1=BG[:, bgc:bgc + 1], scalar2=7.0, op0=ALU.add, op1=ALU.min),
                               [bkey(bg_), "BG"], [glk])
                            op("act", lambda gl=gl, sg=sg, nn=nn: nc.scalar.activation(out=sg[:, 0:nn], in_=gl[:, 0:nn], func=AF.Sigmoid, scale=1.702),
                               [glk], [sgk])
                            op("dve", lambda tu=tu, bu_=bu_, nn=nn, buc=buc: nc.vector.tensor_scalar(
                                out=tu[:, 0:nn], in0=bank(bu_, nn), scalar1=BG1[:, buc:buc + 1], scalar2=8.0, op0=ALU.add, op1=ALU.min),
                               [bkey(bu_), "BG1"], [tuk])
                            op("dve", lambda tu=tu, gl=gl, nn=nn: nc.vector.scalar_tensor_tensor(
                                out=tu[:, 0:nn], in0=tu[:, 0:nn], scalar=-6.0, in1=gl[:, 0:nn], op0=ALU.max, op1=ALU.mult),
                               [tuk, glk], [tuk])
                            op("pool", lambda tu=tu, sg=sg, nn=nn, m=m, n0=n0: nc.gpsimd.tensor_tensor(
                                out=ACTT[:, m, n0:n0 + nn], in0=tu[:, 0:nn], in1=sg[:, 0:nn], op=ALU.mult), [tuk, sgk], ["ACTT"])
                for j in range(4):
                    wb = wr[wslot % 3]; wk = "mw%d" % (wslot % 3); wslot += 1
                    dma("pool", lambda q, j=j, wb=wb: q.dma_start(out=wb[:, 0:FC, :], in_=w_dn_v[:, :, j * 512:(j + 1) * 512]), writes=[wk + "g0", wk + "g1"])
                    for st in range(CT):
                        bi = 4 + (ysi % 2)
                        ys = YS[ysi % 4]; ysk = "YS%d" % (ysi % 4); ysi += 1
                        for m in range(FC):
                            op("pe", lambda m=m, st=st, bi=bi, wb=wb: nc.tensor.matmul(
                                bank(bi, 512), lhsT=ACTT[:, m, st * 128:(st + 1) * 128], rhs=wb[:, m, :], start=(m == 0), stop=False),
                               ["ACTT", wk + "g0", wk + "g1"], [bkey(bi)], signal=False)
                        op("pe", lambda bi=bi, j=j, e=e: nc.tensor.matmul(
                            bank(bi, 512), lhsT=ones_b[0:1, :], rhs=bdb[e % 2][0:1, j * 512:(j + 1) * 512], start=False, stop=True),
                           ["ones_b", "bdb%d" % (e % 2)], [bkey(bi)], signal=True)
                        if ysi % 2 == 0:
                            op("act", lambda ys=ys, bi=bi: nc.scalar.activation(out=ys[:], in_=bank(bi, 512), func=AF.Copy), [bkey(bi)], [ysk])
                        else:
                            op("dve", lambda ys=ys, bi=bi: nc.vector.tensor_copy(out=ys[:], in_=bank(bi, 512)), [bkey(bi)], [ysk])
                        r0 = e * C + st * 128
                        dma("sp", lambda q, r0=r0, j=j, ys=ys: q.dma_start(out=yy_d[r0:r0 + 128, j * 512:(j + 1) * 512], in_=ys[:]), [ysk], ["yy_d"])
            T.wait_all("sp", ["XG0", "XG1", "XGT", "ACTT", "mw0", "mw1", "mw2", "BG", "BG1", "bdf0", "bdf1", "bdb0", "bdb1",
                              "GL0", "GL1", "SG0", "SG1", "TU0", "TU1", "YS0", "YS1", "YS2", "YS3"])

        with contextlib.ExitStack() as f_:
            YG = [[sb(f_, "YG%d_%d" % (i, k), [128, D], F32) for k in range(4)] for i in range(2)]
            HH = [sb(f_, "HH%d" % i, [128, D], F32) for i in range(2)]
            OU = [sb(f_, "OU%d" % i, [128, D], F32) for i in range(2)]
            LB2 = sb(f_, "LB2", [128, 2, D], F32)
            st6 = sb(f_, "fst6", [128, 24], F32)
            mv = sb(f_, "fmv", [128, 2], F32)
            rstd = sb(f_, "frstd", [128, 1], F32)
            for j in range(2):
                dma("sp", lambda q, j=j: q.dma_start(out=LB2[:, j, :], in_=lnv[4 + j:5 + j, :].partition_broadcast(128)), writes=["LF%d" % j])
            for tg in range(NT):
                i = tg % 2
                hh = HH[i]; hhk = "HH%d" % i
                ou = OU[i]; ouk = "OU%d" % i
                dma("sp", lambda q, tg=tg, hh=hh: q.dma_start(out=hh[:], in_=h_d[tg * 128:(tg + 1) * 128, :]), ["h_d"], [hhk])
                for k in range(4):
                    dma("pool", lambda q, tg=tg, k=k, i=i: q.indirect_dma_start(
                        out=YG[i][k][:], out_offset=None, in_=yy_d[:, :],
                        in_offset=bass.IndirectOffsetOnAxis(ap=SL[:, tg, k:k + 1], axis=0)),
                        ["yy_d", "yy_dump", "SL"], ["YG%d_%d" % (i, k)])
                op("act", lambda hh=hh: nc.scalar.activation(out=hh[:], in_=hh[:], func=AF.Copy, scale=ALPHA), [hhk], [hhk])
                for k in range(4):
                    op("dve", lambda tg=tg, k=k, i=i, hh=hh: nc.vector.scalar_tensor_tensor(
                        out=hh[:], in0=YG[i][k][:], scalar=GT[:, tg, k:k + 1], in1=hh[:], op0=ALU.mult, op1=ALU.add),
                       ["YG%d_%d" % (i, k), "GT", hhk], [hhk])
                ln_stats(hh, hhk, st6, mv[:], rstd[:], "f")
                op("dve", lambda hh=hh: nc.vector.tensor_scalar(out=hh[:], in0=hh[:], scalar1=mv[:, 0:1], scalar2=rstd[:],
                                                                op0=ALU.subtract, op1=ALU.mult), [hhk, "fmv", "frstd"], [hhk])
                op("pool", lambda hh=hh: nc.gpsimd.tensor_tensor(out=hh[:], in0=hh[:], in1=LB2[:, 0, :], op=ALU.mult), [hhk, "LF0"], [hhk])
                op("dve", lambda hh=hh, ou=ou: nc.vector.tensor_tensor(out=ou[:], in0=hh[:], in1=LB2[:, 1, :], op=ALU.add), [hhk, "LF1"], [ouk])
                dma("sp", lambda q, tg=tg, ou=ou: q.dma_start(out=y_out[tg * 128:(tg + 1) * 128, :], in_=ou[:]), [ouk], ["y_out"])
            keys = ["y_out", "OU0", "OU1", "HH0", "HH1", "LB2"] + ["YG%d_%d" % (i, k) for i in range(2) for k in range(4)]
            T.wait_all("sp", keys)
            for q in ("sp", "pool", "act"):
                for name in T.dq[q][0]:
                    if T.cnt[name]:
                        nc.sync.wait_ge(T.sem[name], T.cnt[name])
    return nc


def _unit_table(seq_rows):
    units = []
    for si, R in enumerate(seq_rows):
        for b in range(R // 16):
            units.append((si, b))
    return units


def _geometry(R, b):
    mask = np.full((128, NMASK, 128), NEG, np.float32)
    c = np.arange(64)
    cs = np.clip(c - 8, 0, 48)
    colok = (c[None, :] >= cs[:, None]) & (c[None, :] < cs[:, None] + 16)
    for p in range(8):
        for i, o in enumerate(pair_offsets(p)):
            mi = mask_index(p, i)
            for qr in range(2):
                r = 16 * b + 2 * p + qr
                rs = min(max(r - 4, 0), R - 8)
                for kr in range(2):
                    ka = 16 * b + 2 * p + 2 * o + kr
                    if 0 <= ka < R and rs <= ka < rs + 8:
                        blk = np.where(colok, 0.0, NEG).astype(np.float32)
                        mask[qr * 64:(qr + 1) * 64, mi, kr * 64:(kr + 1) * 64] = blk
    L = R * 64
    t0 = 16 * b * 64
    pv = np.zeros(16, np.float32)
    for j in range(8):
        pv[j] = 1.0 if 0 <= t0 - 8 + j < L else 0.0
        pv[8 + j] = 1.0 if 0 <= t0 + 1024 + j < L else 0.0
    rc = np.zeros(64, np.float32)
    for g, w in enumerate((2, 4, 8, 16)):
        half = w // 2
        for j in range(8):
            for (off, tt) in ((0, t0 + j), (8, t0 + 1016 + j)):
                lo = min(max(tt - half, 0), L)
                hi = min(max(tt + half, 0), L)
                rc[g * 16 + off + j] = 1.0 / float(hi - lo)
    return mask, pv, rc


def _bias_table(rpb):
    H = rpb.shape[0]
    out = np.zeros((H, 128, 7, 128), np.float32)
    c = np.arange(64)
    dc = c[None, :] - c[:, None] + 15
    okc = (dc >= 0) & (dc <= 30)
    dcc = np.clip(dc, 0, 30)
    for o in range(-3, 4):
        for qr in range(2):
            for kr in range(2):
                dr = 2 * o + kr - qr + 7
                if 0 <= dr <= 14:
                    blk = np.where(okc[None], rpb[:, dr, :][:, dcc], 0.0)
                    out[:, qr * 64:(qr + 1) * 64, o + 3, kr * 64:(kr + 1) * 64] = blk
    return out.reshape(H * 128, 7 * 128)


def prepare_inputs(cfg, xs, p):
    E, DFF, FC, NU, C = cfg.E, cfg.DFF, cfg.FC, cfg.NU, cfg.C
    seq_rows = [x.shape[0] // GRID_W for x in xs]
    units = _unit_table(seq_rows)
    assert len(units) == NU * cfg.NCORES, (len(units), NU, cfg.NCORES)
    f32 = np.float32
    lnv = np.stack([p["ln_in_g"], p["ln_in_b"], p["ln1_g"], p["ln1_b"], p["ln2_g"], p["ln2_b"]]).astype(f32)
    lncol = np.concatenate([p["ln_in_g"].reshape(16, 128).T, p["ln_in_b"].reshape(16, 128).T], axis=1).astype(f32)
    pscol = np.ascontiguousarray(p["pool_scale"].reshape(8, 128).T).astype(f32)
    bgcol = np.ascontiguousarray(p["b_gate_up"].reshape(E * 2 * FC, 128).T).astype(f32)
    consts = np.zeros((128, 128 * 3 + 8 + E), f32)
    consts[:, 0:128] = np.eye(128, dtype=f32)
    consts[:, 128:256] = np.triu(np.ones((128, 128), f32), 1)
    consts[:, 256:384] = 1.0
    for j in range(4):
        consts[32 * j:32 * (j + 1), 384 + j] = 1.0
    consts[:, 392:392 + E] = (np.arange(E, dtype=f32) * C)[None, :]
    shared = {
        "bias_in": _bias_table(p["rpb"].astype(f32)), "lnv": lnv, "lncol": np.ascontiguousarray(lncol),
        "w_in": p["w_in"], "w_pool": p["w_pool"].reshape(4 * 256, 256), "pscol": pscol, "w_out": p["w_out"],
        "w_router": p["w_router"], "b_router": p["b_router"].reshape(1, E), "w_gu": p["w_gate_up"].reshape(E * D, 2 * DFF),
        "bgcol": bgcol, "w_dn": p["w_down"].reshape(E * DFF, D), "b_dn": p["b_down"].reshape(E, D), "consts": consts,
    }
    in_maps = []
    for c in range(cfg.NCORES):
        xin = np.zeros((NU * UT, D), f32)
        masks = np.zeros((NU * 128, NMASK * 128), f32)
        pvs = np.zeros((NU, 16), f32)
        rcs = np.zeros((NU, 64), f32)
        for ui in range(NU):
            si, b = units[c * NU + ui]
            R = seq_rows[si]
            t_lo = (16 * b - 4) * 64
            t_hi = t_lo + UT
            a, bnd = max(t_lo, 0), min(t_hi, R * 64)
            xin[ui * UT + (a - t_lo): ui * UT + (bnd - t_lo)] = xs[si][a:bnd]
            mk, pv, rc = _geometry(R, b)
            masks[ui * 128:(ui + 1) * 128] = mk.reshape(128, NMASK * 128)
            pvs[ui] = pv
            rcs[ui] = rc
        m = dict(shared)
        m.update({"x_in": xin, "mask_in": masks, "pv_in": pvs, "rc_in": rcs})
        in_maps.append(m)
    return in_maps, units


def assemble(cfg, results, units, seq_lens):
    outs = [np.zeros((L, D), np.float32) for L in seq_lens]
    for c in range(cfg.NCORES):
        y = results[c]["y_out"]
        for ui in range(cfg.NU):
            si, b = units[c * cfg.NU + ui]
            outs[si][b * 1024:(b + 1) * 1024] = y[ui * 1024:(ui + 1) * 1024]
    return outs


_NC_CACHE = {}


def run(cfg, xs, p):
    key = (cfg.E, cfg.DFF, cfg.NU, cfg.NCORES, cfg.C)
    if key not in _NC_CACHE:
        _NC_CACHE[key] = build(cfg)
    nc = _NC_CACHE[key]
    in_maps, units = prepare_inputs(cfg, xs, p)
    res = run_bass_kernel_spmd(nc, in_maps, core_ids=list(range(cfg.NCORES)))
    return assemble(cfg, res.results, units, [x.shape[0] for x in xs])


def kernel(x_prompt, x_sample, ln_in_g, ln_in_b, w_in, rpb, w_pool, pool_scale, w_out, ln1_g, ln1_b,
           w_router, b_router, w_gate_up, b_gate_up, w_down, b_down, ln2_g, ln2_b):
    cfg = Cfg()
    a = lambda v: np.asarray(v, dtype=np.float32)
    p = {"ln_in_g": a(ln_in_g), "ln_in_b": a(ln_in_b), "w_in": a(w_in)[0], "rpb": a(rpb)[0], "w_pool": a(w_pool)[0],
         "pool_scale": a(pool_scale)[0], "w_out": a(w_out)[0], "ln1_g": a(ln1_g)[0], "ln1_b": a(ln1_b)[0],
         "w_router": a(w_router)[0], "b_router": a(b_router)[0], "w_gate_up": a(w_gate_up)[0], "b_gate_up": a(b_gate_up)[0],
         "w_down": a(w_down)[0], "b_down": a(b_down)[0], "ln2_g": a(ln2_g)[0], "ln2_b": a(ln2_b)[0]}
    xp = a(x_prompt)
    xsm = a(x_sample)
    xs = [xp[i] for i in range(xp.shape[0])] + [xsm[i] for i in range(xsm.shape[0])]
    outs = run(cfg, xs, p)
    y_prompt = np.stack(outs[:xp.shape[0]]).astype(np.float32)
    y_sample = np.stack(outs[xp.shape[0]:]).astype(np.float32)
    return (y_prompt, y_sample)
```

```python
import contextlib
import numpy as np
import concourse.bass as bass
import concourse.mybir as mybir
from concourse.bass_utils import run_bass_kernel_spmd

F32 = mybir.dt.float32
BF16 = mybir.dt.bfloat16
I32 = mybir.dt.int32
AF = mybir.ActivationFunctionType
ALU = mybir.AluOpType
AX = mybir.AxisListType

D = 2048
KC = 16
NH = 32
GRID_W = 64
LN_EPS = 1e-5
ALPHA = float(2 ** 0.25)
NEG = -30000.0
VS = 34
UT = 1536
NMASK = 42
DBG = False


class Cfg:
    def __init__(self, E=32, DFF=2048, NU=5, NCORES=8, C=768):
        self.E, self.DFF, self.NU, self.NCORES, self.C = E, DFF, NU, NCORES, C
        self.FC = DFF // 128
        self.CT = C // 128
        self.NT = NU * 8
        self.NSLOT = E * C + 128
        self.DUMP = E * C
        ns, s = [], 0
        while s < C:
            n = min(512, C - s)
            if C % 448 == 0:
                n = min(448, C - s)
            ns.append((s, n)); s += n
        self.NS = ns


def pair_offsets(p):
    if p == 0:
        return [-2, -1, 0, 1, 2, 3]
    if p == 7:
        return [-3, -2, -1, 0, 1, 2]
    return [-2, -1, 0, 1, 2]


def mask_index(p, i):
    if p == 0:
        return i
    return 6 + (p - 1) * 5 + i


class Trk:
    def __init__(self, nc, es):
        self.nc = nc
        self.eng = {"pe": nc.tensor, "act": nc.scalar, "dve": nc.vector, "pool": nc.gpsimd, "sp": nc.sync}
        self.sem = {}
        self.cnt = {}
        for e in ("pe", "act", "dve", "pool"):
            self.sem[e] = es.enter_context(nc.semaphore("sem_" + e))
            self.cnt[e] = 0
        self.dq = {}
        for q, k in (("sp", 8), ("pool", 8), ("act", 4)):
            sems = []
            for i in range(k):
                name = "dq_%s_%d" % (q, i)
                self.sem[name] = es.enter_context(nc.semaphore(name))
                self.cnt[name] = 0
                sems.append(name)
            self.dq[q] = [sems, 0]
        self.state = {}
        self.seen = {e: {} for e in self.eng}

    def _st(self, k):
        s = self.state.get(k)
        if s is None:
            s = {"w": None, "r": {}}
            self.state[k] = s
        return s

    def _waits(self, engine, reads, writes, extra=()):
        need = {}

        def add(ev):
            if ev is None:
                return
            s, v = ev
            if engine == "pe" and s == "pe":
                return
            if need.get(s, 0) < v:
                need[s] = v

        for k in reads:
            add(self._st(k)["w"])
        for k in writes:
            st = self._st(k)
            add(st["w"])
            for s, v in st["r"].items():
                add((s, v))
        for ev in extra:
            add(ev)
        seen = self.seen[engine]
        for s, v in need.items():
            if seen.get(s, 0) >= v:
                continue
            self.eng[engine].wait_ge(self.sem[s], v)
            seen[s] = v

    def _record(self, ev, reads, writes):
        s, v = ev
        for k in reads:
            r = self._st(k)["r"]
            if r.get(s, 0) < v:
                r[s] = v
        for k in writes:
            self.state[k] = {"w": ev, "r": {}}

    def op(self, engine, fn, reads=(), writes=(), signal=True):
        self._waits(engine, reads, writes)
        ins = fn()
        if signal:
            self.cnt[engine] += 1
            ins.then_inc(self.sem[engine], 1)
            ev = (engine, self.cnt[engine])
        else:
            ev = (engine, self.cnt[engine] + 1)
        self._record(ev, reads, writes)
        return ins

    def dma(self, q, fn, reads=(), writes=()):
        sems, i = self.dq[q]
        name = sems[i % len(sems)]
        self.dq[q][1] = i + 1
        prev = (name, self.cnt[name])
        self._waits(q, reads, writes, extra=(prev,) if self.cnt[name] else ())
        ins = fn(self.eng[q])
        self.cnt[name] += 16
        ins.then_inc(self.sem[name], 16)
        ev = (name, self.cnt[name])
        self._record(ev, reads, writes)
        return ev

    def wait_all(self, engine, keys):
        self.barrier()

    def barrier(self):
        evs = [(s, v) for s, v in self.cnt.items() if v > 0]
        for e in ("pe", "act", "dve", "pool", "sp"):
            seen = self.seen[e]
            for s, v in evs:
                if s == e and e == "pe":
                    continue
                if seen.get(s, 0) >= v:
                    continue
                self.eng[e].wait_ge(self.sem[s], v)
                seen[s] = v
        self.state = {}


def build(cfg):
    nc = bass.Bass("TRN2", target_bir_lowering=False)
    E, DFF, FC, NU, C, CT, NT = cfg.E, cfg.DFF, cfg.FC, cfg.NU, cfg.C, cfg.CT, cfg.NT
    NSLOT, DUMP = cfg.NSLOT, cfg.DUMP

    def din(name, shape, dt=F32):
        return nc.dram_tensor(name, list(shape), dt, kind="ExternalInput").ap()

    def dscr(name, shape, dt):
        return nc.dram_tensor(name, list(shape), dt, kind="Internal").ap()

    x_in = din("x_in", [NU * UT, D])
    mask_in = din("mask_in", [NU * 128, NMASK * 128])
    bias_in = din("bias_in", [NH * 128, 7 * 128])
    pv_in = din("pv_in", [NU, 16])
    rc_in = din("rc_in", [NU, 64])
    lnv = din("lnv", [6, D])
    lncol = din("lncol", [128, 32])
    w_in = din("w_in", [D, 4096])
    w_pool = din("w_pool", [4 * 256, 256])
    pscol = din("pscol", [128, 8])
    w_out = din("w_out", [D, D])
    w_router = din("w_router", [D, E])
    b_router = din("b_router", [1, E])
    w_gu = din("w_gu", [E * D, 2 * DFF])
    bgcol = din("bgcol", [128, E * 2 * FC])
    w_dn = din("w_dn", [E * DFF, D])
    b_dn = din("b_dn", [E, D])
    consts = din("consts", [128, 128 * 3 + 8 + E])
    y_out = nc.dram_tensor("y_out", [NU * 1024, D], F32, kind="ExternalOutput").ap()

    qt_d = dscr("qt_d", [1024, 1024], BF16)
    kt_d = dscr("kt_d", [1024, UT], BF16)
    v_d = dscr("v_d", [UT, NH * VS], BF16)
    pm_d = dscr("pm_d", [1024, 1024], BF16)
    h_d = dscr("h_d", [NT * 128, D], F32)
    xg_d = dscr("xg_d", [NSLOT, D], BF16)
    yy_d = dscr("yy_d", [NSLOT, D], F32)

    es = contextlib.ExitStack()
    with es:
        T = Trk(nc, es)
        op, dma = T.op, T.dma

        uid = [0]

        def sb(stack, name, shape, dt):
            uid[0] += 1
            return stack.enter_context(nc.sbuf_tensor("%s_%d" % (name, uid[0]), list(shape), dt))

        PS = [es.enter_context(nc.psum_tensor("ps%d" % i, [128, 1024], F32)) for i in range(2)]
        PO = [es.enter_context(nc.psum_tensor("po%d" % i, [128, 512], F32)) for i in range(2)]
        PT = [es.enter_context(nc.psum_tensor("pt%d" % i, [128, 512], BF16)) for i in range(2)]
        banks = [(PS[0], 0), (PS[0], 512), (PS[1], 0), (PS[1], 512), (PO[0], 0), (PO[1], 0)]

        def bank(i, n=512):
            t, o = banks[i]
            return t[:, o:o + n]

        def bkey(i):
            return "bank%d" % i

        pskeys = [("bank0", "bank1"), ("bank2", "bank3")]
        ptkey = ["ptb0", "ptb1"]

        cst = sb(es, "cst", [128, 128 * 3 + 8 + E], F32)
        ident_b = sb(es, "ident_b", [128, 128], BF16)
        ustr_b = sb(es, "ustr_b", [128, 128], BF16)
        ones_b = sb(es, "ones_b", [128, 128], BF16)
        SL = sb(es, "SL", [128, NT, 4], I32)
        GT = sb(es, "GT", [128, NT, 4], F32)
        cum = sb(es, "cum", [128, E], F32)
        zer = sb(es, "zer", [128, D], F32)
        dma("sp", lambda q: q.dma_start(out=cst[:], in_=consts), writes=["cst"])
        ident_f = cst[:, 0:128]
        hm = cst[:, 384:388]
        ebase = cst[:, 392:392 + E]
        op("dve", lambda: nc.vector.tensor_copy(out=ident_b[:], in_=cst[:, 0:128]), ["cst"], ["ident_b"])
        op("dve", lambda: nc.vector.tensor_copy(out=ustr_b[:], in_=cst[:, 128:256]), ["cst"], ["ustr_b"])
        op("dve", lambda: nc.vector.tensor_copy(out=ones_b[:], in_=cst[:, 256:384]), ["cst"], ["ones_b"])
        op("dve", lambda: nc.vector.memset(cum[:], 0.0), [], ["cum"])
        op("pool", lambda: nc.gpsimd.memset(zer[:], 0.0), [], ["zer"])
        dma("sp", lambda q: q.dma_start(out=yy_d[DUMP:DUMP + 128, :], in_=zer[:]), ["zer"], ["yy_dump"])
        zb = zer[:].bitcast(BF16)
        for r0 in range(0, NSLOT, 256):
            nr = min(256, NSLOT - r0)
            dma("sp", lambda q, r0=r0, nr=nr: q.dma_start(
                out=xg_d[r0:r0 + nr, :].rearrange("(p two) d -> p (two d)", two=nr // 128), in_=zb[:, 0:D * (nr // 128)]),
                ["zer"], ["xg_zero"])

        def ln_stats(xt_ap, xkey, st6, mv, rstd, tag):
            for c in range(4):
                op("dve", lambda c=c: nc.vector.bn_stats(out=st6[:, c * 6:(c + 1) * 6], in_=xt_ap[:, c * 512:(c + 1) * 512]),
                   [xkey], [tag + "st6"])
            op("dve", lambda: nc.vector.bn_aggr(out=mv, in_=st6[:, 0:24]), [tag + "st6"], [tag + "mv"])
            op("dve", lambda: nc.vector.tensor_scalar(out=rstd, in0=mv[:, 1:2], scalar1=LN_EPS, scalar2=None, op0=ALU.add),
               [tag + "mv"], [tag + "rstd"])
            op("act", lambda: nc.scalar.activation(out=rstd, in_=rstd, func=AF.Sqrt), [tag + "rstd"], [tag + "rstd"])
            op("dve", lambda: nc.vector.reciprocal(out=rstd, in_=rstd), [tag + "rstd"], [tag + "rstd"])

        for u in range(NU):
            xu = x_in[u * UT:(u + 1) * UT, :]
            with contextlib.ExitStack() as us:
                stats = sb(us, "stats", [128, 8, 2], F32)
                with contextlib.ExitStack() as a:
                    XT = sb(a, "XT", [128, KC, UT], BF16)
                    xt = [sb(a, "xt%d" % i, [128, D], F32) for i in range(2)]
                    xh = sb(a, "xh", [128, D], BF16)
                    st6 = sb(a, "st6", [128, 24], F32)
                    mv = sb(a, "mv", [128, 2], F32)
                    rstd = sb(a, "rstd", [128, 1], F32)
                    lnc = sb(a, "lnc", [128, 32], F32)
                    wr = [sb(a, "wr%d" % i, [128, KC, 256], BF16) for i in range(3)]
                    QTs = sb(a, "QTs", [128, 1024], BF16)
                    KTs = sb(a, "KTs", [128, UT], BF16)
                    Vb = sb(a, "Vb", [128, 12, NH, VS], BF16)
                    UF = sb(a, "UF", [128, UT], F32)
                    PA = sb(a, "PA", [128, UT], F32)
                    PB = sb(a, "PB", [128, UT], F32)
                    PTg = sb(a, "PTg", [128, 2, 1024], BF16)
                    PMs = sb(a, "PMs", [128, 1024], BF16)
                    wp = sb(a, "wp", [128, 4, 2, 256], BF16)
                    psc = sb(a, "psc", [128, 8], F32)
                    pvb = sb(a, "pvb", [128, 16], F32)
                    rcb = sb(a, "rcb", [128, 64], F32)
                    tmp8 = sb(a, "tmp8", [128, 8], F32)

                    dma("sp", lambda q: q.dma_start(out=lnc[:], in_=lncol), writes=["lnc"])
                    dma("sp", lambda q: q.dma_start(out=psc[:], in_=pscol), writes=["psc"])
                    dma("sp", lambda q: q.dma_start(out=pvb[:], in_=pv_in[u:u + 1, :].partition_broadcast(128)), writes=["pvb"])
                    dma("sp", lambda q: q.dma_start(out=rcb[:], in_=rc_in[u:u + 1, :].partition_broadcast(128)), writes=["rcb"])
                    dma("pool", lambda q: q.dma_start(out=wp[:], in_=w_pool.rearrange("(g k p) n -> p g k n", g=4, k=2, p=128)),
                        writes=["wp"])
                    op("pool", lambda: nc.gpsimd.memset(Vb[:], 1.0), [], ["Vb"])

                    for t in range(12):
                        xb = xt[t % 2]
                        xk = "xt%d" % (t % 2)
                        dma("sp", lambda q, t=t, xb=xb: q.dma_start(out=xb[:], in_=xu[t * 128:(t + 1) * 128, :]), writes=[xk])
                        ln_stats(xb, xk, st6, mv[:], rstd[:], "a")
                        if 2 <= t < 10:
                            op("dve", lambda t=t: nc.vector.tensor_copy(out=stats[:, t - 2, 0:1], in_=mv[:, 0:1]), ["amv"], ["stats"])
                            op("dve", lambda t=t: nc.vector.tensor_copy(out=stats[:, t - 2, 1:2], in_=rstd[:]), ["arstd"], ["stats"])
                        op("dve", lambda xb=xb: nc.vector.tensor_scalar(out=xh[:], in0=xb[:], scalar1=mv[:, 0:1], scalar2=rstd[:],
                                                                       op0=ALU.subtract, op1=ALU.mult),
                           [xk, "amv", "arstd"], ["xh"])
                        for cg in range(4):
                            pt = PT[cg % 2]
                            pk = ptkey[cg % 2]
                            for ci in range(4):
                                c = cg * 4 + ci
                                op("pe", lambda c=c, ci=ci, pt=pt: nc.tensor.transpose(out=pt[:, ci * 128:(ci + 1) * 128],
                                                                                      in_=xh[:, c * 128:(c + 1) * 128], identity=ident_b[:]),
                                   ["xh", "ident_b"], [pk], signal=(ci == 3))
                            for ci in range(4):
                                c = cg * 4 + ci
                                op("act", lambda c=c, ci=ci, pt=pt, t=t: nc.scalar.activation(
                                    out=XT[:, c, t * 128:(t + 1) * 128], in_=pt[:, ci * 128:(ci + 1) * 128], func=AF.Identity,
                                    bias=lnc[:, 16 + c:17 + c], scale=lnc[:, c:c + 1]), [pk, "lnc"], ["XT"])

                    w_in_v = w_in.rearrange("(k p) n -> p k n", p=128)
                    for pc in range(16):
                        wb = wr[pc % 3]
                        wk = "wr%d" % (pc % 3)
                        dma("pool", lambda q, pc=pc, wb=wb: q.dma_start(out=wb[:], in_=w_in_v[:, :, pc * 256:(pc + 1) * 256]), writes=[wk])
                        kind = pc // 4
                        if kind in (0, 1):
                            for mi in range(2):
                                mc = (pc % 4) * 2 + mi
                                ntl = [(256, 512), (768, 512)] if kind == 0 else [(0, 512), (512, 512), (1024, 512)]
                                dst = QTs if kind == 0 else KTs
                                dk = "QTs" if kind == 0 else "KTs"
                                for j, (n0, nn) in enumerate(ntl):
                                    bi = j % 2
                                    for k in range(KC):
                                        op("pe", lambda k=k, mi=mi, n0=n0, nn=nn, bi=bi, wb=wb: nc.tensor.matmul(
                                            bank(bi, nn), lhsT=wb[:, k, mi * 128:(mi + 1) * 128], rhs=XT[:, k, n0:n0 + nn],
                                            start=(k == 0), stop=(k == KC - 1)), [wk, "XT"], [bkey(bi)], signal=(k == KC - 1))
                                    d0 = n0 - 256 if kind == 0 else n0
                                    op("act", lambda d0=d0, nn=nn, bi=bi, dst=dst, kind=kind: nc.scalar.activation(
                                        out=dst[:, d0:d0 + nn], in_=bank(bi, nn), func=AF.Copy,
                                        scale=(32.0 ** -0.5 if kind == 0 else 1.0)), [bkey(bi)], [dk])
                                dd = qt_d if kind == 0 else kt_d
                                dma("sp", lambda q, mc=mc, dst=dst, dd=dd: q.dma_start(out=dd[mc * 128:(mc + 1) * 128, :], in_=dst[:]),
                                    [dk], ["qk_d"])
                        elif kind == 2:
                            h0 = (pc % 4) * 8
                            for t in range(12):
                                bi = t % 2
                                for k in range(KC):
                                    op("pe", lambda k=k, t=t, bi=bi, wb=wb: nc.tensor.matmul(
                                        bank(bi, 256), lhsT=XT[:, k, t * 128:(t + 1) * 128], rhs=wb[:, k, :],
                                        start=(k == 0), stop=(k == KC - 1)), [wk, "XT"], [bkey(bi)], signal=(k == KC - 1))
                                op("dve", lambda t=t, bi=bi, h0=h0: nc.vector.tensor_copy(
                                    out=Vb[:, t, h0:h0 + 8, 0:32], in_=bank(bi, 256).rearrange("p (h d) -> p h d", h=8)),
                                   [bkey(bi)], ["Vb"])
                            if pc % 4 == 3:
                                dma("sp", lambda q: q.dma_start(out=v_d.rearrange("(t p) f -> p t f", p=128),
                                                                in_=Vb[:].rearrange("p t h s -> p t (h s)")), ["Vb"], ["v_d"])
                        else:
                            g = pc % 4
                            for mi in range(2):
                                for j, (n0, nn) in enumerate([(128, 512), (640, 512), (1152, 256)]):
                                    bi = j % 2
                                    for k in range(KC):
                                        op("pe", lambda k=k, mi=mi, n0=n0, nn=nn, bi=bi, wb=wb: nc.tensor.matmul(
                                            bank(bi, nn), lhsT=wb[:, k, mi * 128:(mi + 1) * 128], rhs=XT[:, k, n0:n0 + nn],
                                            start=(k == 0), stop=(k == KC - 1)), [wk, "XT"], [bkey(bi)], signal=(k == KC - 1))
                                    op("act", lambda n0=n0, nn=nn, bi=bi: nc.scalar.activation(
                                        out=UF[:, n0:n0 + nn], in_=bank(bi, nn), func=AF.Copy), [bkey(bi)], ["UF"])
                                op("dve", lambda: nc.vector.tensor_tensor(out=UF[:, 248:256], in0=UF[:, 248:256], in1=pvb[:, 0:8], op=ALU.mult),
                                   ["UF", "pvb"], ["UF"])
                                op("dve", lambda: nc.vector.tensor_tensor(out=UF[:, 1280:1288], in0=UF[:, 1280:1288], in1=pvb[:, 8:16], op=ALU.mult),
                                   ["UF", "pvb"], ["UF"])
                                lo, hi = 192, 1344
                                op("dve", lambda: nc.vector.tensor_tensor(out=PA[:, lo:hi], in0=UF[:, lo - 1:hi - 1], in1=UF[:, lo:hi], op=ALU.add),
                                   ["UF"], ["PA"])
                                cur, oth, ck, ok_ = PA, PB, "PA", "PB"
                                sh = 1
                                for lvl in range(g):
                                    lo += sh; hi -= sh
                                    op("dve", lambda cur=cur, oth=oth, lo=lo, hi=hi, sh=sh: nc.vector.tensor_tensor(
                                        out=oth[:, lo:hi], in0=cur[:, lo - sh:hi - sh], in1=cur[:, lo + sh:hi + sh], op=ALU.add), [ck], [ok_])
                                    cur, oth, ck, ok_ = oth, cur, ok_, ck
                                    sh *= 2
                                wsz = 2 << g
                                op("dve", lambda cur=cur, mi=mi, wsz=wsz: nc.vector.scalar_tensor_tensor(
                                    out=PTg[:, mi, :], in0=cur[:, 256:1280], scalar=1.0 / wsz, in1=UF[:, 256:1280],
                                    op0=ALU.mult, op1=ALU.subtract), [ck, "UF"], ["PTg"])
                                for (c0, r0) in ((256, 0), (1272, 8)):
                                    op("dve", lambda cur=cur, c0=c0, r0=r0, g=g: nc.vector.tensor_tensor(
                                        out=tmp8[:], in0=cur[:, c0:c0 + 8], in1=rcb[:, g * 16 + r0:g * 16 + r0 + 8], op=ALU.mult),
                                       [ck, "rcb"], ["tmp8"])
                                    op("dve", lambda c0=c0, mi=mi: nc.vector.tensor_tensor(
                                        out=PTg[:, mi, c0 - 256:c0 - 248], in0=tmp8[:], in1=UF[:, c0:c0 + 8], op=ALU.subtract),
                                       ["tmp8", "UF"], ["PTg"])
                            for half in range(2):
                                oc = 2 * g + half
                                for nt in range(2):
                                    bi = nt
                                    for kc in range(2):
                                        op("pe", lambda kc=kc, half=half, nt=nt, bi=bi, g=g: nc.tensor.matmul(
                                            bank(bi, 512), lhsT=wp[:, g, kc, half * 128:(half + 1) * 128], rhs=PTg[:, kc, nt * 512:(nt + 1) * 512],
                                            start=(kc == 0), stop=(kc == 1)), ["wp", "PTg"], [bkey(bi)], signal=(kc == 1))
                                    op("act", lambda nt=nt, bi=bi, oc=oc: nc.scalar.activation(
                                        out=PMs[:, nt * 512:(nt + 1) * 512], in_=bank(bi, 512), func=AF.Identity, scale=psc[:, oc:oc + 1]),
                                       [bkey(bi), "psc"], ["PMs"])
                                dma("sp", lambda q, oc=oc: q.dma_start(out=pm_d[oc * 128:(oc + 1) * 128, :], in_=PMs[:]), ["PMs"], ["pm_d"])
                    T.wait_all("sp", ["XT", "wr0", "wr1", "wr2", "Vb", "QTs", "KTs", "PMs", "UF", "PA", "PB", "PTg", "xt0", "xt1", "xh"])

                CATT = sb(us, "CATT", [128, KC, 1024], BF16)
                with contextlib.ExitStack() as b:
                    QT = sb(b, "QT", [128, 8, 1024], BF16)
                    KT = sb(b, "KT", [128, 8, UT], BF16)
                    V = sb(b, "V", [128, 12, NH, VS], BF16)
                    MK = sb(b, "MK", [128, NMASK, 128], BF16)
                    BS = [sb(b, "BS%d" % i, [128, 7, 128], BF16) for i in range(2)]
                    QH = [sb(b, "QH%d" % i, [128, 1024], BF16) for i in range(2)]
                    PE_ = [sb(b, "PE%d" % i, [128, 768], BF16) for i in range(3)]
                    ATM = sb(b, "ATM", [128, 8, 1024], BF16)
                    rden = sb(b, "rden", [128, 8], F32)
                    CB = [sb(b, "CB%d" % i, [128, NMASK, 128], BF16) for i in range(2)]
                    dma("sp", lambda q: q.dma_start(out=QT[:], in_=qt_d.rearrange("(c p) t -> p c t", p=128)), ["qk_d"], ["QT"])
                    dma("sp", lambda q: q.dma_start(out=KT[:], in_=kt_d.rearrange("(c p) t -> p c t", p=128)), ["qk_d"], ["KT"])
                    dma("sp", lambda q: q.dma_start(out=V[:].rearrange("p t h s -> p t (h s)"),
                                                    in_=v_d.rearrange("(t p) f -> p t f", p=128)), ["v_d"], ["V"])
                    dma("pool", lambda q: q.dma_start(out=MK[:].rearrange("p m k -> p (m k)"), in_=mask_in[u * 128:(u + 1) * 128, :]),
                        writes=["MK"])
                    dma("sp", lambda q: q.dma_start(out=CATT[:, 8:16, :], in_=pm_d.rearrange("(c p) t -> p c t", p=128)), ["pm_d"], ["CATTp"])
                    ei = 0
                    for h in range(NH):
                        bs = BS[h % 2]; bsk = "BS%d" % (h % 2)
                        qh = QH[h % 2]; qhk = "QH%d" % (h % 2)
                        dma("pool", lambda q, h=h, bs=bs: q.dma_start(out=bs[:].rearrange("p o k -> p (o k)"),
                                                                      in_=bias_in[h * 128:(h + 1) * 128, :]), writes=[bsk])
                        op("dve", lambda h=h, qh=qh: nc.vector.tensor_scalar(out=qh[:], in0=QT[:, h // 4, :], scalar1=hm[:, h % 4:h % 4 + 1],
                                                                            scalar2=None, op0=ALU.mult), ["QT", "cst"], [qhk])
                        po = PO[h % 2]; pok = "po%d" % (h % 2)
                        cb = CB[h % 2]; cbk = "CB%d" % (h % 2)
                        for p in range(8):
                            offs = pair_offsets(p)
                            n_ = len(offs); mi0 = mask_index(p, 0); o0 = offs[0] + 3
                            op("pool", lambda cb=cb, bs=bs, n_=n_, mi0=mi0, o0=o0: nc.gpsimd.tensor_tensor(
                                out=cb[:, mi0:mi0 + n_, :], in0=MK[:, mi0:mi0 + n_, :], in1=bs[:, o0:o0 + n_, :], op=ALU.add),
                               ["MK", bsk], [cbk])

                        def pv_stage(p, pe_, pek, offs):
                            for i, o in enumerate(offs):
                                kt = 2 + p + o
                                op("pe", lambda i=i, kt=kt: nc.tensor.matmul(
                                    po[:, p * 64:p * 64 + 33], lhsT=pe_[:, i * 128:(i + 1) * 128], rhs=V[:, kt, h, 0:33],
                                    start=(i == 0), stop=(i == len(offs) - 1)), [pek, "V"], [pok], signal=(i == len(offs) - 1))

                        pending = None
                        for p in range(8):
                            offs = pair_offsets(p)
                            ps = PS[(h * 8 + p) % 2]; psk = "psS%d" % ((h * 8 + p) % 2)
                            for i, o in enumerate(offs):
                                kt0 = (2 + p + o) * 128
                                sl = ps[:, i * 128:(i + 1) * 128]
                                op("pe", lambda sl=sl, kt0=kt0: nc.tensor.matmul(
                                    sl, lhsT=KT[:, h // 4, kt0:kt0 + 128], rhs=qh[:, p * 128:(p + 1) * 128], start=True, stop=False),
                                   ["KT", qhk], [psk], signal=False)
                                mi_ = mask_index(p, i)
                                op("pe", lambda sl=sl, mi_=mi_: nc.tensor.matmul(
                                    sl, lhsT=cb[:, mi_, :], rhs=ident_b[:], start=False, stop=True), [cbk, "ident_b"], [psk],
                                   signal=(i == len(offs) - 1))
                            pe_ = PE_[ei % 3]; pek = "PE%d" % (ei % 3); ei += 1
                            n = len(offs) * 128
                            op("act", lambda: nc.scalar.activation(out=pe_[:, 0:512], in_=ps[:, 0:512], func=AF.Exp), [psk], [pek])
                            op("act", lambda: nc.scalar.activation(out=pe_[:, 512:n], in_=ps[:, 512:n], func=AF.Exp), [psk], [pek])
                            if pending is not None:
                                pv_stage(*pending)
                            pending = (p, pe_, pek, offs)
                        pv_stage(*pending)
                        pov = po[:].rearrange("p (a s) -> p a s", s=64)
                        op("dve", lambda pov=pov: nc.vector.reciprocal(out=rden[:].unsqueeze(2), in_=pov[:, :, 32:33]), [pok], ["rden"])
                        op("dve", lambda pov=pov, h=h: nc.vector.tensor_tensor(
                            out=ATM[:, :, h * 32:(h + 1) * 32], in0=pov[:, :, 0:32], in1=rden[:].unsqueeze(2).to_broadcast([128, 8, 32]),
                            op=ALU.mult), [pok, "rden"], ["ATM"])
                    for p in range(8):
                        for cg in range(2):
                            pt = PT[cg % 2]; pk = ptkey[cg % 2]
                            for ci in range(4):
                                c = cg * 4 + ci
                                op("pe", lambda c=c, ci=ci, pt=pt, p=p: nc.tensor.transpose(
                                    out=pt[:, ci * 128:(ci + 1) * 128], in_=ATM[:, p, c * 128:(c + 1) * 128], identity=ident_b[:]),
                                   ["ATM", "ident_b"], [pk], signal=(ci == 3))
                            op("dve", lambda cg=cg, pt=pt, p=p: nc.vector.tensor_copy(
                                out=CATT[:, cg * 4:(cg + 1) * 4, p * 128:(p + 1) * 128], in_=pt[:].rearrange("p (c t) -> p c t", c=4)),
                               [pk], ["CATTa"])
                    T.wait_all("sp", ["QT", "KT", "V", "MK", "BS0", "BS1", "QH0", "QH1", "PE0", "PE1", "PE2", "ATM", "rden"])

                with contextlib.ExitStack() as c_:
                    WO = sb(c_, "WO", [128, KC, D], BF16)
                    LB = sb(c_, "LB", [128, 4, D], F32)
                    WR = sb(c_, "WR", [128, KC, E], F32)
                    BR = sb(c_, "BR", [128, E], F32)
                    xt = [sb(c_, "cxt%d" % i, [128, D], F32) for i in range(1)]
                    HP = sb(c_, "HP", [128, D], F32)
                    H = [sb(c_, "H%d" % i, [128, D], F32) for i in range(2)]
                    HB = [sb(c_, "HB%d" % i, [128, D], BF16) for i in range(1)]
                    HT = sb(c_, "HT", [128, KC, 128], F32)
                    st6 = sb(c_, "cst6", [128, 24], F32)
                    mv = sb(c_, "cmv", [128, 2], F32)
                    rstd = sb(c_, "crstd", [128, 1], F32)
                    L = sb(c_, "L", [128, E], F32)
                    LW = sb(c_, "LW", [128, E], F32)
                    OH = sb(c_, "OH", [128, 4, E], F32)
                    SELf = sb(c_, "SELf", [128, E], F32)
                    SELb = sb(c_, "SELb", [128, E], BF16)
                    CUMb = sb(c_, "CUMb", [128, E], BF16)
                    MX = sb(c_, "MX", [128, 4], F32)
                    EX = sb(c_, "EX", [128, 4], F32)
                    nm0 = sb(c_, "nm0", [128, 1], F32)
                    ssum = sb(c_, "ssum", [128, 1], F32)
                    SLOT = sb(c_, "SLOT", [128, E], F32)
                    OKf = sb(c_, "OKf", [128, E], F32)
                    TMP = sb(c_, "TMP", [128, E], F32)
                    slf = sb(c_, "slf", [128, 4], F32)
                    w_out_v = w_out.rearrange("(k p) n -> p k n", p=128)
                    for j in range(4):
                        dma("pool", lambda q, j=j: q.dma_start(out=WO[:, :, j * 512:(j + 1) * 512], in_=w_out_v[:, :, j * 512:(j + 1) * 512]),
                            writes=["WO%d" % j])
                    for j in range(4):
                        dma("sp", lambda q, j=j: q.dma_start(out=LB[:, j, :], in_=lnv[j:j + 1, :].partition_broadcast(128)), writes=["LB%d" % j if j != 2 else "LB2_"])
                    dma("sp", lambda q: q.dma_start(out=WR[:], in_=w_router.rearrange("(k p) e -> p k e", p=128)), writes=["WR"])
                    dma("sp", lambda q: q.dma_start(out=BR[:], in_=b_router[0:1, :].partition_broadcast(128)), writes=["BR"])
                    for tt in range(8):
                        tg = u * 8 + tt
                        xb = xt[0]; xk = "cxt0"
                        Hc = H[tt % 2]; hk = "H%d" % (tt % 2)
                        HBc = HB[0]; hbk = "HB0"
                        dma("sp", lambda q, tt=tt, xb=xb: q.dma_start(out=xb[:], in_=xu[256 + tt * 128:256 + (tt + 1) * 128, :]), writes=[xk])
                        for j in range(4):
                            for c in range(KC):
                                op("pe", lambda j=j, c=c, tt=tt: nc.tensor.matmul(
                                    bank(j, 512), lhsT=CATT[:, c, tt * 128:(tt + 1) * 128], rhs=WO[:, c, j * 512:(j + 1) * 512],
                                    start=(c == 0), stop=(c == KC - 1)), ["CATTa", "CATTp", "WO%d" % j], [bkey(j)], signal=(c == KC - 1))
                        op("dve", lambda xb=xb, tt=tt: nc.vector.tensor_scalar(out=xb[:], in0=xb[:], scalar1=stats[:, tt, 0:1], scalar2=stats[:, tt, 1:2],
                                                                              op0=ALU.subtract, op1=ALU.mult), [xk, "stats"], [xk])
                        op("dve", lambda xb=xb: nc.vector.tensor_tensor(out=xb[:], in0=xb[:], in1=LB[:, 0, :], op=ALU.mult), [xk, "LB0"], [xk])
                        op("dve", lambda xb=xb: nc.vector.tensor_tensor(out=xb[:], in0=xb[:], in1=LB[:, 1, :], op=ALU.add), [xk, "LB1"], [xk])
                        for j in range(4):
                            op("dve", lambda j=j, xb=xb: nc.vector.scalar_tensor_tensor(
                                out=HP[:, j * 512:(j + 1) * 512], in0=xb[:, j * 512:(j + 1) * 512], scalar=ALPHA, in1=bank(j, 512),
                                op0=ALU.mult, op1=ALU.add), [xk, bkey(j)], ["HP"])
                        ln_stats(HP, "HP", st6, mv[:], rstd[:], "c")
                        op("dve", lambda: nc.vector.tensor_scalar(out=HP[:], in0=HP[:], scalar1=mv[:, 0:1], scalar2=rstd[:],
                                                                  op0=ALU.subtract, op1=ALU.mult), ["HP", "cmv", "crstd"], ["HP"])
                        op("dve", lambda: nc.vector.tensor_tensor(out=HP[:], in0=HP[:], in1=LB[:, 2, :], op=ALU.mult), ["HP", "LB2_"], ["HP"])
                        op("dve", lambda Hc=Hc: nc.vector.tensor_tensor(out=Hc[:], in0=HP[:], in1=LB[:, 3, :], op=ALU.add), ["HP", "LB3"], [hk])
                        dma("sp", lambda q, tg=tg, Hc=Hc: q.dma_start(out=h_d[tg * 128:(tg + 1) * 128, :], in_=Hc[:]), [hk], ["h_d"])
                        op("act", lambda Hc=Hc, HBc=HBc: nc.scalar.activation(out=HBc[:], in_=Hc[:], func=AF.Copy), [hk], [hbk])
                        for cg in range(4):
                            bi = 4 + (cg % 2)
                            for ci in range(4):
                                c = cg * 4 + ci
                                op("pe", lambda c=c, ci=ci, bi=bi, Hc=Hc: nc.tensor.transpose(
                                    out=bank(bi, 512)[:, ci * 128:(ci + 1) * 128], in_=Hc[:, c * 128:(c + 1) * 128], identity=ident_f),
                                   [hk, "cst"], [bkey(bi)], signal=(ci == 3))
                            op("act", lambda cg=cg, bi=bi: nc.scalar.activation(
                                out=HT[:, cg * 4:(cg + 1) * 4, :], in_=bank(bi, 512).rearrange("p (c t) -> p c t", c=4), func=AF.Copy),
                               [bkey(bi)], ["HT"])
                        for c in range(KC):
                            op("pe", lambda c=c: nc.tensor.matmul(bank(0, E), lhsT=HT[:, c, :], rhs=WR[:, c, :], start=(c == 0), stop=(c == KC - 1)),
                               ["HT", "WR"], [bkey(0)], signal=(c == KC - 1))
                        op("dve", lambda: nc.vector.tensor_tensor(out=L[:], in0=bank(0, E), in1=BR[:], op=ALU.add), [bkey(0), "BR"], ["L"])
                        op("dve", lambda: nc.vector.tensor_copy(out=LW[:], in_=L[:]), ["L"], ["LW"])
                        for k in range(4):
                            op("dve", lambda k=k: nc.vector.reduce_max(out=MX[:, k:k + 1], in_=LW[:], axis=AX.X), ["LW"], ["MX"])
                            op("dve", lambda k=k: nc.vector.tensor_scalar(out=OH[:, k, :], in0=LW[:], scalar1=MX[:, k:k + 1], scalar2=None,
                                                                         op0=ALU.is_equal), ["LW", "MX"], ["OH"])
                            op("dve", lambda k=k: nc.vector.scalar_tensor_tensor(out=LW[:], in0=OH[:, k, :], scalar=-1e30, in1=LW[:],
                                                                                op0=ALU.mult, op1=ALU.add), ["OH", "LW"], ["LW"])
                        op("dve", lambda: nc.vector.tensor_tensor(out=SELf[:], in0=OH[:, 0, :], in1=OH[:, 1, :], op=ALU.add), ["OH"], ["SELf"])
                        op("dve", lambda: nc.vector.tensor_tensor(out=SELf[:], in0=SELf[:], in1=OH[:, 2, :], op=ALU.add), ["OH", "SELf"], ["SELf"])
                        op("dve", lambda: nc.vector.tensor_tensor(out=SELf[:], in0=SELf[:], in1=OH[:, 3, :], op=ALU.add), ["OH", "SELf"], ["SELf"])
                        op("dve", lambda: nc.vector.tensor_copy(out=SELb[:], in_=SELf[:]), ["SELf"], ["SELb"])
                        op("dve", lambda: nc.vector.tensor_copy(out=CUMb[:], in_=cum[:]), ["cum"], ["CUMb"])
                        op("dve", lambda: nc.vector.tensor_scalar(out=nm0[:], in0=MX[:, 0:1], scalar1=-1.0, scalar2=None, op0=ALU.mult), ["MX"], ["nm0"])
                        op("act", lambda: nc.scalar.activation(out=EX[:], in_=MX[:], func=AF.Exp, bias=nm0[:], scale=1.0), ["MX", "nm0"], ["EX"])
                        op("dve", lambda: nc.vector.reduce_sum(out=ssum[:], in_=EX[:], axis=AX.X), ["EX"], ["ssum"])
                        op("dve", lambda: nc.vector.reciprocal(out=ssum[:], in_=ssum[:]), ["ssum"], ["ssum"])
                        op("dve", lambda tg=tg: nc.vector.tensor_scalar(out=GT[:, tg, :], in0=EX[:], scalar1=ssum[:], scalar2=None, op0=ALU.mult),
                           ["EX", "ssum"], ["GT"])
                        op("pe", lambda: nc.tensor.matmul(bank(1, E), lhsT=ustr_b[:], rhs=SELb[:], start=True, stop=False),
                           ["ustr_b", "SELb"], [bkey(1)], signal=False)
                        op("pe", lambda: nc.tensor.matmul(bank(1, E), lhsT=ones_b[:], rhs=CUMb[:], start=False, stop=True),
                           ["ones_b", "CUMb"], [bkey(1)], signal=True)
                        op("dve", lambda: nc.vector.tensor_scalar(out=OKf[:], in0=bank(1, E), scalar1=float(C), scalar2=None, op0=ALU.is_lt),
                           [bkey(1)], ["OKf"])
                        op("dve", lambda: nc.vector.tensor_tensor(out=SLOT[:], in0=bank(1, E), in1=ebase, op=ALU.add), [bkey(1), "cst"], ["SLOT"])
                        op("dve", lambda: nc.vector.tensor_scalar(out=SLOT[:], in0=SLOT[:], scalar1=-float(DUMP), scalar2=None, op0=ALU.add),
                           ["SLOT"], ["SLOT"])
                        op("dve", lambda: nc.vector.tensor_tensor(out=SLOT[:], in0=SLOT[:], in1=OKf[:], op=ALU.mult), ["SLOT", "OKf"], ["SLOT"])
                        op("dve", lambda: nc.vector.tensor_scalar(out=SLOT[:], in0=SLOT[:], scalar1=float(DUMP), scalar2=None, op0=ALU.add),
                           ["SLOT"], ["SLOT"])
                        op("dve", lambda: nc.vector.tensor_tensor(out=cum[:], in0=cum[:], in1=SELf[:], op=ALU.add), ["cum", "SELf", "CUMb"], ["cum"])
                        for k in range(4):
                            op("dve", lambda k=k: nc.vector.tensor_tensor(out=TMP[:], in0=OH[:, k, :], in1=SLOT[:], op=ALU.mult), ["OH", "SLOT"], ["TMP"])
                            op("dve", lambda k=k: nc.vector.reduce_sum(out=slf[:, k:k + 1], in_=TMP[:], axis=AX.X), ["TMP"], ["slf"])
                        op("dve", lambda tg=tg: nc.vector.tensor_copy(out=SL[:, tg, :], in_=slf[:]), ["slf"], ["SL"])
                        for k in range(4):
                            dma("pool", lambda q, k=k, tg=tg, HBc=HBc: q.indirect_dma_start(
                                out=xg_d[:, :], out_offset=bass.IndirectOffsetOnAxis(ap=SL[:, tg, k:k + 1], axis=0),
                                in_=HBc[:], in_offset=None), [hbk, "SL"], ["xg_d"])
                    T.wait_all("sp", ["WO", "LB", "WR", "BR", "cxt0", "cxt1", "HP", "H0", "H1", "HB0", "HB1", "HT", "CATTa", "CATTp",
                                      "L", "LW", "OH", "SELf", "SELb", "CUMb", "MX", "EX", "nm0", "ssum", "SLOT", "OKf", "TMP", "slf", "stats"])

        with contextlib.ExitStack() as m_:
            XG = [sb(m_, "XG%d" % i, [128, D], BF16) for i in range(2)]
            XGT = sb(m_, "XGT", [128, KC, C], BF16)
            ACTT = sb(m_, "ACTT", [128, FC, C], BF16)
            wr = [sb(m_, "mw%d" % i, [128, KC, 512], BF16) for i in range(3)]
            BG = sb(m_, "BG", [128, E * 2 * FC], F32)
            BG1 = sb(m_, "BG1", [128, E * 2 * FC], F32)
            bdf = [sb(m_, "bdf%d" % i, [1, D], F32) for i in range(2)]
            bdb = [sb(m_, "bdb%d" % i, [1, D], BF16) for i in range(2)]
            GL = [sb(m_, "GL%d" % i, [128, 512], F32) for i in range(4)]
            SG = [sb(m_, "SG%d" % i, [128, 512], F32) for i in range(4)]
            TU = [sb(m_, "TU%d" % i, [128, 512], F32) for i in range(4)]
            YS = [sb(m_, "YS%d" % i, [128, 512], F32) for i in range(4)]
            dma("sp", lambda q: q.dma_start(out=BG[:], in_=bgcol), writes=["BG"])
            op("dve", lambda: nc.vector.tensor_scalar(out=BG1[:], in0=BG[:], scalar1=1.0, scalar2=None, op0=ALU.add), ["BG"], ["BG1"])
            MB = 2 if FC >= 2 else 1
            wslot = 0
            epi = 0
            ysi = 0
            for e in range(E):
                w_gu_v = w_gu[e * D:(e + 1) * D, :].rearrange("(k p) (g n) -> p k g n", p=128, g=2)
                w_dn_v = w_dn[e * DFF:(e + 1) * DFF, :].rearrange("(k p) n -> p k n", p=128)
                dma("sp", lambda q, e=e: q.dma_start(out=bdf[e % 2][:], in_=b_dn[e:e + 1, :]), writes=["bdf%d" % (e % 2)])
                op("dve", lambda e=e: nc.vector.tensor_copy(out=bdb[e % 2][:], in_=bdf[e % 2][:]), ["bdf%d" % (e % 2)], ["bdb%d" % (e % 2)])
                for st in range(CT):
                    xg = XG[st % 2]; xgk = "XG%d" % (st % 2)
                    r0 = e * C + st * 128
                    dma("sp", lambda q, r0=r0, xg=xg: q.dma_start(out=xg[:], in_=xg_d[r0:r0 + 128, :]), ["xg_d"], [xgk])
                    for cg in range(4):
                        pt = PT[cg % 2]; pk = ptkey[cg % 2]
                        for ci in range(4):
                            c = cg * 4 + ci
                            op("pe", lambda c=c, ci=ci, pt=pt, xg=xg: nc.tensor.transpose(
                                out=pt[:, ci * 128:(ci + 1) * 128], in_=xg[:, c * 128:(c + 1) * 128], identity=ident_b[:]),
                               [xgk, "ident_b"], [pk], signal=(ci == 3))
                        eng = "dve" if cg % 2 == 0 else "act"
                        if eng == "dve":
                            op("dve", lambda cg=cg, pt=pt, st=st: nc.vector.tensor_copy(
                                out=XGT[:, cg * 4:(cg + 1) * 4, st * 128:(st + 1) * 128], in_=pt[:].rearrange("p (c t) -> p c t", c=4)),
                               [pk], ["XGT"])
                        else:
                            op("act", lambda cg=cg, pt=pt, st=st: nc.scalar.activation(
                                out=XGT[:, cg * 4:(cg + 1) * 4, st * 128:(st + 1) * 128], in_=pt[:].rearrange("p (c t) -> p c t", c=4),
                                func=AF.Copy), [pk], ["XGT"])
                for pj in range(FC // MB):
                    wb = wr[wslot % 3]; wk = "mw%d" % (wslot % 3); wslot += 1
                    for g in range(2):
                        dma("pool", lambda q, pj=pj, g=g, wb=wb: q.dma_start(
                            out=wb[:, :, g * 128 * MB:(g + 1) * 128 * MB], in_=w_gu_v[:, :, g, pj * 128 * MB:(pj + 1) * 128 * MB]), writes=[wk + "g%d" % g])
                    for mi in range(MB):
                        m = pj * MB + mi
                        bgc = (e * 2 + 0) * FC + m
                        buc = (e * 2 + 1) * FC + m
                        for (n0, nn) in cfg.NS:
                            bg_, bu_ = (0, 1) if epi % 2 == 0 else (2, 3)
                            gl = GL[epi % 4]; sg = SG[epi % 4]; tu = TU[epi % 4]
                            glk, sgk, tuk = "GL%d" % (epi % 4), "SG%d" % (epi % 4), "TU%d" % (epi % 4)
                            epi += 1
                            for g, bb in ((0, bg_), (1, bu_)):
                                for k in range(KC):
                                    op("pe", lambda k=k, g=g, bb=bb, mi=mi, n0=n0, nn=nn, wb=wb: nc.tensor.matmul(
                                        bank(bb, nn), lhsT=wb[:, k, g * 128 * MB + mi * 128:g * 128 * MB + (mi + 1) * 128],
                                        rhs=XGT[:, k, n0:n0 + nn], start=(k == 0), stop=(k == KC - 1)),
                                       [wk + "g%d" % g, "XGT"], [bkey(bb)], signal=(k == KC - 1))
                            op("dve", lambda gl=gl, bg_=bg_, nn=nn, bgc=bgc: nc.vector.tensor_scalar(
                                out=gl[:, 0:nn], in0=bank(bg_, nn), scalar1=BG[:, bgc:bgc + 1], scalar2=7.0, op0=ALU.add, op1=ALU.min),
                               [bkey(bg_), "BG"], [glk])
                            op("act", lambda gl=gl, sg=sg, nn=nn: nc.scalar.activation(out=sg[:, 0:nn], in_=gl[:, 0:nn], func=AF.Sigmoid, scale=1.702),
                               [glk], [sgk])
                            op("dve", lambda tu=tu, bu_=bu_, nn=nn, buc=buc: nc.vector.tensor_scalar(
                                out=tu[:, 0:nn], in0=bank(bu_, nn), scalar1=BG1[:, buc:buc + 1], scalar2=8.0, op0=ALU.add, op1=ALU.min),
                               [bkey(bu_), "BG1"], [tuk])
                            op("dve", lambda tu=tu, gl=gl, nn=nn: nc.vector.scalar_tensor_tensor(
                                out=tu[:, 0:nn], in0=tu[:, 0:nn], scalar=-6.0, in1=gl[:, 0:nn], op0=ALU.max, op1=ALU.mult),
                               [tuk, glk], [tuk])
                            op("dve", lambda tu=tu, sg=sg, nn=nn, m=m, n0=n0: nc.vector.tensor_tensor(
                                out=ACTT[:, m, n0:n0 + nn], in0=tu[:, 0:nn], in1=sg[:, 0:nn], op=ALU.mult), [tuk, sgk], ["ACTT"])
                for j in range(4):
                    wb = wr[wslot % 3]; wk = "mw%d" % (wslot % 3); wslot += 1
                    dma("pool", lambda q, j=j, wb=wb: q.dma_start(out=wb[:, 0:FC, :], in_=w_dn_v[:, :, j * 512:(j + 1) * 512]), writes=[wk + "g0", wk + "g1"])
                    for st in range(CT):
                        bi = 4 + (ysi % 2)
                        ys = YS[ysi % 4]; ysk = "YS%d" % (ysi % 4); ysi += 1
                        for m in range(FC):
                            op("pe", lambda m=m, st=st, bi=bi, wb=wb: nc.tensor.matmul(
                                bank(bi, 512), lhsT=ACTT[:, m, st * 128:(st + 1) * 128], rhs=wb[:, m, :], start=(m == 0), stop=False),
                               ["ACTT", wk + "g0", wk + "g1"], [bkey(bi)], signal=False)
                        op("pe", lambda bi=bi, j=j, e=e: nc.tensor.matmul(
                            bank(bi, 512), lhsT=ones_b[0:1, :], rhs=bdb[e % 2][0:1, j * 512:(j + 1) * 512], start=False, stop=True),
                           ["ones_b", "bdb%d" % (e % 2)], [bkey(bi)], signal=True)
                        if ysi % 2 == 0:
                            op("act", lambda ys=ys, bi=bi: nc.scalar.activation(out=ys[:], in_=bank(bi, 512), func=AF.Copy), [bkey(bi)], [ysk])
                        else:
                            op("dve", lambda ys=ys, bi=bi: nc.vector.tensor_copy(out=ys[:], in_=bank(bi, 512)), [bkey(bi)], [ysk])
                        r0 = e * C + st * 128
                        dma("sp", lambda q, r0=r0, j=j, ys=ys: q.dma_start(out=yy_d[r0:r0 + 128, j * 512:(j + 1) * 512], in_=ys[:]), [ysk], ["yy_d"])
            T.wait_all("sp", ["XG0", "XG1", "XGT", "ACTT", "mw0", "mw1", "mw2", "BG", "BG1", "bdf0", "bdf1", "bdb0", "bdb1",
                              "GL0", "GL1", "SG0", "SG1", "TU0", "TU1", "YS0", "YS1", "YS2", "YS3"])

        with contextlib.ExitStack() as f_:
            YG = [[sb(f_, "YG%d_%d" % (i, k), [128, D], F32) for k in range(4)] for i in range(2)]
            HH = [sb(f_, "HH%d" % i, [128, D], F32) for i in range(2)]
            OU = [sb(f_, "OU%d" % i, [128, D], F32) for i in range(2)]
            LB2 = sb(f_, "LB2", [128, 2, D], F32)
            st6 = sb(f_, "fst6", [128, 24], F32)
            mv = sb(f_, "fmv", [128, 2], F32)
            rstd = sb(f_, "frstd", [128, 1], F32)
            for j in range(2):
                dma("sp", lambda q, j=j: q.dma_start(out=LB2[:, j, :], in_=lnv[4 + j:5 + j, :].partition_broadcast(128)), writes=["LF%d" % j])
            for tg in range(NT):
                i = tg % 2
                hh = HH[i]; hhk = "HH%d" % i
                ou = OU[i]; ouk = "OU%d" % i
                dma("sp", lambda q, tg=tg, hh=hh: q.dma_start(out=hh[:], in_=h_d[tg * 128:(tg + 1) * 128, :]), ["h_d"], [hhk])
                for k in range(4):
                    dma("pool", lambda q, tg=tg, k=k, i=i: q.indirect_dma_start(
                        out=YG[i][k][:], out_offset=None, in_=yy_d[:, :],
                        in_offset=bass.IndirectOffsetOnAxis(ap=SL[:, tg, k:k + 1], axis=0)),
                        ["yy_d", "yy_dump", "SL"], ["YG%d_%d" % (i, k)])
                op("act", lambda hh=hh: nc.scalar.activation(out=hh[:], in_=hh[:], func=AF.Copy, scale=ALPHA), [hhk], [hhk])
                for k in range(4):
                    op("dve", lambda tg=tg, k=k, i=i, hh=hh: nc.vector.scalar_tensor_tensor(
                        out=hh[:], in0=YG[i][k][:], scalar=GT[:, tg, k:k + 1], in1=hh[:], op0=ALU.mult, op1=ALU.add),
                       ["YG%d_%d" % (i, k), "GT", hhk], [hhk])
                ln_stats(hh, hhk, st6, mv[:], rstd[:], "f")
                op("dve", lambda hh=hh: nc.vector.tensor_scalar(out=hh[:], in0=hh[:], scalar1=mv[:, 0:1], scalar2=rstd[:],
                                                                op0=ALU.subtract, op1=ALU.mult), [hhk, "fmv", "frstd"], [hhk])
                op("dve", lambda hh=hh: nc.vector.tensor_tensor(out=hh[:], in0=hh[:], in1=LB2[:, 0, :], op=ALU.mult), [hhk, "LF0"], [hhk])
                op("dve", lambda hh=hh, ou=ou: nc.vector.tensor_tensor(out=ou[:], in0=hh[:], in1=LB2[:, 1, :], op=ALU.add), [hhk, "LF1"], [ouk])
                dma("sp", lambda q, tg=tg, ou=ou: q.dma_start(out=y_out[tg * 128:(tg + 1) * 128, :], in_=ou[:]), [ouk], ["y_out"])
            keys = ["y_out", "OU0", "OU1", "HH0", "HH1", "LB2"] + ["YG%d_%d" % (i, k) for i in range(2) for k in range(4)]
            T.wait_all("sp", keys)
            for q in ("sp", "pool", "act"):
                for name in T.dq[q][0]:
                    if T.cnt[name]:
                        nc.sync.wait_ge(T.sem[name], T.cnt[name])
    return nc


def _unit_table(seq_rows):
    units = []
    for si, R in enumerate(seq_rows):
        for b in range(R // 16):
            units.append((si, b))
    return units


def _geometry(R, b):
    mask = np.full((128, NMASK, 128), NEG, np.float32)
    c = np.arange(64)
    cs = np.clip(c - 8, 0, 48)
    colok = (c[None, :] >= cs[:, None]) & (c[None, :] < cs[:, None] + 16)
    for p in range(8):
        for i, o in enumerate(pair_offsets(p)):
            mi = mask_index(p, i)
            for qr in range(2):
                r = 16 * b + 2 * p + qr
                rs = min(max(r - 4, 0), R - 8)
                for kr in range(2):
                    ka = 16 * b + 2 * p + 2 * o + kr
                    if 0 <= ka < R and rs <= ka < rs + 8:
                        blk = np.where(colok, 0.0, NEG).astype(np.float32)
                        mask[qr * 64:(qr + 1) * 64, mi, kr * 64:(kr + 1) * 64] = blk
    L = R * 64
    t0 = 16 * b * 64
    pv = np.zeros(16, np.float32)
    for j in range(8):
        pv[j] = 1.0 if 0 <= t0 - 8 + j < L else 0.0
        pv[8 + j] = 1.0 if 0 <= t0 + 1024 + j < L else 0.0
    rc = np.zeros(64, np.float32)
    for g, w in enumerate((2, 4, 8, 16)):
        half = w // 2
        for j in range(8):
            for (off, tt) in ((0, t0 + j), (8, t0 + 1016 + j)):
                lo = min(max(tt - half, 0), L)
                hi = min(max(tt + half, 0), L)
                rc[g * 16 + off + j] = 1.0 / float(hi - lo)
    return mask, pv, rc


def _bias_table(rpb):
    H = rpb.shape[0]
    out = np.zeros((H, 128, 7, 128), np.float32)
    c = np.arange(64)
    dc = c[None, :] - c[:, None] + 15
    okc = (dc >= 0) & (dc <= 30)
    dcc = np.clip(dc, 0, 30)
    for o in range(-3, 4):
        for qr in range(2):
            for kr in range(2):
                dr = 2 * o + kr - qr + 7
                if 0 <= dr <= 14:
                    blk = np.where(okc[None], rpb[:, dr, :][:, dcc], 0.0)
                    out[:, qr * 64:(qr + 1) * 64, o + 3, kr * 64:(kr + 1) * 64] = blk
    return out.reshape(H * 128, 7 * 128)


def prepare_inputs(cfg, xs, p):
    E, DFF, FC, NU, C = cfg.E, cfg.DFF, cfg.FC, cfg.NU, cfg.C
    seq_rows = [x.shape[0] // GRID_W for x in xs]
    units = _unit_table(seq_rows)
    assert len(units) == NU * cfg.NCORES, (len(units), NU, cfg.NCORES)
    f32 = np.float32
    lnv = np.stack([p["ln_in_g"], p["ln_in_b"], p["ln1_g"], p["ln1_b"], p["ln2_g"], p["ln2_b"]]).astype(f32)
    lncol = np.concatenate([p["ln_in_g"].reshape(16, 128).T, p["ln_in_b"].reshape(16, 128).T], axis=1).astype(f32)
    pscol = np.ascontiguousarray(p["pool_scale"].reshape(8, 128).T).astype(f32)
    bgcol = np.ascontiguousarray(p["b_gate_up"].reshape(E * 2 * FC, 128).T).astype(f32)
    consts = np.zeros((128, 128 * 3 + 8 + E), f32)
    consts[:, 0:128] = np.eye(128, dtype=f32)
    consts[:, 128:256] = np.triu(np.ones((128, 128), f32), 1)
    consts[:, 256:384] = 1.0
    for j in range(4):
        consts[32 * j:32 * (j + 1), 384 + j] = 1.0
    consts[:, 392:392 + E] = (np.arange(E, dtype=f32) * C)[None, :]
    shared = {
        "bias_in": _bias_table(p["rpb"].astype(f32)), "lnv": lnv, "lncol": np.ascontiguousarray(lncol),
        "w_in": p["w_in"], "w_pool": p["w_pool"].reshape(4 * 256, 256), "pscol": pscol, "w_out": p["w_out"],
        "w_router": p["w_router"], "b_router": p["b_router"].reshape(1, E), "w_gu": p["w_gate_up"].reshape(E * D, 2 * DFF),
        "bgcol": bgcol, "w_dn": p["w_down"].reshape(E * DFF, D), "b_dn": p["b_down"].reshape(E, D), "consts": consts,
    }
    in_maps = []
    for c in range(cfg.NCORES):
        xin = np.zeros((NU * UT, D), f32)
        masks = np.zeros((NU * 128, NMASK * 128), f32)
        pvs = np.zeros((NU, 16), f32)
        rcs = np.zeros((NU, 64), f32)
        for ui in range(NU):
            si, b = units[c * NU + ui]
            R = seq_rows[si]
            t_lo = (16 * b - 4) * 64
            t_hi = t_lo + UT
            a, bnd = max(t_lo, 0), min(t_hi, R * 64)
            xin[ui * UT + (a - t_lo): ui * UT + (bnd - t_lo)] = xs[si][a:bnd]
            mk, pv, rc = _geometry(R, b)
            masks[ui * 128:(ui + 1) * 128] = mk.reshape(128, NMASK * 128)
            pvs[ui] = pv
            rcs[ui] = rc
        m = dict(shared)
        m.update({"x_in": xin, "mask_in": masks, "pv_in": pvs, "rc_in": rcs})
        in_maps.append(m)
    return in_maps, units


def assemble(cfg, results, units, seq_lens):
    outs = [np.zeros((L, D), np.float32) for L in seq_lens]
    for c in range(cfg.NCORES):
        y = results[c]["y_out"]
        for ui in range(cfg.NU):
            si, b = units[c * cfg.NU + ui]
            outs[si][b * 1024:(b + 1) * 1024] = y[ui * 1024:(ui + 1) * 1024]
    return outs


_NC_CACHE = {}


def run(cfg, xs, p):
    key = (cfg.E, cfg.DFF, cfg.NU, cfg.NCORES, cfg.C)
    if key not in _NC_CACHE:
        _NC_CACHE[key] = build(cfg)
    nc = _NC_CACHE[key]
    in_maps, units = prepare_inputs(cfg, xs, p)
    res = run_bass_kernel_spmd(nc, in_maps, core_ids=list(range(cfg.NCORES)))
    return assemble(cfg, res.results, units, [x.shape[0] for x in xs])


def kernel(x_prompt, x_sample, ln_in_g, ln_in_b, w_in, rpb, w_pool, pool_scale, w_out, ln1_g, ln1_b,
           w_router, b_router, w_gate_up, b_gate_up, w_down, b_down, ln2_g, ln2_b):
    cfg = Cfg()
    a = lambda v: np.asarray(v, dtype=np.float32)
    p = {"ln_in_g": a(ln_in_g), "ln_in_b": a(ln_in_b), "w_in": a(w_in)[0], "rpb": a(rpb)[0], "w_pool": a(w_pool)[0],
         "pool_scale": a(pool_scale)[0], "w_out": a(w_out)[0], "ln1_g": a(ln1_g)[0], "ln1_b": a(ln1_b)[0],
         "w_router": a(w_router)[0], "b_router": a(b_router)[0], "w_gate_up": a(w_gate_up)[0], "b_gate_up": a(b_gate_up)[0],
         "w_down": a(w_down)[0], "b_down": a(b_down)[0], "ln2_g": a(ln2_g)[0], "ln2_b": a(ln2_b)[0]}
    xp = a(x_prompt)
    xsm = a(x_sample)
    xs = [xp[i] for i in range(xp.shape[0])] + [xsm[i] for i in range(xsm.shape[0])]
    outs = run(cfg, xs, p)
    y_prompt = np.stack(outs[:xp.shape[0]]).astype(np.float32)
    y_sample = np.stack(outs[xp.shape[0]:]).astype(np.float32)
    return (y_prompt, y_sample)
```

```python
import contextlib
import numpy as np
import concourse.bass as bass
import concourse.mybir as mybir
from concourse.bass_utils import run_bass_kernel_spmd

F32 = mybir.dt.float32
BF16 = mybir.dt.bfloat16
I32 = mybir.dt.int32
AF = mybir.ActivationFunctionType
ALU = mybir.AluOpType
AX = mybir.AxisListType

D = 2048
KC = 16
NH = 32
GRID_W = 64
LN_EPS = 1e-5
ALPHA = float(2 ** 0.25)
NEG = -30000.0
VS = 34
UT = 1536
NMASK = 42
DBG = False


class Cfg:
    def __init__(self, E=32, DFF=2048, NU=5, NCORES=8, C=768):
        self.E, self.DFF, self.NU, self.NCORES, self.C = E, DFF, NU, NCORES, C
        self.FC = DFF // 128
        self.CT = C // 128
        self.NT = NU * 8
        self.NSLOT = E * C + 128
        self.DUMP = E * C
        ns, s = [], 0
        while s < C:
            n = min(512, C - s)
            if C % 448 == 0:
                n = min(448, C - s)
            ns.append((s, n)); s += n
        self.NS = ns


def pair_offsets(p):
    if p == 0:
        return [-2, -1, 0, 1, 2, 3]
    if p == 7:
        return [-3, -2, -1, 0, 1, 2]
    return [-2, -1, 0, 1, 2]


def mask_index(p, i):
    if p == 0:
        return i
    return 6 + (p - 1) * 5 + i


class Trk:
    def __init__(self, nc, es):
        self.nc = nc
        self.eng = {"pe": nc.tensor, "act": nc.scalar, "dve": nc.vector, "pool": nc.gpsimd, "sp": nc.sync}
        self.sem = {}
        self.cnt = {}
        for e in ("pe", "act", "dve", "pool"):
            self.sem[e] = es.enter_context(nc.semaphore("sem_" + e))
            self.cnt[e] = 0
        self.dq = {}
        for q, k in (("sp", 8), ("pool", 8), ("act", 4)):
            sems = []
            for i in range(k):
                name = "dq_%s_%d" % (q, i)
                self.sem[name] = es.enter_context(nc.semaphore(name))
                self.cnt[name] = 0
                sems.append(name)
            self.dq[q] = [sems, 0]
        self.state = {}
        self.seen = {e: {} for e in self.eng}

    def _st(self, k):
        s = self.state.get(k)
        if s is None:
            s = {"w": None, "r": {}}
            self.state[k] = s
        return s

    def _waits(self, engine, reads, writes, extra=()):
        need = {}

        def add(ev):
            if ev is None:
                return
            s, v = ev
            if engine == "pe" and s == "pe":
                return
            if need.get(s, 0) < v:
                need[s] = v

        for k in reads:
            add(self._st(k)["w"])
        for k in writes:
            st = self._st(k)
            add(st["w"])
            for s, v in st["r"].items():
                add((s, v))
        for ev in extra:
            add(ev)
        seen = self.seen[engine]
        for s, v in need.items():
            if seen.get(s, 0) >= v:
                continue
            self.eng[engine].wait_ge(self.sem[s], v)
            seen[s] = v

    def _record(self, ev, reads, writes):
        s, v = ev
        for k in reads:
            r = self._st(k)["r"]
            if r.get(s, 0) < v:
                r[s] = v
        for k in writes:
            self.state[k] = {"w": ev, "r": {}}

    def op(self, engine, fn, reads=(), writes=(), signal=True):
        self._waits(engine, reads, writes)
        ins = fn()
        if signal:
            self.cnt[engine] += 1
            ins.then_inc(self.sem[engine], 1)
            ev = (engine, self.cnt[engine])
        else:
            ev = (engine, self.cnt[engine] + 1)
        self._record(ev, reads, writes)
        return ins

    def dma(self, q, fn, reads=(), writes=()):
        sems, i = self.dq[q]
        name = sems[i % len(sems)]
        self.dq[q][1] = i + 1
        prev = (name, self.cnt[name])
        self._waits(q, reads, writes, extra=(prev,) if self.cnt[name] else ())
        ins = fn(self.eng[q])
        self.cnt[name] += 16
        ins.then_inc(self.sem[name], 16)
        ev = (name, self.cnt[name])
        self._record(ev, reads, writes)
        return ev

    def wait_all(self, engine, keys):
        self.barrier()

    def barrier(self):
        evs = [(s, v) for s, v in self.cnt.items() if v > 0]
        for e in ("pe", "act", "dve", "pool", "sp"):
            seen = self.seen[e]
            for s, v in evs:
                if s == e and e == "pe":
                    continue
                if seen.get(s, 0) >= v:
                    continue
                self.eng[e].wait_ge(self.sem[s], v)
                seen[s] = v
        self.state = {}


def build(cfg):
    nc = bass.Bass("TRN2", target_bir_lowering=False)
    E, DFF, FC, NU, C, CT, NT = cfg.E, cfg.DFF, cfg.FC, cfg.NU, cfg.C, cfg.CT, cfg.NT
    NSLOT, DUMP = cfg.NSLOT, cfg.DUMP

    def din(name, shape, dt=F32):
        return nc.dram_tensor(name, list(shape), dt, kind="ExternalInput").ap()

    def dscr(name, shape, dt):
        return nc.dram_tensor(name, list(shape), dt, kind="Internal").ap()

    x_in = din("x_in", [NU * UT, D])
    mask_in = din("mask_in", [NU * 128, NMASK * 128])
    bias_in = din("bias_in", [NH * 128, 7 * 128])
    pv_in = din("pv_in", [NU, 16])
    rc_in = din("rc_in", [NU, 64])
    lnv = din("lnv", [6, D])
    lncol = din("lncol", [128, 32])
    w_in = din("w_in", [D, 4096])
    w_pool = din("w_pool", [4 * 256, 256])
    pscol = din("pscol", [128, 8])
    w_out = din("w_out", [D, D])
    w_router = din("w_router", [D, E])
    b_router = din("b_router", [1, E])
    w_gu = din("w_gu", [E * D, 2 * DFF])
    bgcol = din("bgcol", [128, E * 2 * FC])
    w_dn = din("w_dn", [E * DFF, D])
    b_dn = din("b_dn", [E, D])
    consts = din("consts", [128, 128 * 3 + 8 + E])
    y_out = nc.dram_tensor("y_out", [NU * 1024, D], F32, kind="ExternalOutput").ap()

    qt_d = dscr("qt_d", [1024, 1024], BF16)
    kt_d = dscr("kt_d", [1024, UT], BF16)
    v_d = dscr("v_d", [UT, NH * VS], BF16)
    pm_d = dscr("pm_d", [1024, 1024], BF16)
    h_d = dscr("h_d", [NT * 128, D], F32)
    xg_d = dscr("xg_d", [NSLOT, D], BF16)
    yy_d = dscr("yy_d", [NSLOT, D], F32)

    es = contextlib.ExitStack()
    with es:
        T = Trk(nc, es)
        op, dma = T.op, T.dma

        uid = [0]

        def sb(stack, name, shape, dt):
            uid[0] += 1
            return stack.enter_context(nc.sbuf_tensor("%s_%d" % (name, uid[0]), list(shape), dt))

        PS = [es.enter_context(nc.psum_tensor("ps%d" % i, [128, 1024], F32)) for i in range(2)]
        PO = [es.enter_context(nc.psum_tensor("po%d" % i, [128, 512], F32)) for i in range(2)]
        PT = [es.enter_context(nc.psum_tensor("pt%d" % i, [128, 512], BF16)) for i in range(2)]
        banks = [(PS[0], 0), (PS[0], 512), (PS[1], 0), (PS[1], 512), (PO[0], 0), (PO[1], 0)]

        def bank(i, n=512):
            t, o = banks[i]
            return t[:, o:o + n]

        def bkey(i):
            return "bank%d" % i

        pskeys = [("bank0", "bank1"), ("bank2", "bank3")]
        ptkey = ["ptb0", "ptb1"]

        cst = sb(es, "cst", [128, 128 * 3 + 8 + E], F32)
        ident_b = sb(es, "ident_b", [128, 128], BF16)
        ustr_b = sb(es, "ustr_b", [128, 128], BF16)
        ones_b = sb(es, "ones_b", [128, 128], BF16)
        SL = sb(es, "SL", [128, NT, 4], I32)
        GT = sb(es, "GT", [128, NT, 4], F32)
        cum = sb(es, "cum", [128, E], F32)
        zer = sb(es, "zer", [128, D], F32)
        dma("sp", lambda q: q.dma_start(out=cst[:], in_=consts), writes=["cst"])
        ident_f = cst[:, 0:128]
        hm = cst[:, 384:388]
        ebase = cst[:, 392:392 + E]
        op("dve", lambda: nc.vector.tensor_copy(out=ident_b[:], in_=cst[:, 0:128]), ["cst"], ["ident_b"])
        op("dve", lambda: nc.vector.tensor_copy(out=ustr_b[:], in_=cst[:, 128:256]), ["cst"], ["ustr_b"])
        op("dve", lambda: nc.vector.tensor_copy(out=ones_b[:], in_=cst[:, 256:384]), ["cst"], ["ones_b"])
        op("dve", lambda: nc.vector.memset(cum[:], 0.0), [], ["cum"])
        op("pool", lambda: nc.gpsimd.memset(zer[:], 0.0), [], ["zer"])
        dma("sp", lambda q: q.dma_start(out=yy_d[DUMP:DUMP + 128, :], in_=zer[:]), ["zer"], ["yy_dump"])
        zb = zer[:].bitcast(BF16)
        for r0 in range(0, NSLOT, 256):
            nr = min(256, NSLOT - r0)
            dma("sp", lambda q, r0=r0, nr=nr: q.dma_start(
                out=xg_d[r0:r0 + nr, :].rearrange("(p two) d -> p (two d)", two=nr // 128), in_=zb[:, 0:D * (nr // 128)]),
                ["zer"], ["xg_zero"])

        def ln_stats(xt_ap, xkey, st6, mv, rstd, tag):
            for c in range(4):
                op("dve", lambda c=c: nc.vector.bn_stats(out=st6[:, c * 6:(c + 1) * 6], in_=xt_ap[:, c * 512:(c + 1) * 512]),
                   [xkey], [tag + "st6"])
            op("dve", lambda: nc.vector.bn_aggr(out=mv, in_=st6[:, 0:24]), [tag + "st6"], [tag + "mv"])
            op("dve", lambda: nc.vector.tensor_scalar(out=rstd, in0=mv[:, 1:2], scalar1=LN_EPS, scalar2=None, op0=ALU.add),
               [tag + "mv"], [tag + "rstd"])
            op("act", lambda: nc.scalar.activation(out=rstd, in_=rstd, func=AF.Sqrt), [tag + "rstd"], [tag + "rstd"])
            op("dve", lambda: nc.vector.reciprocal(out=rstd, in_=rstd), [tag + "rstd"], [tag + "rstd"])

        for u in range(NU):
            xu = x_in[u * UT:(u + 1) * UT, :]
            with contextlib.ExitStack() as us:
                stats = sb(us, "stats", [128, 8, 2], F32)
                with contextlib.ExitStack() as a:
                    XT = sb(a, "XT", [128, KC, UT], BF16)
                    xt = [sb(a, "xt%d" % i, [128, D], F32) for i in range(2)]
                    xh = sb(a, "xh", [128, D], BF16)
                    st6 = sb(a, "st6", [128, 24], F32)
                    mv = sb(a, "mv", [128, 2], F32)
                    rstd = sb(a, "rstd", [128, 1], F32)
                    lnc = sb(a, "lnc", [128, 32], F32)
                    wr = [sb(a, "wr%d" % i, [128, KC, 256], BF16) for i in range(3)]
                    QTs = sb(a, "QTs", [128, 1024], BF16)
                    KTs = sb(a, "KTs", [128, UT], BF16)
                    Vb = sb(a, "Vb", [128, 12, NH, VS], BF16)
                    UF = sb(a, "UF", [128, UT], F32)
                    PA = sb(a, "PA", [128, UT], F32)
                    PB = sb(a, "PB", [128, UT], F32)
                    PTg = sb(a, "PTg", [128, 2, 1024], BF16)
                    PMs = sb(a, "PMs", [128, 1024], BF16)
                    wp = sb(a, "wp", [128, 4, 2, 256], BF16)
                    psc = sb(a, "psc", [128, 8], F32)
                    pvb = sb(a, "pvb", [128, 16], F32)
                    rcb = sb(a, "rcb", [128, 64], F32)
                    tmp8 = sb(a, "tmp8", [128, 8], F32)

                    dma("sp", lambda q: q.dma_start(out=lnc[:], in_=lncol), writes=["lnc"])
                    dma("sp", lambda q: q.dma_start(out=psc[:], in_=pscol), writes=["psc"])
                    dma("sp", lambda q: q.dma_start(out=pvb[:], in_=pv_in[u:u + 1, :].partition_broadcast(128)), writes=["pvb"])
                    dma("sp", lambda q: q.dma_start(out=rcb[:], in_=rc_in[u:u + 1, :].partition_broadcast(128)), writes=["rcb"])
                    dma("pool", lambda q: q.dma_start(out=wp[:], in_=w_pool.rearrange("(g k p) n -> p g k n", g=4, k=2, p=128)),
                        writes=["wp"])
                    op("dve", lambda: nc.vector.memset(Vb[:, :, :, 32:34], 1.0), [], ["Vb"])

                    for t in range(12):
                        xb = xt[t % 2]
                        xk = "xt%d" % (t % 2)
                        dma("sp", lambda q, t=t, xb=xb: q.dma_start(out=xb[:], in_=xu[t * 128:(t + 1) * 128, :]), writes=[xk])
                        ln_stats(xb, xk, st6, mv[:], rstd[:], "a")
                        if 2 <= t < 10:
                            op("dve", lambda t=t: nc.vector.tensor_copy(out=stats[:, t - 2, 0:1], in_=mv[:, 0:1]), ["amv"], ["stats"])
                            op("dve", lambda t=t: nc.vector.tensor_copy(out=stats[:, t - 2, 1:2], in_=rstd[:]), ["arstd"], ["stats"])
                        op("dve", lambda xb=xb: nc.vector.tensor_scalar(out=xh[:], in0=xb[:], scalar1=mv[:, 0:1], scalar2=rstd[:],
                                                                       op0=ALU.subtract, op1=ALU.mult),
                           [xk, "amv", "arstd"], ["xh"])
                        for cg in range(4):
                            pt = PT[cg % 2]
                            pk = ptkey[cg % 2]
                            for ci in range(4):
                                c = cg * 4 + ci
                                op("pe", lambda c=c, ci=ci, pt=pt: nc.tensor.transpose(out=pt[:, ci * 128:(ci + 1) * 128],
                                                                                      in_=xh[:, c * 128:(c + 1) * 128], identity=ident_b[:]),
                                   ["xh", "ident_b"], [pk], signal=(ci == 3))
                            for ci in range(4):
                                c = cg * 4 + ci
                                op("act", lambda c=c, ci=ci, pt=pt, t=t: nc.scalar.activation(
                                    out=XT[:, c, t * 128:(t + 1) * 128], in_=pt[:, ci * 128:(ci + 1) * 128], func=AF.Identity,
                                    bias=lnc[:, 16 + c:17 + c], scale=lnc[:, c:c + 1]), [pk, "lnc"], ["XT"])

                    w_in_v = w_in.rearrange("(k p) n -> p k n", p=128)
                    for pc in range(16):
                        wb = wr[pc % 3]
                        wk = "wr%d" % (pc % 3)
                        dma("pool", lambda q, pc=pc, wb=wb: q.dma_start(out=wb[:], in_=w_in_v[:, :, pc * 256:(pc + 1) * 256]), writes=[wk])
                        kind = pc // 4
                        if kind in (0, 1):
                            for mi in range(2):
                                mc = (pc % 4) * 2 + mi
                                ntl = [(256, 512), (768, 512)] if kind == 0 else [(0, 512), (512, 512), (1024, 512)]
                                dst = QTs if kind == 0 else KTs
                                dk = "QTs" if kind == 0 else "KTs"
                                for j, (n0, nn) in enumerate(ntl):
                                    bi = j % 2
                                    for k in range(KC):
                                        op("pe", lambda k=k, mi=mi, n0=n0, nn=nn, bi=bi, wb=wb: nc.tensor.matmul(
                                            bank(bi, nn), lhsT=wb[:, k, mi * 128:(mi + 1) * 128], rhs=XT[:, k, n0:n0 + nn],
                                            start=(k == 0), stop=(k == KC - 1)), [wk, "XT"], [bkey(bi)], signal=(k == KC - 1))
                                    d0 = n0 - 256 if kind == 0 else n0
                                    op("act", lambda d0=d0, nn=nn, bi=bi, dst=dst, kind=kind: nc.scalar.activation(
                                        out=dst[:, d0:d0 + nn], in_=bank(bi, nn), func=AF.Copy,
                                        scale=(32.0 ** -0.5 if kind == 0 else 1.0)), [bkey(bi)], [dk])
                                dd = qt_d if kind == 0 else kt_d
                                dma("sp", lambda q, mc=mc, dst=dst, dd=dd: q.dma_start(out=dd[mc * 128:(mc + 1) * 128, :], in_=dst[:]),
                                    [dk], ["qk_d"])
                        elif kind == 2:
                            h0 = (pc % 4) * 8
                            for t in range(12):
                                bi = t % 2
                                for k in range(KC):
                                    op("pe", lambda k=k, t=t, bi=bi, wb=wb: nc.tensor.matmul(
                                        bank(bi, 256), lhsT=XT[:, k, t * 128:(t + 1) * 128], rhs=wb[:, k, :],
                                        start=(k == 0), stop=(k == KC - 1)), [wk, "XT"], [bkey(bi)], signal=(k == KC - 1))
                                op("dve", lambda t=t, bi=bi, h0=h0: nc.vector.tensor_copy(
                                    out=Vb[:, t, h0:h0 + 8, 0:32], in_=bank(bi, 256).rearrange("p (h d) -> p h d", h=8)),
                                   [bkey(bi)], ["Vb"])
                            if pc % 4 == 3:
                                dma("sp", lambda q: q.dma_start(out=v_d.rearrange("(t p) f -> p t f", p=128),
                                                                in_=Vb[:].rearrange("p t h s -> p t (h s)")), ["Vb"], ["v_d"])
                        else:
                            g = pc % 4
                            for mi in range(2):
                                for j, (n0, nn) in enumerate([(128, 512), (640, 512), (1152, 256)]):
                                    bi = j % 2
                                    for k in range(KC):
                                        op("pe", lambda k=k, mi=mi, n0=n0, nn=nn, bi=bi, wb=wb: nc.tensor.matmul(
                                            bank(bi, nn), lhsT=wb[:, k, mi * 128:(mi + 1) * 128], rhs=XT[:, k, n0:n0 + nn],
                                            start=(k == 0), stop=(k == KC - 1)), [wk, "XT"], [bkey(bi)], signal=(k == KC - 1))
                                    op("act", lambda n0=n0, nn=nn, bi=bi: nc.scalar.activation(
                                        out=UF[:, n0:n0 + nn], in_=bank(bi, nn), func=AF.Copy), [bkey(bi)], ["UF"])
                                op("dve", lambda: nc.vector.tensor_tensor(out=UF[:, 248:256], in0=UF[:, 248:256], in1=pvb[:, 0:8], op=ALU.mult),
                                   ["UF", "pvb"], ["UF"])
                                op("dve", lambda: nc.vector.tensor_tensor(out=UF[:, 1280:1288], in0=UF[:, 1280:1288], in1=pvb[:, 8:16], op=ALU.mult),
                                   ["UF", "pvb"], ["UF"])
                                lo, hi = 192, 1344
                                op("dve", lambda: nc.vector.tensor_tensor(out=PA[:, lo:hi], in0=UF[:, lo - 1:hi - 1], in1=UF[:, lo:hi], op=ALU.add),
                                   ["UF"], ["PA"])
                                cur, oth, ck, ok_ = PA, PB, "PA", "PB"
                                sh = 1
                                for lvl in range(g):
                                    lo += sh; hi -= sh
                                    op("dve", lambda cur=cur, oth=oth, lo=lo, hi=hi, sh=sh: nc.vector.tensor_tensor(
                                        out=oth[:, lo:hi], in0=cur[:, lo - sh:hi - sh], in1=cur[:, lo + sh:hi + sh], op=ALU.add), [ck], [ok_])
                                    cur, oth, ck, ok_ = oth, cur, ok_, ck
                                    sh *= 2
                                wsz = 2 << g
                                op("dve", lambda cur=cur, mi=mi, wsz=wsz: nc.vector.scalar_tensor_tensor(
                                    out=PTg[:, mi, :], in0=cur[:, 256:1280], scalar=1.0 / wsz, in1=UF[:, 256:1280],
                                    op0=ALU.mult, op1=ALU.subtract), [ck, "UF"], ["PTg"])
                                for (c0, r0) in ((256, 0), (1272, 8)):
                                    op("dve", lambda cur=cur, c0=c0, r0=r0, g=g: nc.vector.tensor_tensor(
                                        out=tmp8[:], in0=cur[:, c0:c0 + 8], in1=rcb[:, g * 16 + r0:g * 16 + r0 + 8], op=ALU.mult),
                                       [ck, "rcb"], ["tmp8"])
                                    op("dve", lambda c0=c0, mi=mi: nc.vector.tensor_tensor(
                                        out=PTg[:, mi, c0 - 256:c0 - 248], in0=tmp8[:], in1=UF[:, c0:c0 + 8], op=ALU.subtract),
                                       ["tmp8", "UF"], ["PTg"])
                            for half in range(2):
                                oc = 2 * g + half
                                for nt in range(2):
                                    bi = nt
                                    for kc in range(2):
                                        op("pe", lambda kc=kc, half=half, nt=nt, bi=bi, g=g: nc.tensor.matmul(
                                            bank(bi, 512), lhsT=wp[:, g, kc, half * 128:(half + 1) * 128], rhs=PTg[:, kc, nt * 512:(nt + 1) * 512],
                                            start=(kc == 0), stop=(kc == 1)), ["wp", "PTg"], [bkey(bi)], signal=(kc == 1))
                                    op("act", lambda nt=nt, bi=bi, oc=oc: nc.scalar.activation(
                                        out=PMs[:, nt * 512:(nt + 1) * 512], in_=bank(bi, 512), func=AF.Identity, scale=psc[:, oc:oc + 1]),
                                       [bkey(bi), "psc"], ["PMs"])
                                dma("sp", lambda q, oc=oc: q.dma_start(out=pm_d[oc * 128:(oc + 1) * 128, :], in_=PMs[:]), ["PMs"], ["pm_d"])
                    T.wait_all("sp", ["XT", "wr0", "wr1", "wr2", "Vb", "QTs", "KTs", "PMs", "UF", "PA", "PB", "PTg", "xt0", "xt1", "xh"])

                CATT = sb(us, "CATT", [128, KC, 1024], BF16)
                with contextlib.ExitStack() as b:
                    QT = sb(b, "QT", [128, 8, 1024], BF16)
                    KT = sb(b, "KT", [128, 8, UT], BF16)
                    V = sb(b, "V", [128, 12, NH, VS], BF16)
                    MK = sb(b, "MK", [128, NMASK, 128], BF16)
                    BS = [sb(b, "BS%d" % i, [128, 7, 128], BF16) for i in range(2)]
                    QH = [sb(b, "QH%d" % i, [128, 1024], BF16) for i in range(2)]
                    PE_ = [sb(b, "PE%d" % i, [128, 768], BF16) for i in range(3)]
                    ATM = sb(b, "ATM", [128, 8, 1024], BF16)
                    rden = sb(b, "rden", [128, 8], F32)
                    CB = [sb(b, "CB%d" % i, [128, NMASK, 128], BF16) for i in range(2)]
                    dma("sp", lambda q: q.dma_start(out=QT[:], in_=qt_d.rearrange("(c p) t -> p c t", p=128)), ["qk_d"], ["QT"])
                    dma("sp", lambda q: q.dma_start(out=KT[:], in_=kt_d.rearrange("(c p) t -> p c t", p=128)), ["qk_d"], ["KT"])
                    dma("sp", lambda q: q.dma_start(out=V[:].rearrange("p t h s -> p t (h s)"),
                                                    in_=v_d.rearrange("(t p) f -> p t f", p=128)), ["v_d"], ["V"])
                    dma("pool", lambda q: q.dma_start(out=MK[:].rearrange("p m k -> p (m k)"), in_=mask_in[u * 128:(u + 1) * 128, :]),
                        writes=["MK"])
                    dma("sp", lambda q: q.dma_start(out=CATT[:, 8:16, :], in_=pm_d.rearrange("(c p) t -> p c t", p=128)), ["pm_d"], ["CATTp"])
                    ei = 0
                    def head_prep(h):
                        bs = BS[h % 2]; bsk = "BS%d" % (h % 2)
                        qh = QH[h % 2]; qhk = "QH%d" % (h % 2)
                        cb = CB[h % 2]; cbk = "CB%d" % (h % 2)
                        dma("pool", lambda q: q.dma_start(out=bs[:].rearrange("p o k -> p (o k)"),
                                                          in_=bias_in[h * 128:(h + 1) * 128, :]), writes=[bsk])
                        op("dve", lambda: nc.vector.tensor_scalar(out=qh[:], in0=QT[:, h // 4, :], scalar1=hm[:, h % 4:h % 4 + 1],
                                                                  scalar2=None, op0=ALU.mult), ["QT", "cst"], [qhk])
                        for p in range(8):
                            offs = pair_offsets(p)
                            n_ = len(offs); mi0 = mask_index(p, 0); o0 = offs[0] + 3
                            op("dve", lambda: nc.vector.tensor_tensor(
                                out=cb[:, mi0:mi0 + n_, :], in0=MK[:, mi0:mi0 + n_, :], in1=bs[:, o0:o0 + n_, :], op=ALU.add),
                               ["MK", bsk], [cbk])

                    head_prep(0)
                    for h in range(NH):
                        bs = BS[h % 2]; bsk = "BS%d" % (h % 2)
                        qh = QH[h % 2]; qhk = "QH%d" % (h % 2)
                        po = PO[h % 2]; pok = "po%d" % (h % 2)
                        cb = CB[h % 2]; cbk = "CB%d" % (h % 2)
                        if h + 1 < NH:
                            head_prep(h + 1)

                        def pv_stage(p, pe_, pek, offs):
                            for i, o in enumerate(offs):
                                kt = 2 + p + o
                                op("pe", lambda i=i, kt=kt: nc.tensor.matmul(
                                    po[:, p * 64:p * 64 + 33], lhsT=pe_[:, i * 128:(i + 1) * 128], rhs=V[:, kt, h, 0:33],
                                    start=(i == 0), stop=(i == len(offs) - 1)), [pek, "V"], [pok], signal=(i == len(offs) - 1))

                        pending = None
                        for p in range(8):
                            offs = pair_offsets(p)
                            ps = PS[(h * 8 + p) % 2]; psk = "psS%d" % ((h * 8 + p) % 2)
                            for i, o in enumerate(offs):
                                kt0 = (2 + p + o) * 128
                                sl = ps[:, i * 128:(i + 1) * 128]
                                op("pe", lambda sl=sl, kt0=kt0: nc.tensor.matmul(
                                    sl, lhsT=KT[:, h // 4, kt0:kt0 + 128], rhs=qh[:, p * 128:(p + 1) * 128], start=True, stop=False),
                                   ["KT", qhk], [psk], signal=False)
                                mi_ = mask_index(p, i)
                                op("pe", lambda sl=sl, mi_=mi_: nc.tensor.matmul(
                                    sl, lhsT=cb[:, mi_, :], rhs=ident_b[:], start=False, stop=True), [cbk, "ident_b"], [psk],
                                   signal=(i == len(offs) - 1))
                            pe_ = PE_[ei % 3]; pek = "PE%d" % (ei % 3); ei += 1
                            n = len(offs) * 128
                            op("act", lambda: nc.scalar.activation(out=pe_[:, 0:512], in_=ps[:, 0:512], func=AF.Exp), [psk], [pek])
                            op("act", lambda: nc.scalar.activation(out=pe_[:, 512:n], in_=ps[:, 512:n], func=AF.Exp), [psk], [pek])
                            if pending is not None:
                                pv_stage(*pending)
                            pending = (p, pe_, pek, offs)
                        pv_stage(*pending)
                        pov = po[:].rearrange("p (a s) -> p a s", s=64)
                        op("dve", lambda pov=pov: nc.vector.reciprocal(out=rden[:].unsqueeze(2), in_=pov[:, :, 32:33]), [pok], ["rden"])
                        op("dve", lambda pov=pov, h=h: nc.vector.tensor_tensor(
                            out=ATM[:, :, h * 32:(h + 1) * 32], in0=pov[:, :, 0:32], in1=rden[:].unsqueeze(2).to_broadcast([128, 8, 32]),
                            op=ALU.mult), [pok, "rden"], ["ATM"])
                    for p in range(8):
                        for cg in range(2):
                            pt = PT[cg % 2]; pk = ptkey[cg % 2]
                            for ci in range(4):
                                c = cg * 4 + ci
                                op("pe", lambda c=c, ci=ci, pt=pt, p=p: nc.tensor.transpose(
                                    out=pt[:, ci * 128:(ci + 1) * 128], in_=ATM[:, p, c * 128:(c + 1) * 128], identity=ident_b[:]),
                                   ["ATM", "ident_b"], [pk], signal=(ci == 3))
                            op("dve", lambda cg=cg, pt=pt, p=p: nc.vector.tensor_copy(
                                out=CATT[:, cg * 4:(cg + 1) * 4, p * 128:(p + 1) * 128], in_=pt[:].rearrange("p (c t) -> p c t", c=4)),
                               [pk], ["CATTa"])
                    T.wait_all("sp", ["QT", "KT", "V", "MK", "BS0", "BS1", "QH0", "QH1", "PE0", "PE1", "PE2", "ATM", "rden"])

                with contextlib.ExitStack() as c_:
                    WO = sb(c_, "WO", [128, KC, D], BF16)
                    LB = sb(c_, "LB", [128, 4, D], F32)
                    WR = sb(c_, "WR", [128, KC, E], F32)
                    BR = sb(c_, "BR", [128, E], F32)
                    xt = [sb(c_, "cxt%d" % i, [128, D], F32) for i in range(1)]
                    HP = sb(c_, "HP", [128, D], F32)
                    H = [sb(c_, "H%d" % i, [128, D], F32) for i in range(2)]
                    HB = [sb(c_, "HB%d" % i, [128, D], BF16) for i in range(2)]
                    HT = sb(c_, "HT", [128, KC, 128], F32)
                    st6 = sb(c_, "cst6", [128, 24], F32)
                    mv = sb(c_, "cmv", [128, 2], F32)
                    rstd = sb(c_, "crstd", [128, 1], F32)
                    L = sb(c_, "L", [128, E], F32)
                    LW = sb(c_, "LW", [128, E], F32)
                    OH = sb(c_, "OH", [128, 4, E], F32)
                    SELf = sb(c_, "SELf", [128, E], F32)
                    SELb = sb(c_, "SELb", [128, E], BF16)
                    CUMb = sb(c_, "CUMb", [128, E], BF16)
                    MX = sb(c_, "MX", [128, 4], F32)
                    EX = sb(c_, "EX", [128, 4], F32)
                    nm0 = sb(c_, "nm0", [128, 1], F32)
                    ssum = sb(c_, "ssum", [128, 1], F32)
                    SLOT = sb(c_, "SLOT", [128, E], F32)
                    OKf = sb(c_, "OKf", [128, E], F32)
                    TMP = sb(c_, "TMP", [128, E], F32)
                    slf = sb(c_, "slf", [128, 4], F32)
                    w_out_v = w_out.rearrange("(k p) n -> p k n", p=128)
                    for j in range(4):
                        dma("pool", lambda q, j=j: q.dma_start(out=WO[:, :, j * 512:(j + 1) * 512], in_=w_out_v[:, :, j * 512:(j + 1) * 512]),
                            writes=["WO%d" % j])
                    for j in range(4):
                        dma("sp", lambda q, j=j: q.dma_start(out=LB[:, j, :], in_=lnv[j:j + 1, :].partition_broadcast(128)), writes=["LB%d" % j if j != 2 else "LB2_"])
                    dma("sp", lambda q: q.dma_start(out=WR[:], in_=w_router.rearrange("(k p) e -> p k e", p=128)), writes=["WR"])
                    dma("sp", lambda q: q.dma_start(out=BR[:], in_=b_router[0:1, :].partition_broadcast(128)), writes=["BR"])
                    def stage1(tt):
                        tg = u * 8 + tt
                        xb = xt[0]; xk = "cxt0"
                        Hc = H[tt % 2]; hk = "H%d" % (tt % 2)
                        HBc = HB[tt % 2]; hbk = "HB%d" % (tt % 2)
                        dma("sp", lambda q, tt=tt, xb=xb: q.dma_start(out=xb[:], in_=xu[256 + tt * 128:256 + (tt + 1) * 128, :]), writes=[xk])
                        for j in range(4):
                            for c in range(KC):
                                op("pe", lambda j=j, c=c, tt=tt: nc.tensor.matmul(
                                    bank(j, 512), lhsT=CATT[:, c, tt * 128:(tt + 1) * 128], rhs=WO[:, c, j * 512:(j + 1) * 512],
                                    start=(c == 0), stop=(c == KC - 1)), ["CATTa", "CATTp", "WO%d" % j], [bkey(j)], signal=(c == KC - 1))
                        op("dve", lambda xb=xb, tt=tt: nc.vector.tensor_scalar(out=xb[:], in0=xb[:], scalar1=stats[:, tt, 0:1], scalar2=stats[:, tt, 1:2],
                                                                              op0=ALU.subtract, op1=ALU.mult), [xk, "stats"], [xk])
                        op("dve", lambda xb=xb: nc.vector.tensor_tensor(out=xb[:], in0=xb[:], in1=LB[:, 0, :], op=ALU.mult), [xk, "LB0"], [xk])
                        op("dve", lambda xb=xb: nc.vector.tensor_tensor(out=xb[:], in0=xb[:], in1=LB[:, 1, :], op=ALU.add), [xk, "LB1"], [xk])
                        for j in range(4):
                            op("dve", lambda j=j, xb=xb: nc.vector.scalar_tensor_tensor(
                                out=HP[:, j * 512:(j + 1) * 512], in0=xb[:, j * 512:(j + 1) * 512], scalar=ALPHA, in1=bank(j, 512),
                                op0=ALU.mult, op1=ALU.add), [xk, bkey(j)], ["HP"])
                        ln_stats(HP, "HP", st6, mv[:], rstd[:], "c")
                        op("dve", lambda: nc.vector.tensor_scalar(out=HP[:], in0=HP[:], scalar1=mv[:, 0:1], scalar2=rstd[:],
                                                                  op0=ALU.subtract, op1=ALU.mult), ["HP", "cmv", "crstd"], ["HP"])
                        op("dve", lambda: nc.vector.tensor_tensor(out=HP[:], in0=HP[:], in1=LB[:, 2, :], op=ALU.mult), ["HP", "LB2_"], ["HP"])
                        op("dve", lambda Hc=Hc: nc.vector.tensor_tensor(out=Hc[:], in0=HP[:], in1=LB[:, 3, :], op=ALU.add), ["HP", "LB3"], [hk])
                        dma("sp", lambda q, tg=tg, Hc=Hc: q.dma_start(out=h_d[tg * 128:(tg + 1) * 128, :], in_=Hc[:]), [hk], ["h_d"])
                        op("act", lambda Hc=Hc, HBc=HBc: nc.scalar.activation(out=HBc[:], in_=Hc[:], func=AF.Copy), [hk], [hbk])

                    def stage2(tt):
                        tg = u * 8 + tt
                        Hc = H[tt % 2]; hk = "H%d" % (tt % 2)
                        HBc = HB[tt % 2]; hbk = "HB%d" % (tt % 2)
                        for cg in range(4):
                            bi = 4 + (cg % 2)
                            for ci in range(4):
                                c = cg * 4 + ci
                                op("pe", lambda c=c, ci=ci, bi=bi, Hc=Hc: nc.tensor.transpose(
                                    out=bank(bi, 512)[:, ci * 128:(ci + 1) * 128], in_=Hc[:, c * 128:(c + 1) * 128], identity=ident_f),
                                   [hk, "cst"], [bkey(bi)], signal=(ci == 3))
                            op("act", lambda cg=cg, bi=bi: nc.scalar.activation(
                                out=HT[:, cg * 4:(cg + 1) * 4, :], in_=bank(bi, 512).rearrange("p (c t) -> p c t", c=4), func=AF.Copy),
                               [bkey(bi)], ["HT"])
                        for c in range(KC):
                            op("pe", lambda c=c: nc.tensor.matmul(bank(4, E), lhsT=HT[:, c, :], rhs=WR[:, c, :], start=(c == 0), stop=(c == KC - 1)),
                               ["HT", "WR"], [bkey(4)], signal=(c == KC - 1))
                        op("dve", lambda: nc.vector.tensor_tensor(out=L[:], in0=bank(4, E), in1=BR[:], op=ALU.add), [bkey(4), "BR"], ["L"])
                        op("dve", lambda: nc.vector.tensor_copy(out=LW[:], in_=L[:]), ["L"], ["LW"])
                        for k in range(4):
                            op("dve", lambda k=k: nc.vector.reduce_max(out=MX[:, k:k + 1], in_=LW[:], axis=AX.X), ["LW"], ["MX"])
                            op("dve", lambda k=k: nc.vector.tensor_scalar(out=OH[:, k, :], in0=LW[:], scalar1=MX[:, k:k + 1], scalar2=None,
                                                                         op0=ALU.is_equal), ["LW", "MX"], ["OH"])
                            op("dve", lambda k=k: nc.vector.scalar_tensor_tensor(out=LW[:], in0=OH[:, k, :], scalar=-1e30, in1=LW[:],
                                                                                op0=ALU.mult, op1=ALU.add), ["OH", "LW"], ["LW"])
                        op("dve", lambda: nc.vector.tensor_tensor(out=SELf[:], in0=OH[:, 0, :], in1=OH[:, 1, :], op=ALU.add), ["OH"], ["SELf"])
                        op("dve", lambda: nc.vector.tensor_tensor(out=SELf[:], in0=SELf[:], in1=OH[:, 2, :], op=ALU.add), ["OH", "SELf"], ["SELf"])
                        op("dve", lambda: nc.vector.tensor_tensor(out=SELf[:], in0=SELf[:], in1=OH[:, 3, :], op=ALU.add), ["OH", "SELf"], ["SELf"])
                        op("dve", lambda: nc.vector.tensor_copy(out=SELb[:], in_=SELf[:]), ["SELf"], ["SELb"])
                        op("dve", lambda: nc.vector.tensor_copy(out=CUMb[:], in_=cum[:]), ["cum"], ["CUMb"])
                        op("dve", lambda: nc.vector.tensor_scalar(out=nm0[:], in0=MX[:, 0:1], scalar1=-1.0, scalar2=None, op0=ALU.mult), ["MX"], ["nm0"])
                        op("act", lambda: nc.scalar.activation(out=EX[:], in_=MX[:], func=AF.Exp, bias=nm0[:], scale=1.0), ["MX", "nm0"], ["EX"])
                        op("dve", lambda: nc.vector.reduce_sum(out=ssum[:], in_=EX[:], axis=AX.X), ["EX"], ["ssum"])
                        op("dve", lambda: nc.vector.reciprocal(out=ssum[:], in_=ssum[:]), ["ssum"], ["ssum"])
                        op("dve", lambda tg=tg: nc.vector.tensor_scalar(out=GT[:, tg, :], in0=EX[:], scalar1=ssum[:], scalar2=None, op0=ALU.mult),
                           ["EX", "ssum"], ["GT"])
                        op("pe", lambda: nc.tensor.matmul(bank(5, E), lhsT=ustr_b[:], rhs=SELb[:], start=True, stop=False),
                           ["ustr_b", "SELb"], [bkey(5)], signal=False)
                        op("pe", lambda: nc.tensor.matmul(bank(5, E), lhsT=ones_b[:], rhs=CUMb[:], start=False, stop=True),
                           ["ones_b", "CUMb"], [bkey(5)], signal=True)
                        op("dve", lambda: nc.vector.tensor_scalar(out=OKf[:], in0=bank(5, E), scalar1=float(C), scalar2=None, op0=ALU.is_lt),
                           [bkey(5)], ["OKf"])
                        op("dve", lambda: nc.vector.tensor_tensor(out=SLOT[:], in0=bank(5, E), in1=ebase, op=ALU.add), [bkey(5), "cst"], ["SLOT"])
                        op("dve", lambda: nc.vector.tensor_scalar(out=SLOT[:], in0=SLOT[:], scalar1=-float(DUMP), scalar2=None, op0=ALU.add),
                           ["SLOT"], ["SLOT"])
                        op("dve", lambda: nc.vector.tensor_tensor(out=SLOT[:], in0=SLOT[:], in1=OKf[:], op=ALU.mult), ["SLOT", "OKf"], ["SLOT"])
                        op("dve", lambda: nc.vector.tensor_scalar(out=SLOT[:], in0=SLOT[:], scalar1=float(DUMP), scalar2=None, op0=ALU.add),
                           ["SLOT"], ["SLOT"])
                        op("dve", lambda: nc.vector.tensor_tensor(out=cum[:], in0=cum[:], in1=SELf[:], op=ALU.add), ["cum", "SELf", "CUMb"], ["cum"])
                        for k in range(4):
                            op("dve", lambda k=k: nc.vector.tensor_tensor(out=TMP[:], in0=OH[:, k, :], in1=SLOT[:], op=ALU.mult), ["OH", "SLOT"], ["TMP"])
                            op("dve", lambda k=k: nc.vector.reduce_sum(out=slf[:, k:k + 1], in_=TMP[:], axis=AX.X), ["TMP"], ["slf"])
                        op("dve", lambda tg=tg: nc.vector.tensor_copy(out=SL[:, tg, :], in_=slf[:]), ["slf"], ["SL"])
                        for k in range(4):
                            dma("pool", lambda q, k=k, tg=tg, HBc=HBc: q.indirect_dma_start(
                                out=xg_d[:, :], out_offset=bass.IndirectOffsetOnAxis(ap=SL[:, tg, k:k + 1], axis=0),
                                in_=HBc[:], in_offset=None), [hbk, "SL"], ["xg_d"])

                    for i_ in range(9):
                        if i_ < 8:
                            stage1(i_)
                        if i_ >= 1:
                            stage2(i_ - 1)
                    T.wait_all("sp", ["WO", "LB", "WR", "BR", "cxt0", "cxt1", "HP", "H0", "H1", "HB0", "HB1", "HT", "CATTa", "CATTp",
                                      "L", "LW", "OH", "SELf", "SELb", "CUMb", "MX", "EX", "nm0", "ssum", "SLOT", "OKf", "TMP", "slf", "stats"])

        with contextlib.ExitStack() as m_:
            XG = [sb(m_, "XG%d" % i, [128, D], BF16) for i in range(2)]
            XGT = sb(m_, "XGT", [128, KC, C], BF16)
            ACTT = sb(m_, "ACTT", [128, FC, C], BF16)
            wr = [sb(m_, "mw%d" % i, [128, KC, 512], BF16) for i in range(3)]
            BG = sb(m_, "BG", [128, E * 2 * FC], F32)
            BG1 = sb(m_, "BG1", [128, E * 2 * FC], F32)
            bdf = [sb(m_, "bdf%d" % i, [1, D], F32) for i in range(2)]
            bdb = [sb(m_, "bdb%d" % i, [1, D], BF16) for i in range(2)]
            GL = [sb(m_, "GL%d" % i, [128, 512], F32) for i in range(4)]
            SG = [sb(m_, "SG%d" % i, [128, 512], F32) for i in range(4)]
            TU = [sb(m_, "TU%d" % i, [128, 512], F32) for i in range(4)]
            YS = [sb(m_, "YS%d" % i, [128, 512], F32) for i in range(4)]
            dma("sp", lambda q: q.dma_start(out=BG[:], in_=bgcol), writes=["BG"])
            op("dve", lambda: nc.vector.tensor_scalar(out=BG1[:], in0=BG[:], scalar1=1.0, scalar2=None, op0=ALU.add), ["BG"], ["BG1"])
            MB = 2 if FC >= 2 else 1
            wslot = 0
            epi = 0
            ysi = 0
            for e in range(E):
                w_gu_v = w_gu[e * D:(e + 1) * D, :].rearrange("(k p) (g n) -> p k g n", p=128, g=2)
                w_dn_v = w_dn[e * DFF:(e + 1) * DFF, :].rearrange("(k p) n -> p k n", p=128)
                dma("sp", lambda q, e=e: q.dma_start(out=bdf[e % 2][:], in_=b_dn[e:e + 1, :]), writes=["bdf%d" % (e % 2)])
                op("dve", lambda e=e: nc.vector.tensor_copy(out=bdb[e % 2][:], in_=bdf[e % 2][:]), ["bdf%d" % (e % 2)], ["bdb%d" % (e % 2)])
                for st in range(CT):
                    xg = XG[st % 2]; xgk = "XG%d" % (st % 2)
                    r0 = e * C + st * 128
                    dma("sp", lambda q, r0=r0, xg=xg: q.dma_start(out=xg[:], in_=xg_d[r0:r0 + 128, :]), ["xg_d"], [xgk])
                    for cg in range(4):
                        pt = PT[cg % 2]; pk = ptkey[cg % 2]
                        for ci in range(4):
                            c = cg * 4 + ci
                            op("pe", lambda c=c, ci=ci, pt=pt, xg=xg: nc.tensor.transpose(
                                out=pt[:, ci * 128:(ci + 1) * 128], in_=xg[:, c * 128:(c + 1) * 128], identity=ident_b[:]),
                               [xgk, "ident_b"], [pk], signal=(ci == 3))
                        eng = "dve" if cg % 2 == 0 else "act"
                        if eng == "dve":
                            op("dve", lambda cg=cg, pt=pt, st=st: nc.vector.tensor_copy(
                                out=XGT[:, cg * 4:(cg + 1) * 4, st * 128:(st + 1) * 128], in_=pt[:].rearrange("p (c t) -> p c t", c=4)),
                               [pk], ["XGT"])
                        else:
                            op("act", lambda cg=cg, pt=pt, st=st: nc.scalar.activation(
                                out=XGT[:, cg * 4:(cg + 1) * 4, st * 128:(st + 1) * 128], in_=pt[:].rearrange("p (c t) -> p c t", c=4),
                                func=AF.Copy), [pk], ["XGT"])
                for pj in range(FC // MB):
                    wb = wr[wslot % 3]; wk = "mw%d" % (wslot % 3); wslot += 1
                    for g in range(2):
                        dma("pool", lambda q, pj=pj, g=g, wb=wb: q.dma_start(
                            out=wb[:, :, g * 128 * MB:(g + 1) * 128 * MB], in_=w_gu_v[:, :, g, pj * 128 * MB:(pj + 1) * 128 * MB]), writes=[wk + "g%d" % g])
                    for mi in range(MB):
                        m = pj * MB + mi
                        bgc = (e * 2 + 0) * FC + m
                        buc = (e * 2 + 1) * FC + m
                        for (n0, nn) in cfg.NS:
                            bg_, bu_ = (0, 1) if epi % 2 == 0 else (2, 3)
                            gl = GL[epi % 4]; sg = SG[epi % 4]; tu = TU[epi % 4]
                            glk, sgk, tuk = "GL%d" % (epi % 4), "SG%d" % (epi % 4), "TU%d" % (epi % 4)
                            epi += 1
                            for g, bb in ((0, bg_), (1, bu_)):
                                for k in range(KC):
                                    op("pe", lambda k=k, g=g, bb=bb, mi=mi, n0=n0, nn=nn, wb=wb: nc.tensor.matmul(
                                        bank(bb, nn), lhsT=wb[:, k, g * 128 * MB + mi * 128:g * 128 * MB + (mi + 1) * 128],
                                        rhs=XGT[:, k, n0:n0 + nn], start=(k == 0), stop=(k == KC - 1)),
                                       [wk + "g%d" % g, "XGT"], [bkey(bb)], signal=(k == KC - 1))
                            op("dve", lambda gl=gl, bg_=bg_, nn=nn, bgc=bgc: nc.vector.tensor_scalar(
                                out=gl[:, 0:nn], in0=bank(bg_, nn), scalar1=BG[:, bgc:bgc + 1], scalar2=7.0, op0=ALU.add, op1=ALU.min),
                               [bkey(bg_), "BG"], [glk])
                            op("act", lambda gl=gl, sg=sg, nn=nn: nc.scalar.activation(out=sg[:, 0:nn], in_=gl[:, 0:nn], func=AF.Sigmoid, scale=1.702),
                               [glk], [sgk])
                            op("dve", lambda tu=tu, bu_=bu_, nn=nn, buc=buc: nc.vector.tensor_scalar(
                                out=tu[:, 0:nn], in0=bank(bu_, nn), scalar1=BG1[:, buc:buc + 1], scalar2=8.0, op0=ALU.add, op1=ALU.min),
                               [bkey(bu_), "BG1"], [tuk])
                            op("dve", lambda tu=tu, gl=gl, nn=nn: nc.vector.scalar_tensor_tensor(
                                out=tu[:, 0:nn], in0=tu[:, 0:nn], scalar=-6.0, in1=gl[:, 0:nn], op0=ALU.max, op1=ALU.mult),
                               [tuk, glk], [tuk])
                            op("dve", lambda tu=tu, sg=sg, nn=nn, m=m, n0=n0: nc.vector.tensor_tensor(
                                out=ACTT[:, m, n0:n0 + nn], in0=tu[:, 0:nn], in1=sg[:, 0:nn], op=ALU.mult), [tuk, sgk], ["ACTT"])
                for j in range(4):
                    wb = wr[wslot % 3]; wk = "mw%d" % (wslot % 3); wslot += 1
                    dma("pool", lambda q, j=j, wb=wb: q.dma_start(out=wb[:, 0:FC, :], in_=w_dn_v[:, :, j * 512:(j + 1) * 512]), writes=[wk + "g0", wk + "g1"])
                    for st in range(CT):
                        bi = 4 + (ysi % 2)
                        ys = YS[ysi % 4]; ysk = "YS%d" % (ysi % 4); ysi += 1
                        for m in range(FC):
                            op("pe", lambda m=m, st=st, bi=bi, wb=wb: nc.tensor.matmul(
                                bank(bi, 512), lhsT=ACTT[:, m, st * 128:(st + 1) * 128], rhs=wb[:, m, :], start=(m == 0), stop=False),
                               ["ACTT", wk + "g0", wk + "g1"], [bkey(bi)], signal=False)
                        op("pe", lambda bi=bi, j=j, e=e: nc.tensor.matmul(
                            bank(bi, 512), lhsT=ones_b[0:1, :], rhs=bdb[e % 2][0:1, j * 512:(j + 1) * 512], start=False, stop=True),
                           ["ones_b", "bdb%d" % (e % 2)], [bkey(bi)], signal=True)
                        if ysi % 2 == 0:
                            op("act", lambda ys=ys, bi=bi: nc.scalar.activation(out=ys[:], in_=bank(bi, 512), func=AF.Copy), [bkey(bi)], [ysk])
                        else:
                            op("dve", lambda ys=ys, bi=bi: nc.vector.tensor_copy(out=ys[:], in_=bank(bi, 512)), [bkey(bi)], [ysk])
                        r0 = e * C + st * 128
                        dma("sp", lambda q, r0=r0, j=j, ys=ys: q.dma_start(out=yy_d[r0:r0 + 128, j * 512:(j + 1) * 512], in_=ys[:]), [ysk], ["yy_d"])
            T.wait_all("sp", ["XG0", "XG1", "XGT", "ACTT", "mw0", "mw1", "mw2", "BG", "BG1", "bdf0", "bdf1", "bdb0", "bdb1",
                              "GL0", "GL1", "SG0", "SG1", "TU0", "TU1", "YS0", "YS1", "YS2", "YS3"])

        with contextlib.ExitStack() as f_:
            YG = [[sb(f_, "YG%d_%d" % (i, k), [128, D], F32) for k in range(4)] for i in range(2)]
            HH = [sb(f_, "HH%d" % i, [128, D], F32) for i in range(2)]
            OU = [sb(f_, "OU%d" % i, [128, D], F32) for i in range(2)]
            LB2 = sb(f_, "LB2", [128, 2, D], F32)
            st6 = sb(f_, "fst6", [128, 24], F32)
            mv = sb(f_, "fmv", [128, 2], F32)
            rstd = sb(f_, "frstd", [128, 1], F32)
            for j in range(2):
                dma("sp", lambda q, j=j: q.dma_start(out=LB2[:, j, :], in_=lnv[4 + j:5 + j, :].partition_broadcast(128)), writes=["LF%d" % j])
            for tg in range(NT):
                i = tg % 2
                hh = HH[i]; hhk = "HH%d" % i
                ou = OU[i]; ouk = "OU%d" % i
                dma("sp", lambda q, tg=tg, hh=hh: q.dma_start(out=hh[:], in_=h_d[tg * 128:(tg + 1) * 128, :]), ["h_d"], [hhk])
                for k in range(4):
                    dma("pool", lambda q, tg=tg, k=k, i=i: q.indirect_dma_start(
                        out=YG[i][k][:], out_offset=None, in_=yy_d[:, :],
                        in_offset=bass.IndirectOffsetOnAxis(ap=SL[:, tg, k:k + 1], axis=0)),
                        ["yy_d", "yy_dump", "SL"], ["YG%d_%d" % (i, k)])
                op("act", lambda hh=hh: nc.scalar.activation(out=hh[:], in_=hh[:], func=AF.Copy, scale=ALPHA), [hhk], [hhk])
                for k in range(4):
                    op("dve", lambda tg=tg, k=k, i=i, hh=hh: nc.vector.scalar_tensor_tensor(
                        out=hh[:], in0=YG[i][k][:], scalar=GT[:, tg, k:k + 1], in1=hh[:], op0=ALU.mult, op1=ALU.add),
                       ["YG%d_%d" % (i, k), "GT", hhk], [hhk])
                ln_stats(hh, hhk, st6, mv[:], rstd[:], "f")
                op("dve", lambda hh=hh: nc.vector.tensor_scalar(out=hh[:], in0=hh[:], scalar1=mv[:, 0:1], scalar2=rstd[:],
                                                                op0=ALU.subtract, op1=ALU.mult), [hhk, "fmv", "frstd"], [hhk])
                op("dve", lambda hh=hh: nc.vector.tensor_tensor(out=hh[:], in0=hh[:], in1=LB2[:, 0, :], op=ALU.mult), [hhk, "LF0"], [hhk])
                op("dve", lambda hh=hh, ou=ou: nc.vector.tensor_tensor(out=ou[:], in0=hh[:], in1=LB2[:, 1, :], op=ALU.add), [hhk, "LF1"], [ouk])
                dma("sp", lambda q, tg=tg, ou=ou: q.dma_start(out=y_out[tg * 128:(tg + 1) * 128, :], in_=ou[:]), [ouk], ["y_out"])
            keys = ["y_out", "OU0", "OU1", "HH0", "HH1", "LB2"] + ["YG%d_%d" % (i, k) for i in range(2) for k in range(4)]
            T.wait_all("sp", keys)
            for q in ("sp", "pool", "act"):
                for name in T.dq[q][0]:
                    if T.cnt[name]:
                        nc.sync.wait_ge(T.sem[name], T.cnt[name])
    return nc


def _unit_table(seq_rows):
    units = []
    for si, R in enumerate(seq_rows):
        for b in range(R // 16):
            units.append((si, b))
    return units


def _geometry(R, b):
    mask = np.full((128, NMASK, 128), NEG, np.float32)
    c = np.arange(64)
    cs = np.clip(c - 8, 0, 48)
    colok = (c[None, :] >= cs[:, None]) & (c[None, :] < cs[:, None] + 16)
    for p in range(8):
        for i, o in enumerate(pair_offsets(p)):
            mi = mask_index(p, i)
            for qr in range(2):
                r = 16 * b + 2 * p + qr
                rs = min(max(r - 4, 0), R - 8)
                for kr in range(2):
                    ka = 16 * b + 2 * p + 2 * o + kr
                    if 0 <= ka < R and rs <= ka < rs + 8:
                        blk = np.where(colok, 0.0, NEG).astype(np.float32)
                        mask[qr * 64:(qr + 1) * 64, mi, kr * 64:(kr + 1) * 64] = blk
    L = R * 64
    t0 = 16 * b * 64
    pv = np.zeros(16, np.float32)
    for j in range(8):
        pv[j] = 1.0 if 0 <= t0 - 8 + j < L else 0.0
        pv[8 + j] = 1.0 if 0 <= t0 + 1024 + j < L else 0.0
    rc = np.zeros(64, np.float32)
    for g, w in enumerate((2, 4, 8, 16)):
        half = w // 2
        for j in range(8):
            for (off, tt) in ((0, t0 + j), (8, t0 + 1016 + j)):
                lo = min(max(tt - half, 0), L)
                hi = min(max(tt + half, 0), L)
                rc[g * 16 + off + j] = 1.0 / float(hi - lo)
    return mask, pv, rc


def _bias_table(rpb):
    H = rpb.shape[0]
    out = np.zeros((H, 128, 7, 128), np.float32)
    c = np.arange(64)
    dc = c[None, :] - c[:, None] + 15
    okc = (dc >= 0) & (dc <= 30)
    dcc = np.clip(dc, 0, 30)
    for o in range(-3, 4):
        for qr in range(2):
            for kr in range(2):
                dr = 2 * o + kr - qr + 7
                if 0 <= dr <= 14:
                    blk = np.where(okc[None], rpb[:, dr, :][:, dcc], 0.0)
                    out[:, qr * 64:(qr + 1) * 64, o + 3, kr * 64:(kr + 1) * 64] = blk
    return out.reshape(H * 128, 7 * 128)


def prepare_inputs(cfg, xs, p):
    E, DFF, FC, NU, C = cfg.E, cfg.DFF, cfg.FC, cfg.NU, cfg.C
    seq_rows = [x.shape[0] // GRID_W for x in xs]
    units = _unit_table(seq_rows)
    assert len(units) == NU * cfg.NCORES, (len(units), NU, cfg.NCORES)
    f32 = np.float32
    lnv = np.stack([p["ln_in_g"], p["ln_in_b"], p["ln1_g"], p["ln1_b"], p["ln2_g"], p["ln2_b"]]).astype(f32)
    lncol = np.concatenate([p["ln_in_g"].reshape(16, 128).T, p["ln_in_b"].reshape(16, 128).T], axis=1).astype(f32)
    pscol = np.ascontiguousarray(p["pool_scale"].reshape(8, 128).T).astype(f32)
    bgcol = np.ascontiguousarray(p["b_gate_up"].reshape(E * 2 * FC, 128).T).astype(f32)
    consts = np.zeros((128, 128 * 3 + 8 + E), f32)
    consts[:, 0:128] = np.eye(128, dtype=f32)
    consts[:, 128:256] = np.triu(np.ones((128, 128), f32), 1)
    consts[:, 256:384] = 1.0
    for j in range(4):
        consts[32 * j:32 * (j + 1), 384 + j] = 1.0
    consts[:, 392:392 + E] = (np.arange(E, dtype=f32) * C)[None, :]
    shared = {
        "bias_in": _bias_table(p["rpb"].astype(f32)), "lnv": lnv, "lncol": np.ascontiguousarray(lncol),
        "w_in": p["w_in"], "w_pool": p["w_pool"].reshape(4 * 256, 256), "pscol": pscol, "w_out": p["w_out"],
        "w_router": p["w_router"], "b_router": p["b_router"].reshape(1, E), "w_gu": p["w_gate_up"].reshape(E * D, 2 * DFF),
        "bgcol": bgcol, "w_dn": p["w_down"].reshape(E * DFF, D), "b_dn": p["b_down"].reshape(E, D), "consts": consts,
    }
    in_maps = []
    for c in range(cfg.NCORES):
        xin = np.zeros((NU * UT, D), f32)
        masks = np.zeros((NU * 128, NMASK * 128), f32)
        pvs = np.zeros((NU, 16), f32)
        rcs = np.zeros((NU, 64), f32)
        for ui in range(NU):
            si, b = units[c * NU + ui]
            R = seq_rows[si]
            t_lo = (16 * b - 4) * 64
            t_hi = t_lo + UT
            a, bnd = max(t_lo, 0), min(t_hi, R * 64)
            xin[ui * UT + (a - t_lo): ui * UT + (bnd - t_lo)] = xs[si][a:bnd]
            mk, pv, rc = _geometry(R, b)
            masks[ui * 128:(ui + 1) * 128] = mk.reshape(128, NMASK * 128)
            pvs[ui] = pv
            rcs[ui] = rc
        m = dict(shared)
        m.update({"x_in": xin, "mask_in": masks, "pv_in": pvs, "rc_in": rcs})
        in_maps.append(m)
    return in_maps, units


def assemble(cfg, results, units, seq_lens):
    outs = [np.zeros((L, D), np.float32) for L in seq_lens]
    for c in range(cfg.NCORES):
        y = results[c]["y_out"]
        for ui in range(cfg.NU):
            si, b = units[c * cfg.NU + ui]
            outs[si][b * 1024:(b + 1) * 1024] = y[ui * 1024:(ui + 1) * 1024]
    return outs


_NC_CACHE = {}


def run(cfg, xs, p):
    key = (cfg.E, cfg.DFF, cfg.NU, cfg.NCORES, cfg.C)
    if key not in _NC_CACHE:
        _NC_CACHE[key] = build(cfg)
    nc = _NC_CACHE[key]
    in_maps, units = prepare_inputs(cfg, xs, p)
    res = run_bass_kernel_spmd(nc, in_maps, core_ids=list(range(cfg.NCORES)))
    return assemble(cfg, res.results, units, [x.shape[0] for x in xs])


def kernel(x_prompt, x_sample, ln_in_g, ln_in_b, w_in, rpb, w_pool, pool_scale, w_out, ln1_g, ln1_b,
           w_router, b_router, w_gate_up, b_gate_up, w_down, b_down, ln2_g, ln2_b):
    cfg = Cfg()
    a = lambda v: np.asarray(v, dtype=np.float32)
    p = {"ln_in_g": a(ln_in_g), "ln_in_b": a(ln_in_b), "w_in": a(w_in)[0], "rpb": a(rpb)[0], "w_pool": a(w_pool)[0],
         "pool_scale": a(pool_scale)[0], "w_out": a(w_out)[0], "ln1_g": a(ln1_g)[0], "ln1_b": a(ln1_b)[0],
         "w_router": a(w_router)[0], "b_router": a(b_router)[0], "w_gate_up": a(w_gate_up)[0], "b_gate_up": a(b_gate_up)[0],
         "w_down": a(w_down)[0], "b_down": a(b_down)[0], "ln2_g": a(ln2_g)[0], "ln2_b": a(ln2_b)[0]}
    xp = a(x_prompt)
    xsm = a(x_sample)
    xs = [xp[i] for i in range(xp.shape[0])] + [xsm[i] for i in range(xsm.shape[0])]
    outs = run(cfg, xs, p)
    y_prompt = np.stack(outs[:xp.shape[0]]).astype(np.float32)
    y_sample = np.stack(outs[xp.shape[0]:]).astype(np.float32)
    return (y_prompt, y_sample)
```
